# Optimizing a Trainium2 kernel written in Bass

```python
import math
import jax
import jax.numpy as jnp
from jax import lax
import numpy as np

D_MODEL = 1024
BATCH = 8
SEQ = 4096
DEPTH = 4

N_EVEN = (DEPTH + 1) // 2
N_ODD = DEPTH // 2
SB_HEADS = 8
SB_HEAD_DIM = D_MODEL // 16
SB_WIDTH = SB_HEADS * SB_HEAD_DIM
Q_BLOCK = 128
ML_HEADS = 4
ML_HEAD_DIM = D_MODEL // 8
ML_WIDTH = ML_HEADS * ML_HEAD_DIM
ML_CHUNK = 64
CONV_WIDTH = 4
MIX_WIDTH = SB_WIDTH + ML_WIDTH
SPLITS = (SB_WIDTH, 2 * SB_WIDTH, 3 * SB_WIDTH, 3 * SB_WIDTH + 2 * ML_WIDTH,
          3 * SB_WIDTH + 3 * ML_WIDTH, 3 * SB_WIDTH + 4 * ML_WIDTH)
IN_COLS = 3 * SB_WIDTH + 4 * ML_WIDTH + 2 * ML_HEADS
RW_HEAD_DIM = 64
RW_HEADS = D_MODEL // RW_HEAD_DIM
DECAY_LORA = 64
AAA_LORA = 64
GATE_LORA = 128
D_FF = 4 * D_MODEL
NORM_EPS = 1e-6
GN_EPS = 64e-5

kernel_name = 'hybrid_stickbreak_mlstm_rwkv7'


def rmsnorm(x, g):
    xf = x.astype(jnp.float32)
    y = xf * lax.rsqrt(jnp.mean(xf * xf, axis=-1, keepdims=True) + NORM_EPS) * g
    return y.astype(x.dtype)


def causal_depthwise_conv(x, w):
    K, C = w.shape
    return lax.conv_general_dilated(
        x, w.astype(x.dtype)[:, None, :], window_strides=(1,), padding=[(K - 1, 0)],
        dimension_numbers=('NWC', 'WIO', 'NWC'), feature_group_count=C)


def stick_breaking_attention(q, k, v):
    S, d = q.shape[2], q.shape[3]
    scale = 1.0 / math.sqrt(d)
    outs = []
    for blk in range(S // Q_BLOCK):
        q0 = blk * Q_BLOCK
        q1 = q0 + Q_BLOCK
        qb = q[:, :, q0:q1]
        kb = k[:, :, :q1]
        vb = v[:, :, :q1]
        z = jnp.einsum('bhtd,bhsd->bhts', qb, kb) * scale
        t_idx = q0 + jnp.arange(Q_BLOCK)[:, None]
        s_idx = jnp.arange(q1)[None, :]
        causal = s_idx < t_idx
        log_keep = jnp.where(causal, jax.nn.log_sigmoid(-z), 0.0)
        suffix = lax.cumsum(log_keep, axis=3, reverse=True) - log_keep
        weights = jnp.where(causal, jnp.exp(jax.nn.log_sigmoid(z) + suffix), 0.0)
        outs.append(jnp.einsum('bhts,bhsd->bhtd', weights, vb))
    return jnp.concatenate(outs, axis=2)


def mlstm_chunkwise(q, k, v, log_i, log_f):
    B, H, S, d = q.shape
    nc = S // ML_CHUNK
    k = k * (d ** -0.5)
    tril = jnp.tril(jnp.ones((ML_CHUNK, ML_CHUNK), dtype=bool))

    def to_chunks(t):
        t = t.reshape(t.shape[:2] + (nc, ML_CHUNK) + t.shape[3:])
        return jnp.moveaxis(t, 2, 0)

    def step(carry, inp):
        C, n, m = carry
        qc, kc, vc, li, lf = inp
        b = jnp.cumsum(lf, axis=-1)
        dmat = jnp.where(tril, b[..., :, None] - b[..., None, :] + li[..., None, :], -jnp.inf)
        inter = b + m[..., None]
        m_t = jnp.maximum(jnp.max(dmat, axis=-1), inter)
        scores = jnp.einsum('bhtd,bhsd->bhts', qc, kc) * jnp.exp(dmat - m_t[..., None])
        w_inter = jnp.exp(inter - m_t)
        num = jnp.einsum('bhts,bhsd->bhtd', scores, vc) + \
            w_inter[..., None] * jnp.einsum('bhtd,bhde->bhte', qc, C)
        den = jnp.sum(scores, axis=-1) + w_inter * jnp.einsum('bhtd,bhd->bht', qc, n)
        h = num / jnp.maximum(jnp.abs(den), jnp.exp(-m_t))[..., None]
        b_last = b[..., -1]
        g = b_last[..., None] - b + li
        m_new = jnp.maximum(b_last + m, jnp.max(g, axis=-1))
        w_state = jnp.exp(b_last + m - m_new)
        w_tok = jnp.exp(g - m_new[..., None])
        C_new = w_state[..., None, None] * C + jnp.einsum('bhs,bhsd,bhse->bhde', w_tok, kc, vc)
        n_new = w_state[..., None] * n + jnp.einsum('bhs,bhsd->bhd', w_tok, kc)
        return (C_new, n_new, m_new), h

    init = (jnp.zeros((B, H, d, d), jnp.float32), jnp.zeros((B, H, d), jnp.float32),
            jnp.zeros((B, H), jnp.float32))
    _, h = lax.scan(step, init, (to_chunks(q), to_chunks(k), to_chunks(v),
                                 to_chunks(log_i), to_chunks(log_f)))
    return jnp.moveaxis(h, 0, 2).reshape(B, H, S, d)


def stickbreak_mlstm_mix(u, w_in, b_if, conv_w, head_g, w_out):
    B, S, _ = u.shape
    proj = u.astype(jnp.float32) @ w_in
    sb_q, sb_k, sb_v, ml_qk, ml_v, ml_o, ml_if = jnp.split(proj, SPLITS, axis=-1)

    def heads(t, n_heads):
        return t.reshape(B, S, n_heads, -1).transpose(0, 2, 1, 3)

    a_out = stick_breaking_attention(heads(sb_q, SB_HEADS), heads(sb_k, SB_HEADS),
                                     heads(sb_v, SB_HEADS))
    a_out = a_out.transpose(0, 2, 1, 3).reshape(B, S, SB_WIDTH)
    ml_qk = jax.nn.silu(causal_depthwise_conv(ml_qk, conv_w))
    ml_q, ml_k = jnp.split(ml_qk, 2, axis=-1)
    ml_if = ml_if + b_if
    log_i = ml_if[..., :ML_HEADS].transpose(0, 2, 1)
    log_f = jax.nn.log_sigmoid(ml_if[..., ML_HEADS:]).transpose(0, 2, 1)
    h = mlstm_chunkwise(heads(ml_q, ML_HEADS), heads(ml_k, ML_HEADS), heads(ml_v, ML_HEADS),
                        log_i, log_f)
    h = h.transpose(0, 2, 1, 3)
    h = h * lax.rsqrt(jnp.mean(h * h, axis=-1, keepdims=True) + NORM_EPS) * head_g
    h = h.reshape(B, S, ML_WIDTH) * jax.nn.sigmoid(ml_o)
    return jnp.concatenate([a_out, h], axis=-1) @ w_out


def rwkv7_time_mix(u, mu, w_rkv, w0, w1, w2, a0, a1, a2, g1, g2, k_k, k_a, r_k,
                   ln_g, ln_b, w_out):
    B, S, D = u.shape
    xf = u.astype(jnp.float32)
    x_prev = jnp.pad(xf, ((0, 0), (1, 0), (0, 0)))[:, :S]
    xx = x_prev - xf
    xr, xw, xk, xv, xa, xg = (xf + xx * mu[i] for i in range(6))
    r = xr @ w_rkv[0]
    k = xk @ w_rkv[1]
    v = xv @ w_rkv[2]
    log_w = -jax.nn.softplus(-(w0 + jnp.tanh(xw @ w1) @ w2)) - 0.5
    decay = jnp.exp(-jnp.exp(log_w))
    a = jax.nn.sigmoid(a0 + (xa @ a1) @ a2)
    gate = jax.nn.sigmoid(xg @ g1) @ g2

    def heads(t):
        return t.reshape(B, S, RW_HEADS, RW_HEAD_DIM)

    kk = heads(k * k_k)
    kk = kk * lax.rsqrt(jnp.maximum(jnp.sum(kk * kk, axis=-1, keepdims=True), 1e-24))
    k = k * (1.0 + (a - 1.0) * k_a)
    r, k, v, decay, a = heads(r), heads(k), heads(v), heads(decay), heads(a)

    def step(state, inp):
        r_t, w_t, k_t, v_t, kk_t, a_t = inp
        sa = jnp.einsum('bhvk,bhk->bhv', state, kk_t)
        state = state * w_t[:, :, None, :] - sa[..., None] * (kk_t * a_t)[:, :, None, :] \
            + v_t[..., None] * k_t[:, :, None, :]
        return state, jnp.einsum('bhvk,bhk->bhv', state, r_t)

    def time_major(t):
        return jnp.moveaxis(t, 1, 0)

    init = jnp.zeros((B, RW_HEADS, RW_HEAD_DIM, RW_HEAD_DIM), jnp.float32)
    _, y = lax.scan(step, init, (time_major(r), time_major(decay), time_major(k),
                                 time_major(v), time_major(kk), time_major(a)))
    y = jnp.moveaxis(y, 0, 1)
    mean = jnp.mean(y, axis=-1, keepdims=True)
    var = jnp.mean(jnp.square(y - mean), axis=-1, keepdims=True)
    y = (y - mean) * lax.rsqrt(var + GN_EPS) * ln_g + ln_b
    y = y + jnp.sum(r * k * r_k, axis=-1, keepdims=True) * v
    return (y.reshape(B, S, D) * gate) @ w_out


def squared_relu_mlp(u, w_up, w_down):
    return jnp.square(jax.nn.relu(u.astype(jnp.float32) @ w_up)) @ w_down


def setup_inputs(seed: int = 0) -> dict:
    key = jax.random.key(seed)
    ks = iter(jax.random.split(key, 32))
    D = D_MODEL

    def nrm(shape, scale):
        return jax.random.normal(next(ks), shape, jnp.float32) * scale

    def uni(shape, lo, hi):
        return jax.random.uniform(next(ks), shape, jnp.float32, lo, hi)

    x = nrm((BATCH, SEQ, D), 1.0)
    norm_g = 1.0 + nrm((DEPTH, 4, D), 0.02)
    e_w_in = nrm((N_EVEN, D, IN_COLS), D ** -0.5)
    e_b_if = jnp.concatenate([nrm((N_EVEN, ML_HEADS), 0.1),
                              uni((N_EVEN, ML_HEADS), 3.0, 6.0)], axis=-1)
    e_conv_w = nrm((N_EVEN, CONV_WIDTH, 2 * ML_WIDTH), CONV_WIDTH ** -0.5)
    e_head_g = 1.0 + nrm((N_EVEN, ML_HEADS, ML_HEAD_DIM), 0.02)
    e_w_out = nrm((N_EVEN, MIX_WIDTH, D), MIX_WIDTH ** -0.5)
    r_mu = uni((N_ODD, 6, D), 0.0, 1.0)
    r_w_rkv = nrm((N_ODD, 3, D, D), D ** -0.5)
    r_w0 = uni((N_ODD, D), -6.0, 1.0)
    r_w1 = nrm((N_ODD, D, DECAY_LORA), D ** -0.5)
    r_w2 = nrm((N_ODD, DECAY_LORA, D), 0.5 * DECAY_LORA ** -0.5)
    r_a0 = nrm((N_ODD, D), 0.1)
    r_a1 = nrm((N_ODD, D, AAA_LORA), D ** -0.5)
    r_a2 = nrm((N_ODD, AAA_LORA, D), 0.5 * AAA_LORA ** -0.5)
    r_g1 = nrm((N_ODD, D, GATE_LORA), D ** -0.5)
    r_g2 = nrm((N_ODD, GATE_LORA, D), GATE_LORA ** -0.5)
    r_k_k = 0.85 + nrm((N_ODD, D), 0.02)
    r_k_a = 1.0 + nrm((N_ODD, D), 0.02)
    r_r_k = nrm((N_ODD, RW_HEADS, RW_HEAD_DIM), 0.1)
    r_ln_g = 1.0 + nrm((N_ODD, RW_HEADS, RW_HEAD_DIM), 0.02)
    r_ln_b = nrm((N_ODD, RW_HEADS, RW_HEAD_DIM), 0.02)
    r_w_out = nrm((N_ODD, D, D), D ** -0.5)
    mlp_w_up = nrm((DEPTH, D, D_FF), D ** -0.5)
    mlp_w_down = nrm((DEPTH, D_FF, D), D_FF ** -0.5)
    return {'x': x, 'norm_g': norm_g, 'e_w_in': e_w_in, 'e_b_if': e_b_if,
            'e_conv_w': e_conv_w, 'e_head_g': e_head_g, 'e_w_out': e_w_out,
            'r_mu': r_mu, 'r_w_rkv': r_w_rkv, 'r_w0': r_w0, 'r_w1': r_w1, 'r_w2': r_w2,
            'r_a0': r_a0, 'r_a1': r_a1, 'r_a2': r_a2, 'r_g1': r_g1, 'r_g2': r_g2,
            'r_k_k': r_k_k, 'r_k_a': r_k_a, 'r_r_k': r_r_k, 'r_ln_g': r_ln_g,
            'r_ln_b': r_ln_b, 'r_w_out': r_w_out, 'mlp_w_up': mlp_w_up,
            'mlp_w_down': mlp_w_down}


def reference(x, norm_g, e_w_in, e_b_if, e_conv_w, e_head_g, e_w_out, r_mu, r_w_rkv,
              r_w0, r_w1, r_w2, r_a0, r_a1, r_a2, r_g1, r_g2, r_k_k, r_k_a, r_r_k,
              r_ln_g, r_ln_b, r_w_out, mlp_w_up, mlp_w_down):
    h = x
    for layer in range(DEPTH):
        g = norm_g[layer]
        u = rmsnorm(h, g[0])
        if layer % 2 == 0:
            e = layer // 2
            mix = stickbreak_mlstm_mix(u, e_w_in[e], e_b_if[e], e_conv_w[e], e_head_g[e],
                                       e_w_out[e])
        else:
            o = layer // 2
            mix = rwkv7_time_mix(u, r_mu[o], r_w_rkv[o], r_w0[o], r_w1[o], r_w2[o], r_a0[o],
                                 r_a1[o], r_a2[o], r_g1[o], r_g2[o], r_k_k[o], r_k_a[o],
                                 r_r_k[o], r_ln_g[o], r_ln_b[o], r_w_out[o])
        h = h + rmsnorm(mix, g[1]).astype(h.dtype)
        u = rmsnorm(h, g[2])
        ff = squared_relu_mlp(u, mlp_w_up[layer], mlp_w_down[layer])
        h = h + rmsnorm(ff, g[3]).astype(h.dtype)
    return h
```

```python
import numpy as np
from contextlib import ExitStack
import concourse.bass as bass
import concourse.mybir as mybir
from concourse.bass_utils import run_bass_kernel_spmd

F32 = mybir.dt.float32
F32R = mybir.dt.float32r
BF16 = mybir.dt.bfloat16
AF = mybir.ActivationFunctionType
ALU = mybir.AluOpType
AX = mybir.AxisListType

P = 128
D = 1024
DFF = 4096
SEQ = 4096
BATCH = 8
DEPTH = 4
NORM_EPS = 1e-6
GN_EPS = 64e-5
IN_COLS = 3592
NDS = 20
USE_F32R = True
import os as _os
RWSTOP = int(_os.environ.get('RWSTOP', '0'))
EVSTOP = int(_os.environ.get('EVSTOP', '0'))


class Buf:
    __slots__ = ("w", "r", "f32r")

    def __init__(self):
        self.w = None
        self.r = {}
        self.f32r = False


class View:
    __slots__ = ("ap", "buf")

    def __init__(self, ap, buf):
        self.ap = ap
        self.buf = buf


class TT:
    def __init__(self, h, buf=None):
        self.h = h
        self.buf = buf if buf is not None else Buf()
        self._subs = {}

    def __getitem__(self, key):
        return View(self.h[key], self.buf)

    def sub(self, key):
        if key not in self._subs:
            self._subs[key] = TT(self.h)
        return self._subs[key]

    def v(self, ap):
        return View(ap, self.buf)


class K:
    def __init__(self, nc, es):
        self.nc = nc
        self.es = es
        self.E = {"pe": nc.tensor, "act": nc.scalar, "dve": nc.vector, "pool": nc.gpsimd, "sp": nc.sync}
        self.esem = {}
        self.ecnt = {}
        for e in ("pe", "act", "dve", "pool"):
            self.esem[e] = es.enter_context(nc.semaphore("es_" + e))
            self.ecnt[e] = 0
        self.seen = {e: {} for e in self.E}
        self.dq = {}
        for q in ("sp", "pool", "act"):
            sems = [es.enter_context(nc.semaphore("ds_%s%d" % (q, i))) for i in range(NDS)]
            self.dq[q] = {"sems": sems, "cnt": [0] * NDS, "i": 0}
        self.semname = {}
        self.uid = 0
        self.n_ins = 0
        self.dead = False

    def stop(self, n):
        if RWSTOP == n:
            self.dead = True

    def sb(self, shape, dt, name=None, es=None):
        self.uid += 1
        h = (es or self.es).enter_context(self.nc.sbuf_tensor("%s_%d" % (name or "sb", self.uid), list(shape), dt))
        return TT(h)

    def ps(self, shape, dt=F32, name=None, es=None):
        self.uid += 1
        h = (es or self.es).enter_context(self.nc.psum_tensor("%s_%d" % (name or "ps", self.uid), list(shape), dt))
        return TT(h)

    def dram(self, name, shape, dt, kind="Internal"):
        return TT(self.nc.dram_tensor(name, list(shape), dt, kind=kind).ap())

    def _key(self, sem):
        return id(sem)

    def _waits(self, en, outs, ins, extra=()):
        need = {}

        def add(tok):
            if tok is None:
                return
            s, v = tok
            if en == "pe" and s is self.esem["pe"]:
                return
            k = id(s)
            if k not in need or need[k][1] < v:
                need[k] = (s, v)

        for x in ins:
            add(x.buf.w)
        for x in outs:
            add(x.buf.w)
            for t in x.buf.r.values():
                add(t)
        for t in extra:
            add(t)
        eng = self.E[en]
        seen = self.seen[en]
        for k, (s, v) in need.items():
            if seen.get(k, 0) < v:
                eng.wait_ge(s, v)
                seen[k] = v
                self.n_ins += 1

    def _done(self, tok, outs, ins):
        k = id(tok[0])
        for x in ins:
            x.buf.r[k] = tok
        for x in outs:
            x.buf.w = tok
            x.buf.r = {}

    def emit(self, en, fn, outs, ins):
        if self.dead:
            return
        outs = [o for o in outs if isinstance(o, View)]
        ins = [i for i in ins if isinstance(i, View)]
        self._waits(en, outs, ins)
        ins_obj = fn()
        self.ecnt[en] += 1
        tok = (self.esem[en], self.ecnt[en])
        ins_obj.then_inc(tok[0], 1)
        self.n_ins += 1
        self._done(tok, outs, ins)

    def dma(self, q, out, in_):
        if self.dead:
            return
        dq = self.dq[q]
        i = dq["i"] % NDS
        dq["i"] += 1
        sem = dq["sems"][i]
        self._waits(q, [out], [in_], extra=[(sem, dq["cnt"][i])] if dq["cnt"][i] else [])
        ins_obj = self.E[q].dma_start(out=out.ap, in_=in_.ap)
        dq["cnt"][i] += 16
        tok = (sem, dq["cnt"][i])
        ins_obj.then_inc(sem, 16)
        self.n_ins += 1
        self._done(tok, [out], [in_])

    def wait_all(self, en, views):
        self._waits(en, [], views)

    @staticmethod
    def _a(x):
        return x.ap if isinstance(x, View) else x

    @staticmethod
    def _o(x):
        return x.ap.bitcast(F32R) if (x.buf.f32r and USE_F32R) else x.ap

    def mm(self, out, lhsT, rhs, start=True, stop=True, r=None):
        if r is None:
            r = lhsT.buf.f32r and rhs.buf.f32r
        r = r and USE_F32R
        la, ra = (lhsT.ap.bitcast(F32R), rhs.ap.bitcast(F32R)) if r else (lhsT.ap, rhs.ap)
        self.emit("pe", lambda: self.nc.tensor.matmul(out.ap, lhsT=la, rhs=ra, start=start, stop=stop),
                  [out], [lhsT, rhs])

    def tr(self, out, in_, ident):
        self.emit("pe", lambda: self.nc.tensor.transpose(out.ap, in_.ap, ident.ap), [out], [in_, ident])

    def act(self, out, in_, func, bias=None, scale=None, accum=None):
        kw = {}
        if bias is not None:
            kw["bias"] = self._a(bias)
        if scale is not None:
            kw["scale"] = self._a(scale)
        if accum is not None:
            kw["accum_out"] = accum.ap
        self.emit("act", lambda: self.nc.scalar.activation(out=self._o(out), in_=in_.ap, func=func, **kw),
                  [out, accum], [in_, bias, scale])

    def tt(self, en, out, in0, in1, op):
        self.emit(en, lambda: self.E[en].tensor_tensor(out=self._o(out), in0=in0.ap, in1=in1.ap, op=op), [out], [in0, in1])

    def ts(self, en, out, in0, s1, op0, s2=None, op1=None, accum=None):
        kw = {}
        if op1 is not None:
            kw["op1"] = op1
        if accum is not None:
            kw["accum_out"] = accum.ap
        self.emit(en, lambda: self.E[en].tensor_scalar(out=self._o(out), in0=in0.ap, scalar1=self._a(s1),
                                                       scalar2=self._a(s2), op0=op0, **kw),
                  [out, accum], [in0, s1, s2])

    def stt(self, out, in0, scalar, in1, op0, op1, accum=None):
        kw = {}
        if accum is not None:
            kw["accum_out"] = accum.ap
        self.emit("dve", lambda: self.nc.vector.scalar_tensor_tensor(out=self._o(out), in0=in0.ap, scalar=self._a(scalar),
                                                                     in1=in1.ap, op0=op0, op1=op1, **kw),
                  [out, accum], [in0, scalar, in1])

    def copy(self, en, out, in_):
        if en == "act":
            self.emit("act", lambda: self.nc.scalar.copy(out=self._o(out), in_=in_.ap), [out], [in_])
        else:
            self.emit(en, lambda: self.E[en].tensor_copy(out=self._o(out), in_=in_.ap), [out], [in_])

    def recip(self, out, in_):
        self.emit("dve", lambda: self.nc.vector.reciprocal(out=out.ap, in_=in_.ap), [out], [in_])

    def memset(self, en, out, val):
        self.emit(en, lambda: self.E[en].memset(out.ap, val), [out], [])

    def scan(self, out, d0, d1, init, op0, op1):
        self.emit("dve", lambda: self.nc.vector.tensor_tensor_scan(out=out.ap, data0=d0.ap, data1=d1.ap,
                                                                   initial=self._a(init), op0=op0, op1=op1),
                  [out], [d0, d1, init])

    def reduce(self, out, in_, op, axis=AX.X):
        self.emit("dve", lambda: self.nc.vector.tensor_reduce(out=out.ap, in_=in_.ap, axis=axis, op=op), [out], [in_])

    def affsel(self, out, in_, pattern, cmp, fill, base, cm):
        self.emit("pool", lambda: self.nc.gpsimd.affine_select(out=self._o(out), in_=in_.ap, pattern=pattern, compare_op=cmp,
                                                               fill=fill, base=base, channel_multiplier=cm),
                  [out], [in_])


class Ring:
    def __init__(self, k, n, shape, dt, name, es=None, psum=False):
        self.t = [(k.ps if psum else k.sb)(shape, dt, name=name, es=es) for _ in range(n)]
        self.i = 0

    def next(self):
        t = self.t[self.i % len(self.t)]
        self.i += 1
        return t


class Prog:
    def __init__(self, S=SEQ, layers=None, dbg=None):
        self.S = S
        self.NT = S // P
        self.layers = list(range(DEPTH)) if layers is None else layers
        self.dbg = dbg or {}
        nc = bass.Bass("TRN2", target_bir_lowering=False)
        self.nc = nc
        self.es = ExitStack()
        self.k = K(nc, self.es)

    SHAPES = {
        "norm_g": [DEPTH, 4, D], "e_w_in": [2, D, IN_COLS], "e_b_if": [2, 8], "e_conv_w": [2, 4, 1024],
        "e_head_g": [2, 4, 128], "e_w_out": [2, D, D], "r_mu": [2, 6, D], "r_w_rkv": [2, 3, D, D],
        "r_w0": [2, D], "r_w1": [2, D, 64], "r_w2": [2, 64, D], "r_a0": [2, D], "r_a1": [2, D, 64],
        "r_a2": [2, 64, D], "r_g1": [2, D, 128], "r_g2": [2, 128, D], "r_k_k": [2, D], "r_k_a": [2, D],
        "r_r_k": [2, D], "r_ln_g": [2, D], "r_ln_b": [2, D], "r_w_out": [2, D, D],
        "mlp_w_up": [DEPTH, D, DFF], "mlp_w_down": [DEPTH, DFF, D],
    }

    def __getattr__(self, name):
        if name in Prog.SHAPES:
            t = self.k.dram(name, Prog.SHAPES[name], F32, kind="ExternalInput")
            self.used.append(name)
            setattr(self, name, t)
            return t
        raise AttributeError(name)

    def declare(self):
        k = self.k
        S = self.S
        self.used = []
        self.x = k.dram("x", [S, D], F32, kind="ExternalInput")
        self.out = k.dram("out", [S, D], F32, kind="ExternalOutput")
        self.hA = k.dram("hA", [S, D], F32)
        self.hB = k.dram("hB", [S, D], F32)

    def in_map(self, inputs, b):
        m = {"x": np.ascontiguousarray(inputs["x"][b, :self.S])}
        for name in self.used:
            m[name] = np.ascontiguousarray(inputs[name]).reshape(Prog.SHAPES[name])
        return m

    def barrier(self):
        k = self.k
        toks = [(k.esem[e], k.ecnt[e]) for e in k.esem if k.ecnt[e]]
        for q in k.dq.values():
            for s_, c_ in zip(q["sems"], q["cnt"]):
                if c_:
                    toks.append((s_, c_))
        for en in ("pe", "act", "dve", "pool", "sp"):
            seen = k.seen[en]
            for s_, v_ in toks:
                if seen.get(id(s_), 0) < v_:
                    k.E[en].wait_ge(s_, v_)
                    seen[id(s_)] = v_
                    k.n_ins += 1

    def consts(self):
        k = self.k
        nc = self.nc
        self.ident_f = k.sb([P, P], F32, "identf")
        self.ident_b = k.sb([P, P], BF16, "identb")
        k.memset("pool", self.ident_f[:, :], 1.0)
        k.affsel(self.ident_f[:, :], self.ident_f[:, :], [[-1, P]], ALU.is_equal, 0.0, 0, 1)
        k.copy("dve", self.ident_b[:, :], self.ident_f[:, :])
        self.eps_t = k.sb([P, 1], F32, "eps")
        k.memset("dve", self.eps_t[:, :], NORM_EPS)

    def bcast_load(self, dst, src_ap, n):
        self.k.dma("sp", dst, View(src_ap.partition_broadcast(P), Buf()))

    def rstd(self, out, ss, dn, eps_t=None):
        k = self.k
        k.act(out, ss, AF.Ln, bias=(eps_t or self.eps_t)[:, 0:1], scale=1.0 / dn)
        k.act(out, out, AF.Exp, scale=-0.5)

    def norm_T(self, hsrc, ti, ht, hn, ss, rs, junk, tps, uT_dst):
        k = self.k
        k.dma("sp", ht[:, :], hsrc.sub(ti)[ti * P:(ti + 1) * P, :])
        k.act(View(junk.h[:, :], Buf()), ht[:, :], AF.Square, accum=ss[:, 0:1])
        self.rstd(rs[:, 0:1], ss[:, 0:1], D)
        k.act(hn[:, :], ht[:, :], AF.Copy, scale=rs[:, 0:1])
        for c in range(8):
            k.tr(tps[:, c, :], hn[:, c * P:(c + 1) * P], self.ident_b[:, :])
        k.copy("dve", uT_dst, tps[:, :, :])

    def norm_part(self, hsrc, ti, ht, hn, ss, rs, junk):
        k = self.k
        k.dma("sp", ht[:, :], hsrc.sub(ti)[ti * P:(ti + 1) * P, :])
        k.act(View(junk.h[:, :], Buf()), ht[:, :], AF.Square, accum=ss[:, 0:1])
        self.rstd(rs[:, 0:1], ss[:, 0:1], D)
        k.act(hn[:, :], ht[:, :], AF.Copy, scale=rs[:, 0:1])

    def tr_part(self, hn, tps, uT_dst):
        k = self.k
        for c in range(8):
            k.tr(tps[:, c, :], hn[:, c * P:(c + 1) * P], self.ident_b[:, :])
        k.copy("dve", uT_dst, tps[:, :, :])

    def mlp(self, layer, hsrc, hdst):
        k = self.k
        nc = self.nc
        with ExitStack() as es:
            wu = k.sb([P, 8, DFF], BF16, "wu", es)
            wd = k.sb([P, 32, D], BF16, "wd", es)
            g2 = k.sb([P, 8], F32, "g2", es)
            g3b = k.sb([P, D], F32, "g3b", es)
            stage = Ring(k, 2, [P, 2048], F32, "stg", es)
            with nc.allow_non_contiguous_dma(reason="tiny param"):
                k.dma("sp", g2[:, :], self.norm_g.v(self.norm_g.h[layer, 2, :].rearrange("(c p) -> p c", p=P)))
            k.dma("sp", g3b[:, :], self.norm_g.v(self.norm_g.h[layer, 3, :].partition_broadcast(P)))
            n = 0
            for kd in range(8):
                for hf in range(2):
                    st = stage.next()
                    k.dma("sp", st[:, :], self.mlp_w_up[layer, kd * P:(kd + 1) * P, hf * 2048:(hf + 1) * 2048])
                    if n % 2 == 0:
                        k.act(wu[:, kd, hf * 2048:(hf + 1) * 2048], st[:, :], AF.Copy, scale=g2[:, kd:kd + 1])
                    else:
                        k.ts("pool", wu[:, kd, hf * 2048:(hf + 1) * 2048], st[:, :], g2[:, kd:kd + 1], ALU.mult)
                    n += 1
            for c2 in range(16):
                st = stage.next()
                src = self.mlp_w_down.h[layer, c2 * 256:(c2 + 1) * 256, :].rearrange("(c p) n -> p c n", p=P)
                k.dma("sp", st.v(st.h[:, :].rearrange("p (c n) -> p c n", c=2)), self.mlp_w_down.v(src))
                dst = wd.v(wd.h[:, 2 * c2:2 * c2 + 2, :])
                sv = st.v(st.h[:, :].rearrange("p (c n) -> p c n", c=2))
                if n % 2 == 0:
                    k.copy("dve", dst, sv)
                else:
                    k.copy("pool", dst, sv)
                n += 1

            TS = 256
            nsub = TS // P
            hts = Ring(k, 2 * nsub, [P, D], F32, "ht", es)
            hns = Ring(k, 2, [P, D], BF16, "hn", es)
            junk = k.sb([P, D], BF16, "junk", es)
            sss = Ring(k, 8, [P, 4], F32, "ss", es)
            uTs = Ring(k, 2, [P, 8, TS], BF16, "uT", es)
            aT = Ring(k, 1, [P, 32, TS], BF16, "aT", es)
            tpsr = Ring(k, 2, [P, 8, P], BF16, "tps", es, psum=True)
            upsr = Ring(k, 2, [P, 512], F32, "ups", es, psum=True)
            dpsr = Ring(k, 4, [P, 512], F32, "dps", es, psum=True)
            ytmp = Ring(k, 2, [P, D], F32, "yt", es)
            nst = self.S // TS

            def norm_parts(st_i):
                uT = uTs.next()
                hl, hnl = [], []
                for j in range(nsub):
                    ht = hts.next()
                    hn = hns.next()
                    s4 = sss.next()
                    self.norm_part(hsrc, st_i * nsub + j, ht, hn, s4.sub(0), s4.sub(1), junk)
                    hl.append(ht)
                    hnl.append(hn)
                return uT, hl, hnl

            def tr_parts(np_):
                uT, hl, hnl = np_
                for j in range(nsub):
                    self.tr_part(hnl[j], tpsr.next(), uT.v(uT.h[:, :, j * P:(j + 1) * P]))

            cur = norm_parts(0)
            tr_parts(cur)
            for st_i in range(nst):
                uT, hl, _ = cur
                nxt = None
                a = aT.next()
                for fc in range(32):
                    ups = upsr.next()
                    for kd in range(8):
                        k.mm(ups[:, 0:TS], wu[:, kd, fc * P:(fc + 1) * P], uT[:, kd, :], start=(kd == 0), stop=(kd == 7))
                    if fc % 2 == 0:
                        k.act(a[:, fc, :], ups[:, 0:TS], AF.Relu)
                        k.tt("pool", a[:, fc, :], a[:, fc, :], a[:, fc, :], ALU.mult)
                    else:
                        k.ts("dve", a[:, fc, :], ups[:, 0:TS], 0.0, ALU.max)
                        k.tt("pool", a[:, fc, :], a[:, fc, :], a[:, fc, :], ALU.mult)
                    if fc == 15 and st_i + 1 < nst:
                        nxt = norm_parts(st_i + 1)
                dpl = []
                for j in range(nsub):
                    dps = [dpsr.next(), dpsr.next()]
                    for hf in range(2):
                        for kc in range(32):
                            k.mm(dps[hf][:, :], a[:, kc, j * P:(j + 1) * P], wd[:, kc, hf * 512:(hf + 1) * 512],
                                 start=(kc == 0), stop=(kc == 31))
                    dpl.append(dps)
                if nxt is not None:
                    tr_parts(nxt)
                for j in range(nsub):
                    self.post_norm_add(dpl[j], hl[j], g3b, hdst, st_i * nsub + j, sss.next(), junk, ytmp.next())
                cur = nxt

    def post_norm_add(self, dps, ht, gb, hdst, ti, s4, junk, yt):
        k = self.k
        ssa, ssb, rs = s4.sub(0), s4.sub(1), s4.sub(2)
        k.act(View(junk.h[:, 0:512], Buf()), dps[0][:, :], AF.Square, accum=ssa[:, 0:1])
        k.act(View(junk.h[:, 512:1024], Buf()), dps[1][:, :], AF.Square, accum=ssb[:, 1:2])
        k.tt("dve", rs[:, 2:3], ssa[:, 0:1], ssb[:, 1:2], ALU.add)
        self.rstd(rs[:, 2:3], rs[:, 2:3], D)
        for hf in range(2):
            sl = slice(hf * 512, (hf + 1) * 512)
            k.stt(yt[:, sl], dps[hf][:, :], rs[:, 2:3], gb[:, sl], ALU.mult, ALU.mult)
            k.tt("pool", yt[:, sl], yt[:, sl], ht[:, sl], ALU.add)
        k.dma("pool", hdst.sub(ti)[ti * P:(ti + 1) * P, :], yt[:, :])

    def finish(self, last):
        k = self.k
        vs = [last.sub(ti)[:, :] for ti in range(self.NT)]
        k.wait_all("sp", vs)
        k.wait_all("pool", vs)


class PSlots:
    def __init__(self, k, nbanks, es, name):
        self.slots = []
        for b in range(nbanks):
            t = k.ps([P, 4, P], F32, name=name, es=es)
            self.slots.append((t, 0))
        self.i = 0

    def next(self):
        s = self.slots[self.i % len(self.slots)]
        self.i += 1
        return Slot(*s)


class Slot:
    def __init__(self, tt, j):
        self.tt = tt
        self.j = j

    def __getitem__(self, key):
        ps, fs = key
        if self.j is None:
            if fs == slice(None):
                fs = slice(0, P)
            return View(self.tt.h[ps, fs], self.tt.buf)
        return View(self.tt.h[ps, self.j, fs], self.tt.buf)


def bc3(tt, ap2, n):
    shp = list(ap2.shape)
    return View(ap2.unsqueeze(2).to_broadcast([shp[0], shp[1], n]), tt.buf)


def _load_w(self, dst_view_fn, src_rows_fn, nchunks, cols, es_stage, gain=None):
    k = self.k
    for kd in range(nchunks):
        for c0 in range(0, cols, 2048):
            cw = min(2048, cols - c0)
            st = es_stage.next()
            k.dma("sp", st[:, 0:cw], src_rows_fn(kd, c0, cw))
            self._wn = getattr(self, "_wn", 0) + 1
            dst = dst_view_fn(kd, c0, cw)
            if gain is not None:
                if self._wn % 2 == 0:
                    k.act(dst, st[:, 0:cw], AF.Copy, scale=gain[:, kd:kd + 1])
                else:
                    k.ts("pool", dst, st[:, 0:cw], gain[:, kd:kd + 1], ALU.mult)
            else:
                k.copy("dve" if self._wn % 2 == 0 else "pool", dst, st[:, 0:cw])


Prog.load_w = _load_w


def _fm_param(self, dst, src_ap, es=None):
    with self.nc.allow_non_contiguous_dma(reason="tiny param"):
        self.k.dma("sp", dst, View(src_ap.rearrange("(c p) -> p c", p=P), Buf()))


Prog.fm_param = _fm_param


def _rwkv(self, layer, hsrc, hdst):
    k = self.k
    nc = self.nc
    o = layer // 2
    with ExitStack() as es:
        g0 = k.sb([P, 8], F32, "g0", es)
        self.fm_param(g0[:, :], self.norm_g.h[layer, 0, :])
        g1b = k.sb([P, D], F32, "g1b", es)
        k.dma("sp", g1b[:, :], View(self.norm_g.h[layer, 1, :].partition_broadcast(P), Buf()))
        lngb = k.sb([P, D], F32, "lngb", es)
        k.dma("sp", lngb[:, :], View(self.r_ln_g.h[o, :].partition_broadcast(P), Buf()))
        lnbb = k.sb([P, D], F32, "lnbb", es)
        k.dma("sp", lnbb[:, :], View(self.r_ln_b.h[o, :].partition_broadcast(P), Buf()))
        mu = k.sb([P, 6, 8], F32, "mu", es)
        for i in range(6):
            self.fm_param(mu[:, i, :], self.r_mu.h[o, i, :])
        fp = {}
        for nm, src in (("w0", self.r_w0), ("a0", self.r_a0), ("kk", self.r_k_k), ("ka", self.r_k_a), ("rk", self.r_r_k)):
            fp[nm] = k.sb([P, 8], F32, nm, es)
            self.fm_param(fp[nm][:, :], src.h[o, :])
        gneps = k.sb([P, 1], F32, "gneps", es)
        k.memset("dve", gneps[:, :], GN_EPS)
        wr = k.sb([P, 8, D], BF16, "wr", es)
        wk = k.sb([P, 8, D], BF16, "wk", es)
        wv = k.sb([P, 8, D], BF16, "wv", es)
        wo = k.sb([P, 8, D], BF16, "wo", es)
        w1 = k.sb([P, 8, 64], BF16, "w1", es)
        a1 = k.sb([P, 8, 64], BF16, "a1", es)
        g1 = k.sb([P, 8, P], BF16, "g1", es)
        w2 = k.sb([64, D], BF16, "w2", es)
        a2 = k.sb([64, D], BF16, "a2", es)
        g2 = k.sb([P, D], BF16, "g2", es)
        with ExitStack() as es2:
            stage = Ring(k, 2, [P, 2048], F32, "stg", es2)
            for wt, i in ((wr, 0), (wk, 1), (wv, 2)):
                self.load_w(lambda kd, c0, cw, wt=wt: wt[:, kd, c0:c0 + cw],
                            lambda kd, c0, cw, i=i: self.r_w_rkv[o, i, kd * P:(kd + 1) * P, c0:c0 + cw], 8, D, stage, g0)
            self.load_w(lambda kd, c0, cw: wo[:, kd, c0:c0 + cw],
                        lambda kd, c0, cw: self.r_w_out[o, kd * P:(kd + 1) * P, c0:c0 + cw], 8, D, stage, None)
            for wt, src, cols in ((w1, self.r_w1, 64), (a1, self.r_a1, 64), (g1, self.r_g1, P)):
                self.load_w(lambda kd, c0, cw, wt=wt: wt[:, kd, c0:c0 + cw],
                            lambda kd, c0, cw, src=src: src[o, kd * P:(kd + 1) * P, c0:c0 + cw], 8, cols, stage, g0)
            for wt, src in ((w2, self.r_w2), (a2, self.r_a2)):
                st = stage.next()
                k.dma("sp", st[0:64, 0:D], src[o, :, :])
                k.copy("dve", wt[:, :], st[0:64, 0:D])
            st = stage.next()
            k.dma("sp", st[:, 0:D], self.r_g2[o, :, :])
            k.copy("dve", g2[:, :], st[:, 0:D])
            self.barrier()
        m_ts = k.sb([P, P], F32, "m_ts", es)
        mT_s = k.sb([P, P], F32, "mT_s", es)
        mT_i = k.sb([P, P], F32, "mT_i", es)
        bdones = k.sb([P, P], F32, "bdones", es)
        for m_, pat, cm, cmp in ((m_ts, -1, 1, ALU.is_gt), (mT_s, 1, -1, ALU.is_gt), (mT_i, 1, -1, ALU.is_ge)):
            k.memset("pool", m_[:, :], 0.0)
            for b in range(2):
                sl = slice(b * 64, b * 64 + 64)
                k.memset("pool", m_[sl, sl], 1.0)
                k.affsel(m_[sl, sl], m_[sl, sl], [[pat, 64]], cmp, 0.0, 0, cm)
        k.memset("pool", bdones[:, :], 0.0)
        for b in range(2):
            sl = slice(b * 64, b * 64 + 64)
            k.memset("pool", bdones[sl, sl], 1.0)
        headsel = k.sb([P, 8, 16], F32, "headsel", es)
        k.memset("pool", headsel[:, :, :], 0.0)
        for c in range(8):
            for b in range(2):
                k.memset("pool", headsel[b * 64:b * 64 + 64, c, 2 * c + b:2 * c + b + 1], 1.0)
        rmask = k.sb([P, P], F32, "rmask", es)
        k.memset("pool", rmask[:, :], 1.0)
        k.memset("pool", rmask[:, 0:1], 0.0)
        k.memset("pool", rmask[:, 64:65], 0.0)
        Fm = lambda nm: k.sb([P, 8, P], F32, nm, es)
        rT, kT, aT, sg, kk, scr, cum = Fm("rT"), Fm("kT"), Fm("aT"), Fm("sg"), Fm("kk"), Fm("scr"), Fm("cum")
        epos, eneg, eexc, erem = Fm("epos"), Fm("eneg"), Fm("eexc"), Fm("erem")
        ktT, alT, beT = Fm("ktT"), Fm("alT"), Fm("beT")
        kka, rbT = aT, epos
        for t_ in (rT, kT, aT, kk, scr, eneg, eexc, erem, epos, ktT, alT, beT):
            t_.buf.f32r = True
        Tm = lambda nm: k.sb([P, D], F32, nm, es)
        vtok, gate, Atok, Bhat, Khat, ytok, ht = Tm("vtok"), Tm("gate"), Tm("Atok"), Tm("Bhat"), Tm("Khat"), Tm("ytok"), Tm("ht")
        gl = k.sb([P, 16], F32, "gl", es)
        bonus = k.sb([P, 16], F32, "bonus", es)
        uTs = Ring(k, 2, [P, 8, P + 1], BF16, "uTh", es)
        xis = Ring(k, 2, [P, 8, P], BF16, "xi", es)
        hn = k.sb([P, D], BF16, "hn", es)
        junk = k.sb([P, D], BF16, "junk", es)
        sss = Ring(k, 4, [P, 4], F32, "ss", es)
        hw = k.sb([64, P], BF16, "hw", es)
        ha = k.sb([64, P], BF16, "ha", es)
        hg = k.sb([P, P], BF16, "hg", es)
        St = k.sb([P, 8, 64], F32, "St", es)
        for g in range(4):
            k.memset("pool", St.sub(g)[:, 2 * g:2 * g + 2, :], 0.0)
        gnt = [k.sb([P, 16], F32, "gn%d" % i, es) for i in range(4)] + [k.sb([P, D], F32, "ysq", es)]
        WB = []
        for t_ in (rT, kT, aT, kk, scr, eneg, eexc, erem):
            for half in range(2):
                WB.append(TT(t_.h[:, half * 4:(half + 1) * 4, :]))
        xcnt, acnt = [0], [0]
        for t_ in WB:
            t_.buf.f32r = True
        vtok.buf.f32r = True

        def handoff(srcs, dsts):
            toks = {}
            for s_ in srcs:
                for t in ([s_.buf.w] if s_.buf.w else []) + list(s_.buf.r.values()):
                    if id(t[0]) not in toks or toks[id(t[0])][1] < t[1]:
                        toks[id(t[0])] = t
            for d_ in dsts:
                d_.buf.w = None
                d_.buf.r = dict(toks)

        Yl = k.sb([P, 4, 64], F32, "Yl", es)
        Qe = k.sb([P, 2, P], F32, "Qe", es)
        MT = k.sb([P, 4, 64], F32, "MT", es)
        Nn = k.sb([P, 4, 64], F32, "Nn", es)
        glD = k.sb([P, 16, 64], F32, "glD", es)
        sid = k.sb([P, 64], F32, "sid", es)
        k.copy("pool", sid[0:64, :], self.ident_f[0:64, 0:64])
        k.copy("pool", sid[64:128, :], self.ident_f[64:128, 64:128])
        zb = k.sb([P, D], BF16, "zb", es)
        zT = k.sb([P, 8, P], BF16, "zT", es)
        tps = k.ps([P, 8, P], BF16, "tps", es)
        pG = Ring(k, 7, [P, 512], F32, "pG", es, psum=True)

        class _PA:
            def next(self_):
                t_ = pG.next()
                return Slot(t_, None)
        pA = _PA()
        pT = pG

        prev_uT = None
        for ti in range(self.NT):
            uT = uTs.next()
            s4 = sss.next()
            self.norm_T(hsrc, ti, ht, hn, s4.sub(0), s4.sub(1), junk, tps, uT.v(uT.h[:, :, 1:P + 1]))
            if ti == 0:
                k.memset("pool", uT[:, :, 0:1], 0.0)
            else:
                k.copy("pool", uT[:, :, 0:1], prev_uT[:, :, P:P + 1])
            prev_uT = uT
            xx = ktT
            k.tt("dve", xx[:, :, :], uT[:, :, 0:P], uT[:, :, 1:P + 1], ALU.subtract)
            LD = -0.6065306597126334

            def mix(i):
                xi = xis.next()
                k.tt("pool", xi[:, :, :], xx[:, :, :], bc3(mu, mu.h[:, i, :], P), ALU.mult)
                k.tt("dve", xi[:, :, :], xi[:, :, :], uT[:, :, 1:P + 1], ALU.add)
                return xi

            def proj_fm(wt, xi, dst):
                for n_ in range(8):
                    ps = pA.next()
                    for kd in range(8):
                        k.mm(ps[:, :], wt[:, kd, n_ * P:(n_ + 1) * P], xi[:, kd, :], start=(kd == 0), stop=(kd == 7))
                    k.copy("act", dst[:, n_, :], ps[:, :])

            xi_k = mix(2)
            xi_w = mix(1)
            proj_fm(wk, xi_k, kT)
            k.tt("pool", kk[:, :, :], kT[:, :, :], bc3(fp["kk"], fp["kk"].h[:, :], P), ALU.mult)
            k.tt("dve", scr[:, :, :], kk[:, :, :], kk[:, :, :], ALU.mult)
            ps = pA.next()
            for kd in range(8):
                k.mm(ps[0:64, :], w1[:, kd, :], xi_w[:, kd, :], start=(kd == 0), stop=(kd == 7))
            k.act(hw[:, :], ps[0:64, :], AF.Tanh)
            for n_ in range(8):
                ps = pA.next()
                k.mm(ps[:, :], w2[:, n_ * P:(n_ + 1) * P], hw[:, :])
                k.act(sg[:, n_, :], ps[:, :], AF.Sigmoid, bias=fp["w0"][:, n_:n_ + 1])
            xi_a = mix(4)
            for c in range(8):
                k.scan(cum[:, c, :], rmask[:, :], sg[:, c, :], 0.0, ALU.mult, ALU.add)
            k.act(epos[:, :, :], cum[:, :, :], AF.Exp, scale=LD)
            k.act(eneg[:, :, :], cum[:, :, :], AF.Exp, scale=-LD)
            k.tt("pool", sg[:, :, :], cum[:, :, :], sg[:, :, :], ALU.subtract)
            k.act(eexc[:, :, :], sg[:, :, :], AF.Exp, scale=LD)
            c16 = cum.h[:, :, :].rearrange("p c (j t) -> p (c j) t", t=64)
            cl = View(c16[:, :, 63:64].to_broadcast([P, 16, 64]), cum.buf)
            s16 = sg.v(sg.h[:, :, :].rearrange("p c (j t) -> p (c j) t", t=64))
            k.tt("dve", s16, cl, View(c16, cum.buf), ALU.subtract)
            k.act(erem[:, :, :], sg[:, :, :], AF.Exp, scale=LD)
            e16 = epos.h[:, :, :].rearrange("p c (j t) -> p (c j) t", t=64)
            k.copy("pool", gl.v(gl.h[:, :].unsqueeze(2)), View(e16[:, :, 63:64], epos.buf))
            ps = pA.next()
            for kd in range(8):
                k.mm(ps[0:64, :], a1[:, kd, :], xi_a[:, kd, :], start=(kd == 0), stop=(kd == 7))
            k.copy("act", ha[:, :], ps[0:64, :])
            for n_ in range(8):
                ps = pA.next()
                k.mm(ps[:, :], a2[:, n_ * P:(n_ + 1) * P], ha[:, :])
                k.act(aT[:, n_, :], ps[:, :], AF.Sigmoid, bias=fp["a0"][:, n_:n_ + 1])
            xi_r = mix(0)
            for hf in range(2):
                ps = pT.next()
                for c4 in range(4):
                    c = hf * 4 + c4
                    k.mm(ps[:, c4 * P:(c4 + 1) * P], bdones[:, :], scr[:, c, :])
                dstv = scr.v(scr.h[:, hf * 4:(hf + 1) * 4, :].rearrange("p c t -> p (c t)"))
                k.ts("dve", dstv, ps[:, :], 1e-24, ALU.max)
            k.act(scr[:, :, :], scr[:, :, :], AF.Ln)
            k.act(scr[:, :, :], scr[:, :, :], AF.Exp, scale=-0.5)
            k.tt("dve", kk[:, :, :], kk[:, :, :], scr[:, :, :], ALU.mult)
            k.stt(scr[:, :, :], aT[:, :, :], -1.0, bc3(fp["ka"], fp["ka"].h[:, :], P), ALU.add, ALU.mult)
            k.stt(kT[:, :, :], scr[:, :, :], 1.0, kT[:, :, :], ALU.add, ALU.mult)
            k.tt("pool", kka[:, :, :], kk[:, :, :], aT[:, :, :], ALU.mult)
            proj_fm(wr, xi_r, rT)
            xi_v = mix(3)
            k.tt("pool", scr[:, :, :], rT[:, :, :], kT[:, :, :], ALU.mult)
            k.tt("dve", scr[:, :, :], scr[:, :, :], bc3(fp["rk"], fp["rk"].h[:, :], P), ALU.mult)
            for hf in range(2):
                ps = pT.next()
                for kd in range(8):
                    k.mm(ps[:, :], xi_v[:, kd, :], wv[:, kd, hf * 512:(hf + 1) * 512], start=(kd == 0), stop=(kd == 7))
                k.copy("act", vtok[:, hf * 512:(hf + 1) * 512], ps[:, :])
            xi_g = mix(5)
            ps = pA.next()
            for c in range(8):
                k.mm(ps[:, 0:16], scr[:, c, :], headsel[:, c, :], start=(c == 0), stop=(c == 7))
            k.copy("act", bonus[:, :], ps[:, 0:16])
            ps = pA.next()
            for kd in range(8):
                k.mm(ps[:, :], g1[:, kd, :], xi_g[:, kd, :], start=(kd == 0), stop=(kd == 7))
            k.act(hg[:, :], ps[:, :], AF.Sigmoid)
            for hf in range(2):
                ps = pT.next()
                k.mm(ps[:, :], hg[:, :], g2[:, hf * 512:(hf + 1) * 512])
                k.copy("act", gate[:, hf * 512:(hf + 1) * 512], ps[:, :])
            k.stt(alT[:, :, :], kk[:, :, :], -1.0, eexc[:, :, :], ALU.mult, ALU.mult)
            k.tt("dve", beT[:, :, :], kka[:, :, :], eneg[:, :, :], ALU.mult)
            k.tt("pool", eexc[:, :, :], kka[:, :, :], erem[:, :, :], ALU.mult)
            k.tt("dve", ktT[:, :, :], kT[:, :, :], eneg[:, :, :], ALU.mult)
            k.tt("dve", erem[:, :, :], kT[:, :, :], erem[:, :, :], ALU.mult)
            k.tt("dve", rbT[:, :, :], rT[:, :, :], epos[:, :, :], ALU.mult)
            n_ = 0
            for src, dst in ((alT, Atok), (eexc, Bhat), (erem, Khat)):
                for hf in range(2):
                    ps = pT.next()
                    for c4 in range(4):
                        k.tr(ps[:, c4 * P:(c4 + 1) * P], src[:, hf * 4 + c4, :], self.ident_f[:, :])
                    k.copy("act" if n_ % 2 == 0 else "dve", dst[:, hf * 512:(hf + 1) * 512], ps[:, :])
                    n_ += 1
            k.stop(1)
            handoff([rT, kT, aT, kk, scr, eneg, eexc, erem], WB)
            k.tt("pool", glD[:, :, :], View(sid.h[:, :].unsqueeze(1).to_broadcast([P, 16, 64]), sid.buf),
                 bc3(gl, gl.h[:, :], 64), ALU.mult)
            k.stop(2)
            q4 = lambda v_: v_.h[:, :].rearrange("p (q t) -> p q t", q=4)
            names5 = ("A0", "AT0", "AakT", "ArbT", "ArkT")
            gsets = [{n_: WB[i_] for i_, n_ in zip((0, 1, 2, 3, 4), names5)},
                     {n_: WB[i_] for i_, n_ in zip((7, 12, 13, 14, 15), names5)}]
            src = {"al": alT, "be": beT, "kt": ktT, "rb": rbT}

            def p1_units(g_):
                Gs = gsets[g_ % 2]
                units = []
                for (l_, r_, msk, dst) in (("al", "be", m_ts, "A0"), ("be", "al", mT_s, "AT0"), ("kt", "al", mT_s, "AakT"),
                                           ("be", "rb", mT_i, "ArbT"), ("kt", "rb", mT_i, "ArkT")):
                    for pi in range(2):
                        def unit(l_=l_, r_=r_, msk=msk, dst=dst, pi=pi):
                            ps = pG.next()
                            hs = slice(pi * 64, pi * 64 + 64)
                            for cl in range(2):
                                c = 2 * g_ + cl
                                k.mm(ps[:, cl * P:(cl + 1) * P], src[l_][hs, c, :], src[r_][hs, c, :])
                            k.tt("dve", Gs[dst][:, 2 * pi:2 * pi + 2, :], ps.v(ps.h[:, 0:256].rearrange("p (q t) -> p q t", q=2)),
                                 View(msk.h[:, :].unsqueeze(1).to_broadcast([P, 2, P]), msk.buf), ALU.mult)
                        units.append(unit)
                return units

            for g in range(4):
                heads = [(2 * (2 * g + cl) + pi, 2 * g + cl, pi * 64) for pi in range(2) for cl in range(2)]
                G_ = gsets[g % 2]
                if g == 0:
                    for u_ in p1_units(0):
                        u_()
                k.stop(3)
                X = WB[5 + (xcnt[0] % 2)]
                xcnt[0] += 1
                for pi in range(2):
                    k.copy("pool", X.v(X.h[:, 2 * pi:2 * pi + 2, 0:64]),
                           Atok.v(Atok.h[:, g * 256:(g + 1) * 256].rearrange("p (cl pi d) -> p pi cl d", pi=2, cl=2)[:, pi]))
                ps = pG.next()
                for q, (h, c, po) in enumerate(heads):
                    k.mm(ps[:, q * 64:(q + 1) * 64], G_["AakT"][:, q, :], vtok[:, h * 64:(h + 1) * 64])
                k.copy("act", X.v(X.h[:, :, 64:128]), ps.v(ps.h[:, 0:256].rearrange("p (q d) -> p q d", q=4)))
                A, AT = G_["A0"], G_["AT0"]
                nxt_units = p1_units(g + 1) if g + 1 < 4 else []
                for lvl in range(6):
                    ps = pG.next()
                    for q in range(4):
                        k.mm(ps[:, q * P:(q + 1) * P], AT[:, q, :], X[:, q, :])
                    Xn = WB[5 + (xcnt[0] % 2)]
                    xcnt[0] += 1
                    k.tt("dve", Xn[:, :, :], ps.v(q4(ps)), X[:, :, :], ALU.add)
                    X = Xn
                    if lvl < 5:
                        ps2 = pG.next()
                        for q in range(4):
                            k.mm(ps2[:, q * P:(q + 1) * P], A[:, q, :], AT[:, q, :])
                        A2T = WB[8 + (acnt[0] % 4)]
                        acnt[0] += 1
                        k.copy("act", A2T[:, :, :], ps2.v(q4(ps2)))
                        A2 = None
                        if lvl < 4:
                            ps3 = pG.next()
                            for q in range(4):
                                k.mm(ps3[:, q * P:(q + 1) * P], AT[:, q, :], A[:, q, :])
                            A2 = WB[8 + (acnt[0] % 4)]
                            acnt[0] += 1
                            k.copy("act", A2[:, :, :], ps3.v(q4(ps3)))
                        A, AT = A2, A2T
                    for u_ in nxt_units[2 * lvl:2 * lvl + 2]:
                        u_()
                k.stop(4)
                ps = pG.next()
                for q, (h, c, po) in enumerate(heads):
                    k.mm(ps[:, q * 64:(q + 1) * 64], G_["ArkT"][:, q, :], vtok[:, h * 64:(h + 1) * 64], start=True, stop=False)
                    k.mm(ps[:, q * 64:(q + 1) * 64], G_["ArbT"][:, q, :], X[:, q, 64:128], start=False, stop=True)
                k.copy("act", Yl[:, :, :], ps.v(ps.h[:, 0:256].rearrange("p (q d) -> p q d", q=4)))
                ps = pG.next()
                for q, (h, c, po) in enumerate(heads):
                    hs = slice(po, po + 64)
                    cl = q % 2
                    k.mm(ps[hs, cl * P:(cl + 1) * P], X[:, q, 0:64], G_["ArbT"][:, q, :], r=False)
                k.tt("dve", Qe[:, :, :], ps.v(ps.h[:, 0:256].rearrange("p (c t) -> p c t", c=2)), rbT[:, 2 * g:2 * g + 2, :], ALU.add)
                for cc in range(2):
                    cs = slice(cc * 64, cc * 64 + 64)
                    psM = pG.next()
                    psN = pG.next()
                    for q, (h, c, po) in enumerate(heads):
                        hs = slice(po, po + 64)
                        hc = slice(h * 64, h * 64 + 64)
                        cl = q % 2
                        k.mm(psM[hs, cl * 64:(cl + 1) * 64], X[cs, q, 0:64], Bhat[cs, hc], r=False)
                        k.mm(psN[hs, cl * 64:(cl + 1) * 64], Bhat[cs, hc], X[cs, q, 64:128], start=True, stop=False, r=False)
                        k.mm(psN[hs, cl * 64:(cl + 1) * 64], Khat[cs, hc], vtok[cs, hc], start=False, stop=True, r=False)
                    gd = glD.v(glD.h[:, 4 * g:4 * g + 4, :].rearrange("p (cl cc) d -> p cl cc d", cc=2)[:, :, cc, :])
                    k.tt("dve", MT.v(MT.h[:, :, :].rearrange("p (cl cc) d -> p cl cc d", cc=2)[:, :, cc, :]),
                         psM.v(psM.h[:, 0:128].rearrange("p (c d) -> p c d", c=2)), gd, ALU.add)
                    k.copy("act", Nn.v(Nn.h[:, :, :].rearrange("p (cl cc) d -> p cl cc d", cc=2)[:, :, cc, :]),
                           psN.v(psN.h[:, 0:128].rearrange("p (c d) -> p c d", c=2)))
                k.stop(5)
                Sg = St.sub(g)
                psy = [pG.next(), pG.next()]
                for cc in range(2):
                    cs = slice(cc * 64, cc * 64 + 64)
                    psS = [pG.next(), pG.next()]
                    for q, (h, c, po) in enumerate(heads):
                        hs = slice(po, po + 64)
                        pi, cl = q // 2, q % 2
                        k.mm(psy[pi][cs, cl * 64:(cl + 1) * 64], Qe[hs, cl, cs], Sg[hs, c, :])
                        k.mm(psS[pi][hs, cl * 64:(cl + 1) * 64], MT[hs, cl * 2 + cc, :], Sg[hs, c, :])
                    for pi in range(2):
                        hs = slice(pi * 64, pi * 64 + 64)
                        k.tt("dve", Sg[hs, 2 * g:2 * g + 2, :], psS[pi].v(psS[pi].h[hs, 0:128].rearrange("p (c d) -> p c d", c=2)),
                             Nn.v(Nn.h[hs, :, :].rearrange("p (cl cc) d -> p cl cc d", cc=2)[:, :, cc, :]), ALU.add)
                for pi in range(2):
                    k.tt("dve", ytok.v(ytok.h[:, g * 256:(g + 1) * 256].rearrange("p (cl pi d) -> p pi cl d", pi=2, cl=2)[:, pi]),
                         psy[pi].v(psy[pi].h[:, 0:128].rearrange("p (c d) -> p c d", c=2)), Yl[:, 2 * pi:2 * pi + 2, :], ALU.add)
            k.stop(6)
            handoff(WB, [rT, kT, aT, kk, scr, eneg, eexc, erem])
            y3 = lambda t_: t_.v(t_.h[:, :].rearrange("p (h d) -> p h d", d=64))
            sA, sB, sM, sR, ysq = gnt
            k.reduce(sA[:, :], y3(ytok), ALU.add)
            k.tt("pool", ysq[:, :], ytok[:, :], ytok[:, :], ALU.mult)
            k.reduce(sB[:, :], y3(ysq), ALU.add)
            k.ts("dve", sM[:, :], sA[:, :], 1.0 / 64, ALU.mult)
            k.tt("dve", sA[:, :], sM[:, :], sM[:, :], ALU.mult)
            k.stt(sB[:, :], sB[:, :], 1.0 / 64, sA[:, :], ALU.mult, ALU.subtract)
            k.act(sR[:, :], sB[:, :], AF.Ln, bias=gneps[:, 0:1])
            k.act(sR[:, :], sR[:, :], AF.Exp, scale=-0.5)
            k.tt("dve", y3(ytok), y3(ytok), bc3(sM, sM.h[:, :], 64), ALU.subtract)
            k.tt("dve", y3(ytok), y3(ytok), bc3(sR, sR.h[:, :], 64), ALU.mult)
            k.tt("pool", ytok[:, :], ytok[:, :], lngb[:, :], ALU.mult)
            k.tt("pool", ytok[:, :], ytok[:, :], lnbb[:, :], ALU.add)
            k.tt("dve", y3(ysq), y3(vtok), bc3(bonus, bonus.h[:, :], 64), ALU.mult)
            k.tt("pool", ytok[:, :], ytok[:, :], ysq[:, :], ALU.add)
            k.tt("dve", zb[:, :], ytok[:, :], gate[:, :], ALU.mult)
            for c in range(8):
                k.tr(tps[:, c, :], zb[:, c * P:(c + 1) * P], self.ident_b[:, :])
            k.copy("dve", zT[:, :, :], tps[:, :, :])
            dps = [pT.next(), pT.next()]
            for hf in range(2):
                for c in range(8):
                    k.mm(dps[hf][:, :], zT[:, c, :], wo[:, c, hf * 512:(hf + 1) * 512], start=(c == 0), stop=(c == 7))
            self.post_norm_add(dps, ht, g1b, hdst, ti, sss.next(), junk, ysq)
        k.dead = False
        self.barrier()


Prog.rwkv = _rwkv


def U(ap):
    return View(ap, Buf())


def _even(self, layer, hsrc, hdst):
    k = self.k
    nc = self.nc
    e = layer // 2
    S, NT = self.S, self.NT
    RS8 = 1.0 / 8.0
    RSD = 1.0 / (128.0 ** 0.5)
    if not hasattr(self, "dQT"):
        self.dQT = nc.dram_tensor("dQT", [P, 4, S], BF16).ap()
        self.dKT = nc.dram_tensor("dKT", [P, 4, S], BF16).ap()
        self.dMQK = nc.dram_tensor("dMQK", [P, 8, S], BF16).ap()
        self.dV = nc.dram_tensor("dV", [S, 512], BF16).ap()
        self.dMV = nc.dram_tensor("dMV", [S, 512], BF16).ap()
        self.dOG = nc.dram_tensor("dOG", [S, 512], BF16).ap()
        self.dAH = nc.dram_tensor("dAH", [P, 8, S], BF16).ap()
    dQT, dKT, dMQK, dV, dMV, dOG, dAH = self.dQT, self.dKT, self.dMQK, self.dV, self.dMV, self.dOG, self.dAH
    with ExitStack() as esg:
        ECt = k.sb([P, NT, 4], F32, "ECt", esg)
        EC2t = k.sb([P, NT, 4], F32, "EC2t", esg)
        THRt = k.sb([P, NT, 4], F32, "THRt", esg)
        wstB = k.sb([P, 4, NT], F32, "wstB", esg)
        esr = ExitStack()
        R1 = k.sb([4, S], F32, "R1", esr)
        R2 = k.sb([4, S], F32, "R2", esr)
        R3 = k.sb([4, S], F32, "R3", esr)
        with ExitStack() as es:
            g0 = k.sb([P, 8], F32, "g0", es)
            self.fm_param(g0[:, :], self.norm_g.h[layer, 0, :])
            win = k.sb([P, 8, IN_COLS], BF16, "win", es)
            with ExitStack() as es2:
                stage = Ring(k, 2, [P, 2048], F32, "stg", es2)
                self.load_w(lambda kd, c0, cw: win[:, kd, c0:c0 + cw],
                            lambda kd, c0, cw: self.e_w_in[e, kd * P:(kd + 1) * P, c0:c0 + cw], 8, IN_COLS, stage, g0)
                self.barrier()
            cw_ = k.sb([P, 8, 4], F32, "cw", es)
            for j in range(4):
                self.fm_param(cw_[:, :, j], self.e_conv_w.h[e, j, :])
            bi = k.sb([4, 1], F32, "bi", es)
            nbf = k.sb([4, 1], F32, "nbf", es)
            with nc.allow_non_contiguous_dma(reason="tiny"):
                k.dma("sp", bi[:, :], U(self.e_b_if.h[e, 0:4].rearrange("(p o) -> p o", o=1)))
                k.dma("sp", nbf[:, :], U(self.e_b_if.h[e, 4:8].rearrange("(p o) -> p o", o=1)))
            k.ts("dve", nbf[:, :], nbf[:, :], -1.0, ALU.mult)
            hts1 = Ring(k, 2, [P, D], F32, "ht", es)
            hns1 = Ring(k, 2, [P, D], BF16, "hn", es)
            junk = k.sb([P, D], BF16, "junk", es)
            sss = Ring(k, 4, [P, 4], F32, "ss", es)
            uTs = Ring(k, 2, [P, 8, P], BF16, "uT", es)
            cq = k.sb([P, 8, P + 3], F32, "cq", es)
            k.memset("pool", cq[:, :, 0:3], 0.0)
            acc = k.sb([P, 8, P], F32, "acc", es)
            qk_o = Ring(k, 2, [P, 8, P], BF16, "qko", es)
            sq_o = Ring(k, 2, [P, 4, P], BF16, "sqo", es)
            sk_o = Ring(k, 2, [P, 4, P], BF16, "sko", es)
            tok_o = Ring(k, 3, [P, 512], BF16, "toko", es)
            gtmp = k.sb([4, P], F32, "gtmp", es)
            tps = k.ps([P, 8, P], BF16, "tps", es)
            pA = PSlots(k, 3, es, "pA")
            pT = Ring(k, 2, [P, 512], F32, "pT", es, psum=True)
            s4 = sss.next()
            hn_n = hns1.next()
            self.norm_part(hsrc, 0, hts1.next(), hn_n, s4.sub(0), s4.sub(1), junk)
            uT_n = uTs.next()
            self.tr_part(hn_n, tps, uT_n[:, :, :])
            for ti in range(NT):
                ts_ = slice(ti * P, (ti + 1) * P)
                uT = uT_n
                sq, sk, qk = sq_o.next(), sk_o.next(), qk_o.next()
                for n_ in range(4):
                    ps = pA.next()
                    for kd in range(8):
                        k.mm(ps[:, :], win[:, kd, n_ * P:(n_ + 1) * P], uT[:, kd, :], start=(kd == 0), stop=(kd == 7))
                    k.copy("act", sq[:, n_, :], ps[:, :])
                for n_ in range(4):
                    ps = pA.next()
                    for kd in range(8):
                        k.mm(ps[:, :], win[:, kd, 512 + n_ * P:512 + (n_ + 1) * P], uT[:, kd, :], start=(kd == 0), stop=(kd == 7))
                    k.act(sk[:, n_, :], ps[:, :], AF.Copy, scale=RS8)
                k.dma("pool", U(dQT[:, :, ts_]), sq[:, :, :])
                k.dma("pool", U(dKT[:, :, ts_]), sk[:, :, :])
                if ti + 1 < NT:
                    s4 = sss.next()
                    hn_n = hns1.next()
                    self.norm_part(hsrc, ti + 1, hts1.next(), hn_n, s4.sub(0), s4.sub(1), junk)
                for n_ in range(8):
                    ps = pA.next()
                    for kd in range(8):
                        k.mm(ps[:, :], win[:, kd, 1536 + n_ * P:1536 + (n_ + 1) * P], uT[:, kd, :], start=(kd == 0), stop=(kd == 7))
                    k.copy("act", cq[:, n_, 3:P + 3], ps[:, :])
                for n_ in range(8):
                    k.ts("dve", acc[:, n_, :], cq[:, n_, 0:P], cw_[:, n_, 0:1], ALU.mult)
                    for j in range(1, 4):
                        k.stt(acc[:, n_, :], cq[:, n_, j:j + P], cw_[:, n_, j:j + 1], acc[:, n_, :], ALU.mult, ALU.add)
                k.act(qk[:, :, :], acc[:, :, :], AF.Silu)
                k.copy("pool", acc[:, :, 0:3], cq[:, :, P:P + 3])
                k.copy("pool", cq[:, :, 0:3], acc[:, :, 0:3])
                k.dma("pool", U(dMQK[:, :, ts_]), qk[:, :, :])
                for col0, dd, fn in ((1024, dV, None), (2560, dMV, None), (3072, dOG, AF.Sigmoid)):
                    ps = pT.next()
                    for kd in range(8):
                        k.mm(ps[:, :], uT[:, kd, :], win[:, kd, col0:col0 + 512], start=(kd == 0), stop=(kd == 7))
                    to = tok_o.next()
                    if fn is None:
                        k.copy("dve", to[:, :], ps[:, :])
                    else:
                        k.act(to[:, :], ps[:, :], fn)
                    k.dma("pool", U(dd[ts_, :]), to[:, :])
                ps = pA.next()
                for kd in range(8):
                    k.mm(ps[0:4, :], win[:, kd, 3584:3588], uT[:, kd, :], start=(kd == 0), stop=(kd == 7))
                k.act(R1[:, ts_], ps[0:4, :], AF.Identity, bias=bi[:, 0:1])
                ps = pA.next()
                for kd in range(8):
                    k.mm(ps[0:4, :], win[:, kd, 3588:3592], uT[:, kd, :], start=(kd == 0), stop=(kd == 7))
                k.act(gtmp[:, :], ps[0:4, :], AF.Exp, bias=nbf[:, 0:1], scale=-1.0)
                k.act(R2[:, ts_], gtmp[:, :], AF.Ln, bias=1.0)
                if ti + 1 < NT:
                    uT_n = uTs.next()
                    self.tr_part(hn_n, tps, uT_n[:, :, :])
            self.barrier()
        if EVSTOP == 1:
            k.dead = True
        with ExitStack() as es:
            k.scan(R3[:, :], R2[:, :], R2[:, :], 0.0, ALU.add, ALU.max)
            k.tt("dve", R1[:, :], R1[:, :], R3[:, :], ALU.add)
            k.scan(R2[:, :], R1[:, :], R1[:, :], 0.0, ALU.max, ALU.max)
            ge = k.sb([4, NT], F32, "ge", es)
            rc = k.sb([4, NT], F32, "rc", es)
            wst = k.sb([4, NT], F32, "wst", es)
            g3 = R2.h[:, :].rearrange("p (c t) -> p c t", t=P)
            k.copy("dve", ge.v(ge.h[:, :].unsqueeze(2)), View(g3[:, :, P - 1:P], R2.buf))
            k.memset("dve", rc[:, :], 0.0)
            if NT > 1:
                k.copy("dve", rc[:, 1:NT], ge[:, 0:NT - 1])
            k.tt("dve", wst[:, :], rc[:, :], ge[:, :], ALU.subtract)
            k.act(wst[:, :], wst[:, :], AF.Exp)
            rcb = lambda: View(rc.h[:, :].unsqueeze(2).to_broadcast([4, NT, P]), rc.buf)
            r13 = R1.v(R1.h[:, :].rearrange("p (c t) -> p c t", t=P))
            r33 = R3.v(R3.h[:, :].rearrange("p (c t) -> p c t", t=P))
            k.tt("dve", r13, r13, rcb(), ALU.subtract)
            k.act(R1[:, :], R1[:, :], AF.Exp)
            k.tt("dve", r33, r33, rcb(), ALU.subtract)
            k.act(R3[:, :], R3[:, :], AF.Exp)
            pt1 = k.ps([P, 512], F32, "pt1", es)
            pt2 = k.ps([P, 512], F32, "pt2", es)
            pt3 = k.ps([P, 512], F32, "pt3", es)
            for c in range(NT):
                k.tr(pt1[:, c * 4:(c + 1) * 4], R1[0:4, c * P:(c + 1) * P], self.ident_f[0:4, 0:4])
            k.copy("dve", ECt.v(ECt.h[:, :, :].rearrange("p c h -> p (c h)")), pt1[:, 0:NT * 4])
            for c in range(NT):
                k.tr(pt2[:, c * 4:(c + 1) * 4], R3[0:4, c * P:(c + 1) * P], self.ident_f[0:4, 0:4])
            k.copy("dve", THRt.v(THRt.h[:, :, :].rearrange("p c h -> p (c h)")), pt2[:, 0:NT * 4])
            sel = k.sb([4, 4, P], F32, "sel", es)
            k.memset("pool", sel[:, :, :], 1.0)
            k.affsel(sel[:, :, :], sel[:, :, :], [[-1, 4], [0, P]], ALU.is_equal, 0.0, 0, 1)
            for h in range(4):
                k.mm(pt3[:, h * NT:(h + 1) * NT], sel[:, h, :], wst[:, :])
            k.copy("dve", wstB.v(wstB.h[:, :, :].rearrange("p h c -> p (h c)")), pt3[:, 0:4 * NT])
            k.tt("dve", EC2t[:, :, :], ECt[:, :, :], wstB.v(wstB.h[:, :, :].rearrange("p h c -> p c h")), ALU.mult)
            self.barrier()
        esr.close()
        with ExitStack() as es:
            QW = min(512, S)
            QT = k.sb([P, 4, S], BF16, "QT", es)
            KT = k.sb([P, 4, S], BF16, "KT", es)
            Vt = k.sb([P, NT, 512], BF16, "Vt", es)
            AO = k.sb([P, 4, S], BF16, "AO", es)
            k.dma("sp", QT[:, :, :], U(dQT[:, :, :]))
            k.dma("sp", KT[:, :, :], U(dKT[:, :, :]))
            k.dma("sp", Vt[:, :, :], U(dV.rearrange("(n p) d -> p n d", p=P)))
            ntri = k.sb([P, P], F32, "ntri", es)
            nones = k.sb([P, P], F32, "nones", es)
            ntri.buf.f32r = True
            nones.buf.f32r = True
            ctmp = k.sb([P, P], F32, "ctmp", es)
            k.memset("pool", ctmp[:, :], -1.0)
            k.copy("dve", nones[:, :], ctmp[:, :])
            k.affsel(ctmp[:, :], ctmp[:, :], [[-1, P]], ALU.is_ge, 0.0, 0, 1)
            k.copy("dve", ntri[:, :], ctmp[:, :])
            nd = QW // P
            dm = k.sb([P, nd, QW], F32, "dm", es)
            dmb = k.sb([P, nd, QW], BF16, "dmb", es)
            k.memset("pool", dm[:, :, :], 1.0)
            for jj in range(nd):
                k.affsel(dm[:, jj, :], dm[:, jj, :], [[1, QW]], ALU.is_gt, 0.0, -P * jj, -1)
            k.copy("dve", dmb[:, :, :], dm[:, :, :])
            er = [Ring(k, 2, [P, QW], F32, "e", es) for _ in range(2)]
            spr = [Ring(k, 4, [P, QW], F32, "sp", es) for _ in range(2)]
            rsr = [Ring(k, 3, [P, QW], F32, "rs", es) for _ in range(2)]
            wr_ = [Ring(k, 3, [P, QW], BF16, "w", es) for _ in range(2)]
            for par in range(2):
                for t_ in spr[par].t + rsr[par].t:
                    t_.buf.f32r = True
            pz = [Ring(k, 1, [P, 512], F32, "pz", es, psum=True) for _ in range(2)]
            pd = [Ring(k, 1, [P, 512], F32, "pd", es, psum=True) for _ in range(2)]
            pav = [Ring(k, 1, [P, 512], F32, "pav", es, psum=True) for _ in range(2)]
            for tq in range(S // QW):
                t0 = tq * QW
                Jtop = (t0 + QW) // P - 1
                for hp in range(4):
                    c = hp
                    RSs = [None, None]
                    avs = [pav[0].next(), pav[1].next()]
                    hsl = [slice(par * 64, par * 64 + 64) for par in range(2)]
                    qvs = [QT[hsl[par], c, t0:t0 + QW] for par in range(2)]

                    def front(J):
                        s0 = J * P
                        diag = (s0 + P - 1 >= t0)
                        jj = (s0 - t0) // P if diag else None
                        kvs = [KT[hsl[par], c, s0:s0 + P] for par in range(2)]
                        zts, es_, sps = [], [], []
                        for par in range(2):
                            zt = pz[par].next()
                            k.mm(zt[:, 0:QW], kvs[par], qvs[par])
                            zts.append(zt)
                        for par in range(2):
                            e_ = er[par].next()
                            k.act(e_[:, :], zts[par][:, 0:QW], AF.Exp)
                            es_.append(e_)
                        for par in range(2):
                            sp = spr[par].next()
                            k.act(sp[:, :], es_[par][:, :], AF.Ln, bias=1.0)
                            sps.append(sp)
                        if diag:
                            for par in range(2):
                                k.tt("dve", sps[par][:, :], sps[par][:, :], dm[:, jj, :], ALU.mult)
                        return (J, diag, jj, kvs, sps)

                    def back1(fr):
                        J, diag, jj, kvs, sps = fr
                        ds, ws = [], []
                        for par in range(2):
                            RS = RSs[par]
                            d_ = pd[par].next()
                            k.mm(d_[:, 0:QW], kvs[par], qvs[par], start=True, stop=False)
                            k.mm(d_[:, 0:QW], ntri[:, :], sps[par][:, :], start=False, stop=(RS is None))
                            if RS is not None:
                                k.mm(d_[:, 0:QW], nones[:, :], RS[:, :], start=False, stop=True)
                            ds.append(d_)
                        for par in range(2):
                            w_ = wr_[par].next()
                            k.act(w_[:, :], ds[par][:, 0:QW], AF.Exp)
                            ws.append(w_)
                        if diag:
                            for par in range(2):
                                k.tt("dve", ws[par][:, :], ws[par][:, :], dmb[:, jj, :], ALU.mult)
                        if J > 0:
                            for par in range(2):
                                if RSs[par] is None:
                                    RSs[par] = sps[par]
                                else:
                                    RSn = rsr[par].next()
                                    k.tt("dve", RSn[:, :], RSs[par][:, :], sps[par][:, :], ALU.add)
                                    RSs[par] = RSn
                        return (J, ws)

                    def back2(pend):
                        J, ws = pend
                        for par in range(2):
                            h = 2 * hp + par
                            k.mm(avs[par][hsl[par], 0:QW], Vt[:, J, h * 64:(h + 1) * 64], ws[par][:, :],
                                 start=(J == Jtop), stop=(J == 0))

                    fr = front(Jtop)
                    pend = None
                    for J in range(Jtop, -1, -1):
                        nxt = front(J - 1) if J > 0 else None
                        cur = back1(fr)
                        if pend is not None:
                            back2(pend)
                        pend = cur
                        fr = nxt
                    back2(pend)
                    for par in range(2):
                        k.copy("dve", AO[hsl[par], c, t0:t0 + QW], avs[par][hsl[par], 0:QW])
            k.dma("sp", U(dAH[:, 0:4, :]), AO[:, :, :])
            self.barrier()
        if EVSTOP == 3:
            k.dead = True
        with ExitStack() as es:
            MQK = k.sb([P, 8, S], BF16, "MQK", es)
            MV = k.sb([P, NT, 512], BF16, "MV", es)
            OG = k.sb([P, NT, 512], BF16, "OG", es)
            k.dma("sp", MQK[:, :, :], U(dMQK[:, :, :]))
            k.dma("sp", MV[:, :, :], U(dMV.rearrange("(n p) d -> p n d", p=P)))
            k.dma("sp", OG[:, :, :], U(dOG.rearrange("(n p) d -> p n d", p=P)))
            hgb = k.sb([P, 512], F32, "hgb", es)
            k.dma("sp", hgb[:, :], U(self.e_head_g.h[e, :, :].rearrange("h d -> (h d)").partition_broadcast(P)))
            mlm = k.sb([P, P], F32, "mlm", es)
            k.memset("pool", mlm[:, :], RSD)
            k.affsel(mlm[:, :], mlm[:, :], [[1, P]], ALU.is_ge, 0.0, 0, -1)
            Cst = [k.sb([P, 132], F32, "Cst%d" % h, es) for h in range(4)]
            Cbf = [k.sb([P, 132], BF16, "Cbf%d" % h, es) for h in range(4)]
            for h in range(4):
                k.memset("pool", Cst[h][:, :], 0.0)
                k.memset("pool", Cbf[h][:, :], 0.0)
            va1 = Ring(k, 4, [P, 132], BF16, "va1", es)
            va2 = Ring(k, 4, [P, 132], BF16, "va2", es)
            pmr = Ring(k, 4, [P, P], BF16, "pm", es)
            ktr = Ring(k, 2, [P, 2, P], BF16, "kt", es)
            sm = Ring(k, 8, [P, 8], F32, "sm", es)
            tmpf = Ring(k, 4, [P, P], F32, "tmpf", es)
            junk = k.sb([P, 2, P], BF16, "junk", es)
            Htk = Ring(k, 2, [P, 512], BF16, "Htk", es)
            HTs = Ring(k, 2, [P, 4, P], BF16, "HTs", es)
            pst = Ring(k, 2, [P, 512], F32, "pst", es, psum=True)
            px = Ring(k, 2, [P, 512], F32, "px", es, psum=True)
            pu = Ring(k, 2, [P, 512], F32, "pu", es, psum=True)
            pkt = Ring(k, 1, [P, 8, P], BF16, "pkt", es, psum=True)
            pht = Ring(k, 1, [P, 8, P], BF16, "pht", es, psum=True)
            for c in range(NT):
                cs = slice(c * P, (c + 1) * P)
                Ht = Htk.next()
                for hp2 in range(2):
                    hh = [2 * hp2, 2 * hp2 + 1]
                    hcs = [slice(h * P, (h + 1) * P) for h in hh]
                    qTs = [MQK[:, h, cs] for h in hh]
                    kTs = [MQK[:, 4 + h, cs] for h in hh]
                    v1s, v2s = [], []
                    for i, h in enumerate(hh):
                        v1, v2 = va1.next(), va2.next()
                        k.ts("pool", v1[:, 0:P], MV[:, c, hcs[i]], ECt[:, c, h:h + 1], ALU.mult)
                        k.copy("pool", v1[:, P:P + 1], ECt[:, c, h:h + 1])
                        k.ts("pool", v2[:, 0:P], MV[:, c, hcs[i]], EC2t[:, c, h:h + 1], ALU.mult)
                        k.copy("pool", v2[:, P:P + 1], EC2t[:, c, h:h + 1])
                        v1s.append(v1)
                        v2s.append(v2)
                    sts = []
                    for i in range(2):
                        st_ = pst.next()
                        k.mm(st_[:, 0:P], kTs[i], qTs[i])
                        sts.append(st_)
                    kp_ = pkt.next()
                    for i in range(2):
                        k.tr(kp_[:, i, :], kTs[i], self.ident_b[:, :])
                    pms = []
                    for i in range(2):
                        pm = pmr.next()
                        k.tt("dve", pm[:, :], sts[i][:, 0:P], mlm[:, :], ALU.mult)
                        pms.append(pm)
                    kt_ = ktr.next()
                    k.copy("act", kt_[:, :, :], kp_[:, 0:2, :])
                    xs, us = [], []
                    for i, h in enumerate(hh):
                        x_ = px.next()
                        k.mm(x_[:, 0:P + 1], pms[i][:, :], v1s[i][:, 0:P + 1], start=True, stop=False)
                        k.mm(x_[:, 0:P + 1], qTs[i], Cbf[h][:, 0:P + 1], start=False, stop=True)
                        xs.append(x_)
                    for i in range(2):
                        u_ = pu.next()
                        k.mm(u_[:, 0:P + 1], kt_[:, i, :], v2s[i][:, 0:P + 1])
                        us.append(u_)
                    for i, h in enumerate(hh):
                        k.stt(Cst[h][:, 0:P + 1], Cst[h][:, 0:P + 1], wstB[:, h, c:c + 1], us[i][:, 0:P + 1], ALU.mult, ALU.add)
                    for i, h in enumerate(hh):
                        k.ts("pool", Cbf[h][:, 0:P + 1], Cst[h][:, 0:P + 1], RSD, ALU.mult)
                    ss_ = [sm.next(), sm.next()]
                    for i in range(2):
                        k.ts("dve", ss_[i][:, 5:6], xs[i][:, P:P + 1], -1.0, ALU.mult)
                    for i in range(2):
                        k.tt("dve", ss_[i][:, 0:1], xs[i][:, P:P + 1], ss_[i][:, 5:6], ALU.max)
                    for i, h in enumerate(hh):
                        k.ts("dve", ss_[i][:, 0:1], ss_[i][:, 0:1], THRt[:, c, h:h + 1], ALU.max)
                    for i in range(2):
                        k.recip(ss_[i][:, 1:2], ss_[i][:, 0:1])
                    for i in range(2):
                        k.act(View(junk.h[:, i, :], Buf()), xs[i][:, 0:P], AF.Square, scale=ss_[i][:, 1:2], accum=ss_[i][:, 2:3])
                    for i in range(2):
                        k.act(ss_[i][:, 3:4], ss_[i][:, 2:3], AF.Ln, bias=self.eps_t[:, 0:1], scale=1.0 / P)
                    for i in range(2):
                        k.act(ss_[i][:, 3:4], ss_[i][:, 3:4], AF.Exp, scale=-0.5)
                    for i in range(2):
                        k.tt("dve", ss_[i][:, 4:5], ss_[i][:, 3:4], ss_[i][:, 1:2], ALU.mult)
                    tfs = []
                    for i in range(2):
                        tf = tmpf.next()
                        k.stt(tf[:, :], xs[i][:, 0:P], ss_[i][:, 4:5], hgb[:, hcs[i]], ALU.mult, ALU.mult)
                        tfs.append(tf)
                    for i in range(2):
                        k.tt("pool", Ht[:, hcs[i]], tfs[i][:, :], OG[:, c, hcs[i]], ALU.mult)
                hp = pht.next()
                for h in range(4):
                    k.tr(hp[:, h, :], Ht[:, h * P:(h + 1) * P], self.ident_b[:, :])
                HT = HTs.next()
                k.copy("act", HT[:, :, :], hp[:, 0:4, :])
                k.dma("pool", U(dAH[:, 4:8, cs]), HT[:, :, :])
            self.barrier()
        if EVSTOP == 4:
            k.dead = True
        with ExitStack() as es:
            wo = k.sb([P, 8, D], BF16, "wo", es)
            with ExitStack() as es2:
                stage = Ring(k, 2, [P, 2048], F32, "stg", es2)
                self.load_w(lambda kd, c0, cw: wo[:, kd, c0:c0 + cw],
                            lambda kd, c0, cw: self.e_w_out[e, kd * P:(kd + 1) * P, c0:c0 + cw], 8, D, stage, None)
                self.barrier()
            g1b = k.sb([P, D], F32, "g1b", es)
            k.dma("sp", g1b[:, :], U(self.norm_g.h[layer, 1, :].partition_broadcast(P)))
            ahs = Ring(k, 2, [P, 8, P], BF16, "ah", es)
            hts = Ring(k, 2, [P, D], F32, "ht", es)
            yts = Ring(k, 2, [P, D], F32, "yt", es)
            junk = k.sb([P, D], BF16, "junk", es)
            sss = Ring(k, 4, [P, 4], F32, "ss", es)
            dpsr = Ring(k, 4, [P, 512], F32, "dps", es, psum=True)
            for ti in range(NT):
                ts_ = slice(ti * P, (ti + 1) * P)
                ah = ahs.next()
                k.dma("sp", ah[:, :, :], U(dAH[:, :, ts_]))
                ht = hts.next()
                k.dma("sp", ht[:, :], hsrc.sub(ti)[ts_, :])
                dps = [dpsr.next(), dpsr.next()]
                for hf in range(2):
                    for c in range(8):
                        k.mm(dps[hf][:, :], ah[:, c, :], wo[:, c, hf * 512:(hf + 1) * 512], start=(c == 0), stop=(c == 7))
                self.post_norm_add(dps, ht, g1b, hdst, ti, sss.next(), junk, yts.next())
            k.dead = False
            self.barrier()


Prog.even = _even


def build_full(S=SEQ, depth=DEPTH):
    prog = Prog(S=S)
    prog.declare()
    prog.consts()
    cur = prog.x
    bufs = [prog.hA, prog.hB]
    bi = 0
    for layer in range(depth):
        last = (layer == depth - 1)
        mid = bufs[bi]
        bi ^= 1
        if layer % 2 == 0:
            prog.even(layer, cur, mid)
        else:
            prog.rwkv(layer, cur, mid)
        prog.barrier()
        dst = prog.out if last else bufs[bi]
        bi ^= 1
        prog.mlp(layer, mid, dst)
        prog.barrier()
        cur = dst
    prog.finish(prog.out)
    return prog


def kernel(**inputs):
    inputs = {k_: np.asarray(v) for k_, v in inputs.items()}
    prog = build_full()
    in_maps = [prog.in_map(inputs, b) for b in range(BATCH)]
    res = run_bass_kernel_spmd(prog.nc, in_maps, core_ids=list(range(BATCH)))
    return np.stack([np.asarray(res.results[b]["out"]) for b in range(BATCH)], axis=0).astype(np.float32)
```

```python
import numpy as np
from contextlib import ExitStack
import concourse.bass as bass
import concourse.mybir as mybir
from concourse.bass_utils import run_bass_kernel_spmd

F32 = mybir.dt.float32
F32R = mybir.dt.float32r
BF16 = mybir.dt.bfloat16
AF = mybir.ActivationFunctionType
ALU = mybir.AluOpType
AX = mybir.AxisListType

P = 128
D = 1024
DFF = 4096
SEQ = 4096
BATCH = 8
DEPTH = 4
NORM_EPS = 1e-6
GN_EPS = 64e-5
IN_COLS = 3592
NDS = 20
USE_F32R = True
import os as _os
RWSTOP = int(_os.environ.get('RWSTOP', '0'))
EVSTOP = int(_os.environ.get('EVSTOP', '0'))


class Buf:
    __slots__ = ("w", "r", "f32r")

    def __init__(self):
        self.w = None
        self.r = {}
        self.f32r = False


class View:
    __slots__ = ("ap", "buf")

    def __init__(self, ap, buf):
        self.ap = ap
        self.buf = buf


class TT:
    def __init__(self, h, buf=None):
        self.h = h
        self.buf = buf if buf is not None else Buf()
        self._subs = {}

    def __getitem__(self, key):
        return View(self.h[key], self.buf)

    def sub(self, key):
        if key not in self._subs:
            self._subs[key] = TT(self.h)
        return self._subs[key]

    def v(self, ap):
        return View(ap, self.buf)


class K:
    def __init__(self, nc, es):
        self.nc = nc
        self.es = es
        self.E = {"pe": nc.tensor, "act": nc.scalar, "dve": nc.vector, "pool": nc.gpsimd, "sp": nc.sync}
        self.esem = {}
        self.ecnt = {}
        for e in ("pe", "act", "dve", "pool"):
            self.esem[e] = es.enter_context(nc.semaphore("es_" + e))
            self.ecnt[e] = 0
        self.seen = {e: {} for e in self.E}
        self.dq = {}
        for q in ("sp", "pool", "act"):
            sems = [es.enter_context(nc.semaphore("ds_%s%d" % (q, i))) for i in range(NDS)]
            self.dq[q] = {"sems": sems, "cnt": [0] * NDS, "i": 0}
        self.semname = {}
        self.uid = 0
        self.n_ins = 0
        self.dead = False

    def stop(self, n):
        if RWSTOP == n:
            self.dead = True

    def sb(self, shape, dt, name=None, es=None):
        self.uid += 1
        h = (es or self.es).enter_context(self.nc.sbuf_tensor("%s_%d" % (name or "sb", self.uid), list(shape), dt))
        return TT(h)

    def ps(self, shape, dt=F32, name=None, es=None):
        self.uid += 1
        h = (es or self.es).enter_context(self.nc.psum_tensor("%s_%d" % (name or "ps", self.uid), list(shape), dt))
        return TT(h)

    def dram(self, name, shape, dt, kind="Internal"):
        return TT(self.nc.dram_tensor(name, list(shape), dt, kind=kind).ap())

    def _key(self, sem):
        return id(sem)

    def _waits(self, en, outs, ins, extra=()):
        need = {}

        def add(tok):
            if tok is None:
                return
            s, v = tok
            if en == "pe" and s is self.esem["pe"]:
                return
            k = id(s)
            if k not in need or need[k][1] < v:
                need[k] = (s, v)

        for x in ins:
            add(x.buf.w)
        for x in outs:
            add(x.buf.w)
            for t in x.buf.r.values():
                add(t)
        for t in extra:
            add(t)
        eng = self.E[en]
        seen = self.seen[en]
        for k, (s, v) in need.items():
            if seen.get(k, 0) < v:
                eng.wait_ge(s, v)
                seen[k] = v
                self.n_ins += 1

    def _done(self, tok, outs, ins):
        k = id(tok[0])
        for x in ins:
            x.buf.r[k] = tok
        for x in outs:
            x.buf.w = tok
            x.buf.r = {}

    def emit(self, en, fn, outs, ins):
        if self.dead:
            return
        outs = [o for o in outs if isinstance(o, View)]
        ins = [i for i in ins if isinstance(i, View)]
        self._waits(en, outs, ins)
        ins_obj = fn()
        self.ecnt[en] += 1
        tok = (self.esem[en], self.ecnt[en])
        ins_obj.then_inc(tok[0], 1)
        self.n_ins += 1
        self._done(tok, outs, ins)

    def dma(self, q, out, in_):
        if self.dead:
            return
        dq = self.dq[q]
        i = dq["i"] % NDS
        dq["i"] += 1
        sem = dq["sems"][i]
        self._waits(q, [out], [in_], extra=[(sem, dq["cnt"][i])] if dq["cnt"][i] else [])
        ins_obj = self.E[q].dma_start(out=out.ap, in_=in_.ap)
        dq["cnt"][i] += 16
        tok = (sem, dq["cnt"][i])
        ins_obj.then_inc(sem, 16)
        self.n_ins += 1
        self._done(tok, [out], [in_])

    def wait_all(self, en, views):
        self._waits(en, [], views)

    @staticmethod
    def _a(x):
        return x.ap if isinstance(x, View) else x

    @staticmethod
    def _o(x):
        return x.ap.bitcast(F32R) if (x.buf.f32r and USE_F32R) else x.ap

    def mm(self, out, lhsT, rhs, start=True, stop=True, r=None):
        if r is None:
            r = lhsT.buf.f32r and rhs.buf.f32r
        r = r and USE_F32R
        la, ra = (lhsT.ap.bitcast(F32R), rhs.ap.bitcast(F32R)) if r else (lhsT.ap, rhs.ap)
        self.emit("pe", lambda: self.nc.tensor.matmul(out.ap, lhsT=la, rhs=ra, start=start, stop=stop),
                  [out], [lhsT, rhs])

    def tr(self, out, in_, ident):
        self.emit("pe", lambda: self.nc.tensor.transpose(out.ap, in_.ap, ident.ap), [out], [in_, ident])

    def act(self, out, in_, func, bias=None, scale=None, accum=None):
        kw = {}
        if bias is not None:
            kw["bias"] = self._a(bias)
        if scale is not None:
            kw["scale"] = self._a(scale)
        if accum is not None:
            kw["accum_out"] = accum.ap
        self.emit("act", lambda: self.nc.scalar.activation(out=self._o(out), in_=in_.ap, func=func, **kw),
                  [out, accum], [in_, bias, scale])

    def tt(self, en, out, in0, in1, op):
        self.emit(en, lambda: self.E[en].tensor_tensor(out=self._o(out), in0=in0.ap, in1=in1.ap, op=op), [out], [in0, in1])

    def ts(self, en, out, in0, s1, op0, s2=None, op1=None, accum=None):
        kw = {}
        if op1 is not None:
            kw["op1"] = op1
        if accum is not None:
            kw["accum_out"] = accum.ap
        self.emit(en, lambda: self.E[en].tensor_scalar(out=self._o(out), in0=in0.ap, scalar1=self._a(s1),
                                                       scalar2=self._a(s2), op0=op0, **kw),
                  [out, accum], [in0, s1, s2])

    def stt(self, out, in0, scalar, in1, op0, op1, accum=None):
        kw = {}
        if accum is not None:
            kw["accum_out"] = accum.ap
        self.emit("dve", lambda: self.nc.vector.scalar_tensor_tensor(out=self._o(out), in0=in0.ap, scalar=self._a(scalar),
                                                                     in1=in1.ap, op0=op0, op1=op1, **kw),
                  [out, accum], [in0, scalar, in1])

    def copy(self, en, out, in_):
        if en == "act":
            self.emit("act", lambda: self.nc.scalar.copy(out=self._o(out), in_=in_.ap), [out], [in_])
        else:
            self.emit(en, lambda: self.E[en].tensor_copy(out=self._o(out), in_=in_.ap), [out], [in_])

    def recip(self, out, in_):
        self.emit("dve", lambda: self.nc.vector.reciprocal(out=out.ap, in_=in_.ap), [out], [in_])

    def memset(self, en, out, val):
        self.emit(en, lambda: self.E[en].memset(out.ap, val), [out], [])

    def scan(self, out, d0, d1, init, op0, op1):
        self.emit("dve", lambda: self.nc.vector.tensor_tensor_scan(out=out.ap, data0=d0.ap, data1=d1.ap,
                                                                   initial=self._a(init), op0=op0, op1=op1),
                  [out], [d0, d1, init])

    def reduce(self, out, in_, op, axis=AX.X):
        self.emit("dve", lambda: self.nc.vector.tensor_reduce(out=out.ap, in_=in_.ap, axis=axis, op=op), [out], [in_])

    def affsel(self, out, in_, pattern, cmp, fill, base, cm):
        self.emit("pool", lambda: self.nc.gpsimd.affine_select(out=self._o(out), in_=in_.ap, pattern=pattern, compare_op=cmp,
                                                               fill=fill, base=base, channel_multiplier=cm),
                  [out], [in_])


class Ring:
    def __init__(self, k, n, shape, dt, name, es=None, psum=False):
        self.t = [(k.ps if psum else k.sb)(shape, dt, name=name, es=es) for _ in range(n)]
        self.i = 0

    def next(self):
        t = self.t[self.i % len(self.t)]
        self.i += 1
        return t


class Prog:
    def __init__(self, S=SEQ, layers=None, dbg=None):
        self.S = S
        self.NT = S // P
        self.layers = list(range(DEPTH)) if layers is None else layers
        self.dbg = dbg or {}
        nc = bass.Bass("TRN2", target_bir_lowering=False)
        self.nc = nc
        self.es = ExitStack()
        self.k = K(nc, self.es)

    SHAPES = {
        "norm_g": [DEPTH, 4, D], "e_w_in": [2, D, IN_COLS], "e_b_if": [2, 8], "e_conv_w": [2, 4, 1024],
        "e_head_g": [2, 4, 128], "e_w_out": [2, D, D], "r_mu": [2, 6, D], "r_w_rkv": [2, 3, D, D],
        "r_w0": [2, D], "r_w1": [2, D, 64], "r_w2": [2, 64, D], "r_a0": [2, D], "r_a1": [2, D, 64],
        "r_a2": [2, 64, D], "r_g1": [2, D, 128], "r_g2": [2, 128, D], "r_k_k": [2, D], "r_k_a": [2, D],
        "r_r_k": [2, D], "r_ln_g": [2, D], "r_ln_b": [2, D], "r_w_out": [2, D, D],
        "mlp_w_up": [DEPTH, D, DFF], "mlp_w_down": [DEPTH, DFF, D],
    }

    def __getattr__(self, name):
        if name in Prog.SHAPES:
            t = self.k.dram(name, Prog.SHAPES[name], F32, kind="ExternalInput")
            self.used.append(name)
            setattr(self, name, t)
            return t
        raise AttributeError(name)

    def declare(self):
        k = self.k
        S = self.S
        self.used = []
        self.x = k.dram("x", [S, D], F32, kind="ExternalInput")
        self.out = k.dram("out", [S, D], F32, kind="ExternalOutput")
        self.hA = k.dram("hA", [S, D], F32)
        self.hB = k.dram("hB", [S, D], F32)

    def in_map(self, inputs, b):
        m = {"x": np.ascontiguousarray(inputs["x"][b, :self.S])}
        for name in self.used:
            m[name] = np.ascontiguousarray(inputs[name]).reshape(Prog.SHAPES[name])
        return m

    def barrier(self):
        k = self.k
        toks = [(k.esem[e], k.ecnt[e]) for e in k.esem if k.ecnt[e]]
        for q in k.dq.values():
            for s_, c_ in zip(q["sems"], q["cnt"]):
                if c_:
                    toks.append((s_, c_))
        for en in ("pe", "act", "dve", "pool", "sp"):
            seen = k.seen[en]
            for s_, v_ in toks:
                if seen.get(id(s_), 0) < v_:
                    k.E[en].wait_ge(s_, v_)
                    seen[id(s_)] = v_
                    k.n_ins += 1

    def consts(self):
        k = self.k
        nc = self.nc
        self.ident_f = k.sb([P, P], F32, "identf")
        self.ident_b = k.sb([P, P], BF16, "identb")
        k.memset("pool", self.ident_f[:, :], 1.0)
        k.affsel(self.ident_f[:, :], self.ident_f[:, :], [[-1, P]], ALU.is_equal, 0.0, 0, 1)
        k.copy("dve", self.ident_b[:, :], self.ident_f[:, :])
        self.eps_t = k.sb([P, 1], F32, "eps")
        k.memset("dve", self.eps_t[:, :], NORM_EPS)

    def bcast_load(self, dst, src_ap, n):
        self.k.dma("sp", dst, View(src_ap.partition_broadcast(P), Buf()))

    def rstd(self, out, ss, dn, eps_t=None):
        k = self.k
        k.act(out, ss, AF.Ln, bias=(eps_t or self.eps_t)[:, 0:1], scale=1.0 / dn)
        k.act(out, out, AF.Exp, scale=-0.5)

    def norm_T(self, hsrc, ti, ht, hn, ss, rs, junk, tps, uT_dst):
        k = self.k
        k.dma("sp", ht[:, :], hsrc.sub(ti)[ti * P:(ti + 1) * P, :])
        k.act(View(junk.h[:, :], Buf()), ht[:, :], AF.Square, accum=ss[:, 0:1])
        self.rstd(rs[:, 0:1], ss[:, 0:1], D)
        k.act(hn[:, :], ht[:, :], AF.Copy, scale=rs[:, 0:1])
        for c in range(8):
            k.tr(tps[:, c, :], hn[:, c * P:(c + 1) * P], self.ident_b[:, :])
        k.copy("dve", uT_dst, tps[:, :, :])

    def norm_part(self, hsrc, ti, ht, hn, ss, rs, junk):
        k = self.k
        k.dma("sp", ht[:, :], hsrc.sub(ti)[ti * P:(ti + 1) * P, :])
        k.act(View(junk.h[:, :], Buf()), ht[:, :], AF.Square, accum=ss[:, 0:1])
        self.rstd(rs[:, 0:1], ss[:, 0:1], D)
        k.act(hn[:, :], ht[:, :], AF.Copy, scale=rs[:, 0:1])

    def tr_part(self, hn, tps, uT_dst):
        k = self.k
        for c in range(8):
            k.tr(tps[:, c, :], hn[:, c * P:(c + 1) * P], self.ident_b[:, :])
        k.copy("dve", uT_dst, tps[:, :, :])

    def mlp(self, layer, hsrc, hdst):
        k = self.k
        nc = self.nc
        with ExitStack() as es:
            wu = k.sb([P, 8, DFF], BF16, "wu", es)
            wd = k.sb([P, 32, D], BF16, "wd", es)
            g2 = k.sb([P, 8], F32, "g2", es)
            g3b = k.sb([P, D], F32, "g3b", es)
            stage = Ring(k, 2, [P, 2048], F32, "stg", es)
            with nc.allow_non_contiguous_dma(reason="tiny param"):
                k.dma("sp", g2[:, :], self.norm_g.v(self.norm_g.h[layer, 2, :].rearrange("(c p) -> p c", p=P)))
            k.dma("sp", g3b[:, :], self.norm_g.v(self.norm_g.h[layer, 3, :].partition_broadcast(P)))
            n = 0
            for kd in range(8):
                for hf in range(2):
                    st = stage.next()
                    k.dma("sp", st[:, :], self.mlp_w_up[layer, kd * P:(kd + 1) * P, hf * 2048:(hf + 1) * 2048])
                    if n % 2 == 0:
                        k.act(wu[:, kd, hf * 2048:(hf + 1) * 2048], st[:, :], AF.Copy, scale=g2[:, kd:kd + 1])
                    else:
                        k.ts("pool", wu[:, kd, hf * 2048:(hf + 1) * 2048], st[:, :], g2[:, kd:kd + 1], ALU.mult)
                    n += 1
            for c2 in range(16):
                st = stage.next()
                src = self.mlp_w_down.h[layer, c2 * 256:(c2 + 1) * 256, :].rearrange("(c p) n -> p c n", p=P)
                k.dma("sp", st.v(st.h[:, :].rearrange("p (c n) -> p c n", c=2)), self.mlp_w_down.v(src))
                dst = wd.v(wd.h[:, 2 * c2:2 * c2 + 2, :])
                sv = st.v(st.h[:, :].rearrange("p (c n) -> p c n", c=2))
                if n % 2 == 0:
                    k.copy("dve", dst, sv)
                else:
                    k.copy("pool", dst, sv)
                n += 1

            TS = 256
            nsub = TS // P
            hts = Ring(k, 2 * nsub, [P, D], F32, "ht", es)
            hns = Ring(k, 2, [P, D], BF16, "hn", es)
            junk = k.sb([P, D], BF16, "junk", es)
            sss = Ring(k, 4, [P, 4], F32, "ss", es)
            uTs = Ring(k, 2, [P, 8, TS], BF16, "uT", es)
            aT = Ring(k, 1, [P, 32, TS], BF16, "aT", es)
            tpsr = Ring(k, 2, [P, 8, P], BF16, "tps", es, psum=True)
            upsr = Ring(k, 2, [P, 512], F32, "ups", es, psum=True)
            dpsr = Ring(k, 4, [P, 512], F32, "dps", es, psum=True)
            ytmp = Ring(k, 2, [P, D], F32, "yt", es)
            for st_i in range(self.S // TS):
                uT = uTs.next()
                hl = []
                for j in range(nsub):
                    ht = hts.next()
                    hl.append(ht)
                    s4 = sss.next()
                    self.norm_T(hsrc, st_i * nsub + j, ht, hns.next(), s4.sub(0), s4.sub(1), junk, tpsr.next(),
                                uT.v(uT.h[:, :, j * P:(j + 1) * P]))
                a = aT.next()
                for fc in range(32):
                    ups = upsr.next()
                    for kd in range(8):
                        k.mm(ups[:, 0:TS], wu[:, kd, fc * P:(fc + 1) * P], uT[:, kd, :], start=(kd == 0), stop=(kd == 7))
                    if fc % 2 == 0:
                        k.act(a[:, fc, :], ups[:, 0:TS], AF.Relu)
                        k.tt("pool", a[:, fc, :], a[:, fc, :], a[:, fc, :], ALU.mult)
                    else:
                        k.ts("dve", a[:, fc, :], ups[:, 0:TS], 0.0, ALU.max)
                        k.tt("pool", a[:, fc, :], a[:, fc, :], a[:, fc, :], ALU.mult)
                for j in range(nsub):
                    dps = [dpsr.next(), dpsr.next()]
                    for hf in range(2):
                        for kc in range(32):
                            k.mm(dps[hf][:, :], a[:, kc, j * P:(j + 1) * P], wd[:, kc, hf * 512:(hf + 1) * 512],
                                 start=(kc == 0), stop=(kc == 31))
                    self.post_norm_add(dps, hl[j], g3b, hdst, st_i * nsub + j, sss.next(), junk, ytmp.next())

    def post_norm_add(self, dps, ht, gb, hdst, ti, s4, junk, yt):
        k = self.k
        ssa, ssb, rs = s4.sub(0), s4.sub(1), s4.sub(2)
        k.act(View(junk.h[:, 0:512], Buf()), dps[0][:, :], AF.Square, accum=ssa[:, 0:1])
        k.act(View(junk.h[:, 512:1024], Buf()), dps[1][:, :], AF.Square, accum=ssb[:, 1:2])
        k.tt("dve", rs[:, 2:3], ssa[:, 0:1], ssb[:, 1:2], ALU.add)
        self.rstd(rs[:, 2:3], rs[:, 2:3], D)
        for hf in range(2):
            sl = slice(hf * 512, (hf + 1) * 512)
            k.stt(yt[:, sl], dps[hf][:, :], rs[:, 2:3], gb[:, sl], ALU.mult, ALU.mult)
            k.tt("pool", yt[:, sl], yt[:, sl], ht[:, sl], ALU.add)
        k.dma("pool", hdst.sub(ti)[ti * P:(ti + 1) * P, :], yt[:, :])

    def finish(self, last):
        k = self.k
        vs = [last.sub(ti)[:, :] for ti in range(self.NT)]
        k.wait_all("sp", vs)
        k.wait_all("pool", vs)


class PSlots:
    def __init__(self, k, nbanks, es, name):
        self.slots = []
        for b in range(nbanks):
            t = k.ps([P, 4, P], F32, name=name, es=es)
            self.slots.append((t, 0))
        self.i = 0

    def next(self):
        s = self.slots[self.i % len(self.slots)]
        self.i += 1
        return Slot(*s)


class Slot:
    def __init__(self, tt, j):
        self.tt = tt
        self.j = j

    def __getitem__(self, key):
        ps, fs = key
        if self.j is None:
            if fs == slice(None):
                fs = slice(0, P)
            return View(self.tt.h[ps, fs], self.tt.buf)
        return View(self.tt.h[ps, self.j, fs], self.tt.buf)


def bc3(tt, ap2, n):
    shp = list(ap2.shape)
    return View(ap2.unsqueeze(2).to_broadcast([shp[0], shp[1], n]), tt.buf)


def _load_w(self, dst_view_fn, src_rows_fn, nchunks, cols, es_stage, gain=None):
    k = self.k
    for kd in range(nchunks):
        for c0 in range(0, cols, 2048):
            cw = min(2048, cols - c0)
            st = es_stage.next()
            k.dma("sp", st[:, 0:cw], src_rows_fn(kd, c0, cw))
            self._wn = getattr(self, "_wn", 0) + 1
            dst = dst_view_fn(kd, c0, cw)
            if gain is not None:
                if self._wn % 2 == 0:
                    k.act(dst, st[:, 0:cw], AF.Copy, scale=gain[:, kd:kd + 1])
                else:
                    k.ts("pool", dst, st[:, 0:cw], gain[:, kd:kd + 1], ALU.mult)
            else:
                k.copy("dve" if self._wn % 2 == 0 else "pool", dst, st[:, 0:cw])


Prog.load_w = _load_w


def _fm_param(self, dst, src_ap, es=None):
    with self.nc.allow_non_contiguous_dma(reason="tiny param"):
        self.k.dma("sp", dst, View(src_ap.rearrange("(c p) -> p c", p=P), Buf()))


Prog.fm_param = _fm_param


def _rwkv(self, layer, hsrc, hdst):
    k = self.k
    nc = self.nc
    o = layer // 2
    with ExitStack() as es:
        g0 = k.sb([P, 8], F32, "g0", es)
        self.fm_param(g0[:, :], self.norm_g.h[layer, 0, :])
        g1b = k.sb([P, D], F32, "g1b", es)
        k.dma("sp", g1b[:, :], View(self.norm_g.h[layer, 1, :].partition_broadcast(P), Buf()))
        lngb = k.sb([P, D], F32, "lngb", es)
        k.dma("sp", lngb[:, :], View(self.r_ln_g.h[o, :].partition_broadcast(P), Buf()))
        lnbb = k.sb([P, D], F32, "lnbb", es)
        k.dma("sp", lnbb[:, :], View(self.r_ln_b.h[o, :].partition_broadcast(P), Buf()))
        mu = k.sb([P, 6, 8], F32, "mu", es)
        for i in range(6):
            self.fm_param(mu[:, i, :], self.r_mu.h[o, i, :])
        fp = {}
        for nm, src in (("w0", self.r_w0), ("a0", self.r_a0), ("kk", self.r_k_k), ("ka", self.r_k_a), ("rk", self.r_r_k)):
            fp[nm] = k.sb([P, 8], F32, nm, es)
            self.fm_param(fp[nm][:, :], src.h[o, :])
        gneps = k.sb([P, 1], F32, "gneps", es)
        k.memset("dve", gneps[:, :], GN_EPS)
        wr = k.sb([P, 8, D], BF16, "wr", es)
        wk = k.sb([P, 8, D], BF16, "wk", es)
        wv = k.sb([P, 8, D], BF16, "wv", es)
        wo = k.sb([P, 8, D], BF16, "wo", es)
        w1 = k.sb([P, 8, 64], BF16, "w1", es)
        a1 = k.sb([P, 8, 64], BF16, "a1", es)
        g1 = k.sb([P, 8, P], BF16, "g1", es)
        w2 = k.sb([64, D], BF16, "w2", es)
        a2 = k.sb([64, D], BF16, "a2", es)
        g2 = k.sb([P, D], BF16, "g2", es)
        with ExitStack() as es2:
            stage = Ring(k, 2, [P, 2048], F32, "stg", es2)
            for wt, i in ((wr, 0), (wk, 1), (wv, 2)):
                self.load_w(lambda kd, c0, cw, wt=wt: wt[:, kd, c0:c0 + cw],
                            lambda kd, c0, cw, i=i: self.r_w_rkv[o, i, kd * P:(kd + 1) * P, c0:c0 + cw], 8, D, stage, g0)
            self.load_w(lambda kd, c0, cw: wo[:, kd, c0:c0 + cw],
                        lambda kd, c0, cw: self.r_w_out[o, kd * P:(kd + 1) * P, c0:c0 + cw], 8, D, stage, None)
            for wt, src, cols in ((w1, self.r_w1, 64), (a1, self.r_a1, 64), (g1, self.r_g1, P)):
                self.load_w(lambda kd, c0, cw, wt=wt: wt[:, kd, c0:c0 + cw],
                            lambda kd, c0, cw, src=src: src[o, kd * P:(kd + 1) * P, c0:c0 + cw], 8, cols, stage, g0)
            for wt, src in ((w2, self.r_w2), (a2, self.r_a2)):
                st = stage.next()
                k.dma("sp", st[0:64, 0:D], src[o, :, :])
                k.copy("dve", wt[:, :], st[0:64, 0:D])
            st = stage.next()
            k.dma("sp", st[:, 0:D], self.r_g2[o, :, :])
            k.copy("dve", g2[:, :], st[:, 0:D])
            self.barrier()
        m_ts = k.sb([P, P], F32, "m_ts", es)
        mT_s = k.sb([P, P], F32, "mT_s", es)
        mT_i = k.sb([P, P], F32, "mT_i", es)
        bdones = k.sb([P, P], F32, "bdones", es)
        for m_, pat, cm, cmp in ((m_ts, -1, 1, ALU.is_gt), (mT_s, 1, -1, ALU.is_gt), (mT_i, 1, -1, ALU.is_ge)):
            k.memset("pool", m_[:, :], 0.0)
            for b in range(2):
                sl = slice(b * 64, b * 64 + 64)
                k.memset("pool", m_[sl, sl], 1.0)
                k.affsel(m_[sl, sl], m_[sl, sl], [[pat, 64]], cmp, 0.0, 0, cm)
        k.memset("pool", bdones[:, :], 0.0)
        for b in range(2):
            sl = slice(b * 64, b * 64 + 64)
            k.memset("pool", bdones[sl, sl], 1.0)
        headsel = k.sb([P, 8, 16], F32, "headsel", es)
        k.memset("pool", headsel[:, :, :], 0.0)
        for c in range(8):
            for b in range(2):
                k.memset("pool", headsel[b * 64:b * 64 + 64, c, 2 * c + b:2 * c + b + 1], 1.0)
        rmask = k.sb([P, P], F32, "rmask", es)
        k.memset("pool", rmask[:, :], 1.0)
        k.memset("pool", rmask[:, 0:1], 0.0)
        k.memset("pool", rmask[:, 64:65], 0.0)
        Fm = lambda nm: k.sb([P, 8, P], F32, nm, es)
        rT, kT, aT, sg, kk, scr, cum = Fm("rT"), Fm("kT"), Fm("aT"), Fm("sg"), Fm("kk"), Fm("scr"), Fm("cum")
        epos, eneg, eexc, erem = Fm("epos"), Fm("eneg"), Fm("eexc"), Fm("erem")
        ktT, alT, beT = Fm("ktT"), Fm("alT"), Fm("beT")
        kka, rbT = aT, epos
        for t_ in (rT, kT, aT, kk, scr, eneg, eexc, erem, epos, ktT, alT, beT):
            t_.buf.f32r = True
        Tm = lambda nm: k.sb([P, D], F32, nm, es)
        vtok, gate, Atok, Bhat, Khat, ytok, ht = Tm("vtok"), Tm("gate"), Tm("Atok"), Tm("Bhat"), Tm("Khat"), Tm("ytok"), Tm("ht")
        gl = k.sb([P, 16], F32, "gl", es)
        bonus = k.sb([P, 16], F32, "bonus", es)
        uTs = Ring(k, 2, [P, 8, P + 1], BF16, "uTh", es)
        xis = Ring(k, 2, [P, 8, P], BF16, "xi", es)
        hn = k.sb([P, D], BF16, "hn", es)
        junk = k.sb([P, D], BF16, "junk", es)
        sss = Ring(k, 4, [P, 4], F32, "ss", es)
        hw = k.sb([64, P], BF16, "hw", es)
        ha = k.sb([64, P], BF16, "ha", es)
        hg = k.sb([P, P], BF16, "hg", es)
        St = k.sb([P, 8, 64], F32, "St", es)
        for g in range(4):
            k.memset("pool", St.sub(g)[:, 2 * g:2 * g + 2, :], 0.0)
        gnt = [k.sb([P, 16], F32, "gn%d" % i, es) for i in range(4)] + [k.sb([P, D], F32, "ysq", es)]
        WB = []
        for t_ in (rT, kT, aT, kk, scr, eneg, eexc, erem):
            for half in range(2):
                WB.append(TT(t_.h[:, half * 4:(half + 1) * 4, :]))
        xcnt, acnt = [0], [0]
        for t_ in WB:
            t_.buf.f32r = True
        vtok.buf.f32r = True

        def handoff(srcs, dsts):
            toks = {}
            for s_ in srcs:
                for t in ([s_.buf.w] if s_.buf.w else []) + list(s_.buf.r.values()):
                    if id(t[0]) not in toks or toks[id(t[0])][1] < t[1]:
                        toks[id(t[0])] = t
            for d_ in dsts:
                d_.buf.w = None
                d_.buf.r = dict(toks)

        Yl = k.sb([P, 4, 64], F32, "Yl", es)
        Qe = k.sb([P, 2, P], F32, "Qe", es)
        MT = k.sb([P, 4, 64], F32, "MT", es)
        Nn = k.sb([P, 4, 64], F32, "Nn", es)
        glD = k.sb([P, 16, 64], F32, "glD", es)
        sid = k.sb([P, 64], F32, "sid", es)
        k.copy("pool", sid[0:64, :], self.ident_f[0:64, 0:64])
        k.copy("pool", sid[64:128, :], self.ident_f[64:128, 64:128])
        zb = k.sb([P, D], BF16, "zb", es)
        zT = k.sb([P, 8, P], BF16, "zT", es)
        tps = k.ps([P, 8, P], BF16, "tps", es)
        pG = Ring(k, 7, [P, 512], F32, "pG", es, psum=True)

        class _PA:
            def next(self_):
                t_ = pG.next()
                return Slot(t_, None)
        pA = _PA()
        pT = pG

        prev_uT = None
        for ti in range(self.NT):
            uT = uTs.next()
            s4 = sss.next()
            self.norm_T(hsrc, ti, ht, hn, s4.sub(0), s4.sub(1), junk, tps, uT.v(uT.h[:, :, 1:P + 1]))
            if ti == 0:
                k.memset("pool", uT[:, :, 0:1], 0.0)
            else:
                k.copy("pool", uT[:, :, 0:1], prev_uT[:, :, P:P + 1])
            prev_uT = uT
            xx = ktT
            k.tt("dve", xx[:, :, :], uT[:, :, 0:P], uT[:, :, 1:P + 1], ALU.subtract)
            LD = -0.6065306597126334

            def mix(i):
                xi = xis.next()
                k.tt("pool", xi[:, :, :], xx[:, :, :], bc3(mu, mu.h[:, i, :], P), ALU.mult)
                k.tt("dve", xi[:, :, :], xi[:, :, :], uT[:, :, 1:P + 1], ALU.add)
                return xi

            def proj_fm(wt, xi, dst):
                for n_ in range(8):
                    ps = pA.next()
                    for kd in range(8):
                        k.mm(ps[:, :], wt[:, kd, n_ * P:(n_ + 1) * P], xi[:, kd, :], start=(kd == 0), stop=(kd == 7))
                    k.copy("act", dst[:, n_, :], ps[:, :])

            xi_k = mix(2)
            xi_w = mix(1)
            proj_fm(wk, xi_k, kT)
            k.tt("pool", kk[:, :, :], kT[:, :, :], bc3(fp["kk"], fp["kk"].h[:, :], P), ALU.mult)
            k.tt("dve", scr[:, :, :], kk[:, :, :], kk[:, :, :], ALU.mult)
            ps = pA.next()
            for kd in range(8):
                k.mm(ps[0:64, :], w1[:, kd, :], xi_w[:, kd, :], start=(kd == 0), stop=(kd == 7))
            k.act(hw[:, :], ps[0:64, :], AF.Tanh)
            for n_ in range(8):
                ps = pA.next()
                k.mm(ps[:, :], w2[:, n_ * P:(n_ + 1) * P], hw[:, :])
                k.act(sg[:, n_, :], ps[:, :], AF.Sigmoid, bias=fp["w0"][:, n_:n_ + 1])
            xi_a = mix(4)
            for c in range(8):
                k.scan(cum[:, c, :], rmask[:, :], sg[:, c, :], 0.0, ALU.mult, ALU.add)
            k.act(epos[:, :, :], cum[:, :, :], AF.Exp, scale=LD)
            k.act(eneg[:, :, :], cum[:, :, :], AF.Exp, scale=-LD)
            k.tt("pool", sg[:, :, :], cum[:, :, :], sg[:, :, :], ALU.subtract)
            k.act(eexc[:, :, :], sg[:, :, :], AF.Exp, scale=LD)
            c16 = cum.h[:, :, :].rearrange("p c (j t) -> p (c j) t", t=64)
            cl = View(c16[:, :, 63:64].to_broadcast([P, 16, 64]), cum.buf)
            s16 = sg.v(sg.h[:, :, :].rearrange("p c (j t) -> p (c j) t", t=64))
            k.tt("dve", s16, cl, View(c16, cum.buf), ALU.subtract)
            k.act(erem[:, :, :], sg[:, :, :], AF.Exp, scale=LD)
            e16 = epos.h[:, :, :].rearrange("p c (j t) -> p (c j) t", t=64)
            k.copy("pool", gl.v(gl.h[:, :].unsqueeze(2)), View(e16[:, :, 63:64], epos.buf))
            ps = pA.next()
            for kd in range(8):
                k.mm(ps[0:64, :], a1[:, kd, :], xi_a[:, kd, :], start=(kd == 0), stop=(kd == 7))
            k.copy("act", ha[:, :], ps[0:64, :])
            for n_ in range(8):
                ps = pA.next()
                k.mm(ps[:, :], a2[:, n_ * P:(n_ + 1) * P], ha[:, :])
                k.act(aT[:, n_, :], ps[:, :], AF.Sigmoid, bias=fp["a0"][:, n_:n_ + 1])
            xi_r = mix(0)
            for hf in range(2):
                ps = pT.next()
                for c4 in range(4):
                    c = hf * 4 + c4
                    k.mm(ps[:, c4 * P:(c4 + 1) * P], bdones[:, :], scr[:, c, :])
                dstv = scr.v(scr.h[:, hf * 4:(hf + 1) * 4, :].rearrange("p c t -> p (c t)"))
                k.ts("dve", dstv, ps[:, :], 1e-24, ALU.max)
            k.act(scr[:, :, :], scr[:, :, :], AF.Ln)
            k.act(scr[:, :, :], scr[:, :, :], AF.Exp, scale=-0.5)
            k.tt("dve", kk[:, :, :], kk[:, :, :], scr[:, :, :], ALU.mult)
            k.stt(scr[:, :, :], aT[:, :, :], -1.0, bc3(fp["ka"], fp["ka"].h[:, :], P), ALU.add, ALU.mult)
            k.stt(kT[:, :, :], scr[:, :, :], 1.0, kT[:, :, :], ALU.add, ALU.mult)
            k.tt("pool", kka[:, :, :], kk[:, :, :], aT[:, :, :], ALU.mult)
            proj_fm(wr, xi_r, rT)
            xi_v = mix(3)
            k.tt("pool", scr[:, :, :], rT[:, :, :], kT[:, :, :], ALU.mult)
            k.tt("dve", scr[:, :, :], scr[:, :, :], bc3(fp["rk"], fp["rk"].h[:, :], P), ALU.mult)
            for hf in range(2):
                ps = pT.next()
                for kd in range(8):
                    k.mm(ps[:, :], xi_v[:, kd, :], wv[:, kd, hf * 512:(hf + 1) * 512], start=(kd == 0), stop=(kd == 7))
                k.copy("act", vtok[:, hf * 512:(hf + 1) * 512], ps[:, :])
            xi_g = mix(5)
            ps = pA.next()
            for c in range(8):
                k.mm(ps[:, 0:16], scr[:, c, :], headsel[:, c, :], start=(c == 0), stop=(c == 7))
            k.copy("act", bonus[:, :], ps[:, 0:16])
            ps = pA.next()
            for kd in range(8):
                k.mm(ps[:, :], g1[:, kd, :], xi_g[:, kd, :], start=(kd == 0), stop=(kd == 7))
            k.act(hg[:, :], ps[:, :], AF.Sigmoid)
            for hf in range(2):
                ps = pT.next()
                k.mm(ps[:, :], hg[:, :], g2[:, hf * 512:(hf + 1) * 512])
                k.copy("act", gate[:, hf * 512:(hf + 1) * 512], ps[:, :])
            k.stt(alT[:, :, :], kk[:, :, :], -1.0, eexc[:, :, :], ALU.mult, ALU.mult)
            k.tt("dve", beT[:, :, :], kka[:, :, :], eneg[:, :, :], ALU.mult)
            k.tt("pool", eexc[:, :, :], kka[:, :, :], erem[:, :, :], ALU.mult)
            k.tt("dve", ktT[:, :, :], kT[:, :, :], eneg[:, :, :], ALU.mult)
            k.tt("dve", erem[:, :, :], kT[:, :, :], erem[:, :, :], ALU.mult)
            k.tt("dve", rbT[:, :, :], rT[:, :, :], epos[:, :, :], ALU.mult)
            n_ = 0
            for src, dst in ((alT, Atok), (eexc, Bhat), (erem, Khat)):
                for hf in range(2):
                    ps = pT.next()
                    for c4 in range(4):
                        k.tr(ps[:, c4 * P:(c4 + 1) * P], src[:, hf * 4 + c4, :], self.ident_f[:, :])
                    k.copy("act" if n_ % 2 == 0 else "dve", dst[:, hf * 512:(hf + 1) * 512], ps[:, :])
                    n_ += 1
            k.stop(1)
            handoff([rT, kT, aT, kk, scr, eneg, eexc, erem], WB)
            k.tt("pool", glD[:, :, :], View(sid.h[:, :].unsqueeze(1).to_broadcast([P, 16, 64]), sid.buf),
                 bc3(gl, gl.h[:, :], 64), ALU.mult)
            k.stop(2)
            q4 = lambda v_: v_.h[:, :].rearrange("p (q t) -> p q t", q=4)
            names5 = ("A0", "AT0", "AakT", "ArbT", "ArkT")
            gsets = [{n_: WB[i_] for i_, n_ in zip((0, 1, 2, 3, 4), names5)},
                     {n_: WB[i_] for i_, n_ in zip((7, 12, 13, 14, 15), names5)}]
            src = {"al": alT, "be": beT, "kt": ktT, "rb": rbT}

            def p1_units(g_):
                Gs = gsets[g_ % 2]
                units = []
                for (l_, r_, msk, dst) in (("al", "be", m_ts, "A0"), ("be", "al", mT_s, "AT0"), ("kt", "al", mT_s, "AakT"),
                                           ("be", "rb", mT_i, "ArbT"), ("kt", "rb", mT_i, "ArkT")):
                    for pi in range(2):
                        def unit(l_=l_, r_=r_, msk=msk, dst=dst, pi=pi):
                            ps = pG.next()
                            hs = slice(pi * 64, pi * 64 + 64)
                            for cl in range(2):
                                c = 2 * g_ + cl
                                k.mm(ps[:, cl * P:(cl + 1) * P], src[l_][hs, c, :], src[r_][hs, c, :])
                            k.tt("dve", Gs[dst][:, 2 * pi:2 * pi + 2, :], ps.v(ps.h[:, 0:256].rearrange("p (q t) -> p q t", q=2)),
                                 View(msk.h[:, :].unsqueeze(1).to_broadcast([P, 2, P]), msk.buf), ALU.mult)
                        units.append(unit)
                return units

            pending = [None]
            for g in range(4):
                heads = [(2 * (2 * g + cl) + pi, 2 * g + cl, pi * 64) for pi in range(2) for cl in range(2)]
                G_ = gsets[g % 2]
                if g == 0:
                    for u_ in p1_units(0):
                        u_()
                k.stop(3)
                X = WB[5 + (xcnt[0] % 2)]
                xcnt[0] += 1
                for pi in range(2):
                    k.copy("pool", X.v(X.h[:, 2 * pi:2 * pi + 2, 0:64]),
                           Atok.v(Atok.h[:, g * 256:(g + 1) * 256].rearrange("p (cl pi d) -> p pi cl d", pi=2, cl=2)[:, pi]))
                ps = pG.next()
                for q, (h, c, po) in enumerate(heads):
                    k.mm(ps[:, q * 64:(q + 1) * 64], G_["AakT"][:, q, :], vtok[:, h * 64:(h + 1) * 64])
                k.copy("act", X.v(X.h[:, :, 64:128]), ps.v(ps.h[:, 0:256].rearrange("p (q d) -> p q d", q=4)))
                A, AT = G_["A0"], G_["AT0"]
                nxt_units = p1_units(g + 1) if g + 1 < 4 else []
                for lvl in range(6):
                    ps = pG.next()
                    for q in range(4):
                        k.mm(ps[:, q * P:(q + 1) * P], AT[:, q, :], X[:, q, :])
                    Xn = WB[5 + (xcnt[0] % 2)]
                    xcnt[0] += 1
                    k.tt("dve", Xn[:, :, :], ps.v(q4(ps)), X[:, :, :], ALU.add)
                    X = Xn
                    if lvl < 5:
                        ps2 = pG.next()
                        for q in range(4):
                            k.mm(ps2[:, q * P:(q + 1) * P], A[:, q, :], AT[:, q, :])
                        A2T = WB[8 + (acnt[0] % 4)]
                        acnt[0] += 1
                        k.copy("act", A2T[:, :, :], ps2.v(q4(ps2)))
                        A2 = None
                        if lvl < 4:
                            ps3 = pG.next()
                            for q in range(4):
                                k.mm(ps3[:, q * P:(q + 1) * P], AT[:, q, :], A[:, q, :])
                            A2 = WB[8 + (acnt[0] % 4)]
                            acnt[0] += 1
                            k.copy("act", A2[:, :, :], ps3.v(q4(ps3)))
                        A, AT = A2, A2T
                    for u_ in nxt_units[2 * lvl:2 * lvl + 2]:
                        u_()
                    if lvl == 0 and pending[0] is not None:
                        pending[0]()
                        pending[0] = None
                k.stop(4)
                ps = pG.next()
                for q, (h, c, po) in enumerate(heads):
                    k.mm(ps[:, q * 64:(q + 1) * 64], G_["ArkT"][:, q, :], vtok[:, h * 64:(h + 1) * 64], start=True, stop=False)
                    k.mm(ps[:, q * 64:(q + 1) * 64], G_["ArbT"][:, q, :], X[:, q, 64:128], start=False, stop=True)
                k.copy("act", Yl[:, :, :], ps.v(ps.h[:, 0:256].rearrange("p (q d) -> p q d", q=4)))
                ps = pG.next()
                for q, (h, c, po) in enumerate(heads):
                    hs = slice(po, po + 64)
                    cl = q % 2
                    k.mm(ps[hs, cl * P:(cl + 1) * P], X[:, q, 0:64], G_["ArbT"][:, q, :], r=False)
                k.tt("dve", Qe[:, :, :], ps.v(ps.h[:, 0:256].rearrange("p (c t) -> p c t", c=2)), rbT[:, 2 * g:2 * g + 2, :], ALU.add)
                for cc in range(2):
                    cs = slice(cc * 64, cc * 64 + 64)
                    psM = pG.next()
                    psN = pG.next()
                    for q, (h, c, po) in enumerate(heads):
                        hs = slice(po, po + 64)
                        hc = slice(h * 64, h * 64 + 64)
                        cl = q % 2
                        k.mm(psM[hs, cl * 64:(cl + 1) * 64], X[cs, q, 0:64], Bhat[cs, hc], r=False)
                        k.mm(psN[hs, cl * 64:(cl + 1) * 64], Bhat[cs, hc], X[cs, q, 64:128], start=True, stop=False, r=False)
                        k.mm(psN[hs, cl * 64:(cl + 1) * 64], Khat[cs, hc], vtok[cs, hc], start=False, stop=True, r=False)
                    gd = glD.v(glD.h[:, 4 * g:4 * g + 4, :].rearrange("p (cl cc) d -> p cl cc d", cc=2)[:, :, cc, :])
                    k.tt("dve", MT.v(MT.h[:, :, :].rearrange("p (cl cc) d -> p cl cc d", cc=2)[:, :, cc, :]),
                         psM.v(psM.h[:, 0:128].rearrange("p (c d) -> p c d", c=2)), gd, ALU.add)
                    k.copy("act", Nn.v(Nn.h[:, :, :].rearrange("p (cl cc) d -> p cl cc d", cc=2)[:, :, cc, :]),
                           psN.v(psN.h[:, 0:128].rearrange("p (c d) -> p c d", c=2)))
                k.stop(5)

                def chain_fn(g=g, heads=heads):
                    Sg = St.sub(g)
                    psy = [pG.next(), pG.next()]
                    for cc in range(2):
                        cs = slice(cc * 64, cc * 64 + 64)
                        psS = [pG.next(), pG.next()]
                        for q, (h, c, po) in enumerate(heads):
                            hs = slice(po, po + 64)
                            pi, cl = q // 2, q % 2
                            k.mm(psy[pi][cs, cl * 64:(cl + 1) * 64], Qe[hs, cl, cs], Sg[hs, c, :])
                            k.mm(psS[pi][hs, cl * 64:(cl + 1) * 64], MT[hs, cl * 2 + cc, :], Sg[hs, c, :])
                        for pi in range(2):
                            hs = slice(pi * 64, pi * 64 + 64)
                            k.tt("dve", Sg[hs, 2 * g:2 * g + 2, :], psS[pi].v(psS[pi].h[hs, 0:128].rearrange("p (c d) -> p c d", c=2)),
                                 Nn.v(Nn.h[hs, :, :].rearrange("p (cl cc) d -> p cl cc d", cc=2)[:, :, cc, :]), ALU.add)
                    for pi in range(2):
                        k.tt("dve", ytok.v(ytok.h[:, g * 256:(g + 1) * 256].rearrange("p (cl pi d) -> p pi cl d", pi=2, cl=2)[:, pi]),
                             psy[pi].v(psy[pi].h[:, 0:128].rearrange("p (c d) -> p c d", c=2)), Yl[:, 2 * pi:2 * pi + 2, :], ALU.add)

                if g == 3:
                    chain_fn()
                else:
                    pending[0] = chain_fn
            k.stop(6)
            handoff(WB, [rT, kT, aT, kk, scr, eneg, eexc, erem])
            y3 = lambda t_: t_.v(t_.h[:, :].rearrange("p (h d) -> p h d", d=64))
            sA, sB, sM, sR, ysq = gnt
            k.reduce(sA[:, :], y3(ytok), ALU.add)
            k.tt("pool", ysq[:, :], ytok[:, :], ytok[:, :], ALU.mult)
            k.reduce(sB[:, :], y3(ysq), ALU.add)
            k.ts("dve", sM[:, :], sA[:, :], 1.0 / 64, ALU.mult)
            k.tt("dve", sA[:, :], sM[:, :], sM[:, :], ALU.mult)
            k.stt(sB[:, :], sB[:, :], 1.0 / 64, sA[:, :], ALU.mult, ALU.subtract)
            k.act(sR[:, :], sB[:, :], AF.Ln, bias=gneps[:, 0:1])
            k.act(sR[:, :], sR[:, :], AF.Exp, scale=-0.5)
            k.tt("dve", y3(ytok), y3(ytok), bc3(sM, sM.h[:, :], 64), ALU.subtract)
            k.tt("dve", y3(ytok), y3(ytok), bc3(sR, sR.h[:, :], 64), ALU.mult)
            k.tt("pool", ytok[:, :], ytok[:, :], lngb[:, :], ALU.mult)
            k.tt("pool", ytok[:, :], ytok[:, :], lnbb[:, :], ALU.add)
            k.tt("dve", y3(ysq), y3(vtok), bc3(bonus, bonus.h[:, :], 64), ALU.mult)
            k.tt("pool", ytok[:, :], ytok[:, :], ysq[:, :], ALU.add)
            k.tt("dve", zb[:, :], ytok[:, :], gate[:, :], ALU.mult)
            for c in range(8):
                k.tr(tps[:, c, :], zb[:, c * P:(c + 1) * P], self.ident_b[:, :])
            k.copy("dve", zT[:, :, :], tps[:, :, :])
            dps = [pT.next(), pT.next()]
            for hf in range(2):
                for c in range(8):
                    k.mm(dps[hf][:, :], zT[:, c, :], wo[:, c, hf * 512:(hf + 1) * 512], start=(c == 0), stop=(c == 7))
            self.post_norm_add(dps, ht, g1b, hdst, ti, sss.next(), junk, ysq)
        k.dead = False
        self.barrier()


Prog.rwkv = _rwkv


def U(ap):
    return View(ap, Buf())


def _even(self, layer, hsrc, hdst):
    k = self.k
    nc = self.nc
    e = layer // 2
    S, NT = self.S, self.NT
    RS8 = 1.0 / 8.0
    RSD = 1.0 / (128.0 ** 0.5)
    if not hasattr(self, "dQT"):
        self.dQT = nc.dram_tensor("dQT", [P, 4, S], BF16).ap()
        self.dKT = nc.dram_tensor("dKT", [P, 4, S], BF16).ap()
        self.dMQK = nc.dram_tensor("dMQK", [P, 8, S], BF16).ap()
        self.dV = nc.dram_tensor("dV", [S, 512], BF16).ap()
        self.dMV = nc.dram_tensor("dMV", [S, 512], BF16).ap()
        self.dOG = nc.dram_tensor("dOG", [S, 512], BF16).ap()
        self.dAH = nc.dram_tensor("dAH", [P, 8, S], BF16).ap()
    dQT, dKT, dMQK, dV, dMV, dOG, dAH = self.dQT, self.dKT, self.dMQK, self.dV, self.dMV, self.dOG, self.dAH
    with ExitStack() as esg:
        ECt = k.sb([P, NT, 4], F32, "ECt", esg)
        EC2t = k.sb([P, NT, 4], F32, "EC2t", esg)
        THRt = k.sb([P, NT, 4], F32, "THRt", esg)
        wstB = k.sb([P, 4, NT], F32, "wstB", esg)
        esr = ExitStack()
        R1 = k.sb([4, S], F32, "R1", esr)
        R2 = k.sb([4, S], F32, "R2", esr)
        R3 = k.sb([4, S], F32, "R3", esr)
        with ExitStack() as es:
            g0 = k.sb([P, 8], F32, "g0", es)
            self.fm_param(g0[:, :], self.norm_g.h[layer, 0, :])
            win = k.sb([P, 8, IN_COLS], BF16, "win", es)
            with ExitStack() as es2:
                stage = Ring(k, 2, [P, 2048], F32, "stg", es2)
                self.load_w(lambda kd, c0, cw: win[:, kd, c0:c0 + cw],
                            lambda kd, c0, cw: self.e_w_in[e, kd * P:(kd + 1) * P, c0:c0 + cw], 8, IN_COLS, stage, g0)
                self.barrier()
            cw_ = k.sb([P, 8, 4], F32, "cw", es)
            for j in range(4):
                self.fm_param(cw_[:, :, j], self.e_conv_w.h[e, j, :])
            bi = k.sb([4, 1], F32, "bi", es)
            nbf = k.sb([4, 1], F32, "nbf", es)
            with nc.allow_non_contiguous_dma(reason="tiny"):
                k.dma("sp", bi[:, :], U(self.e_b_if.h[e, 0:4].rearrange("(p o) -> p o", o=1)))
                k.dma("sp", nbf[:, :], U(self.e_b_if.h[e, 4:8].rearrange("(p o) -> p o", o=1)))
            k.ts("dve", nbf[:, :], nbf[:, :], -1.0, ALU.mult)
            hts1 = Ring(k, 2, [P, D], F32, "ht", es)
            hns1 = Ring(k, 2, [P, D], BF16, "hn", es)
            junk = k.sb([P, D], BF16, "junk", es)
            sss = Ring(k, 4, [P, 4], F32, "ss", es)
            uTs = Ring(k, 2, [P, 8, P], BF16, "uT", es)
            cq = k.sb([P, 8, P + 3], F32, "cq", es)
            k.memset("pool", cq[:, :, 0:3], 0.0)
            acc = k.sb([P, 8, P], F32, "acc", es)
            qk_o = Ring(k, 2, [P, 8, P], BF16, "qko", es)
            sq_o = Ring(k, 2, [P, 4, P], BF16, "sqo", es)
            sk_o = Ring(k, 2, [P, 4, P], BF16, "sko", es)
            tok_o = Ring(k, 3, [P, 512], BF16, "toko", es)
            gtmp = k.sb([4, P], F32, "gtmp", es)
            tps = k.ps([P, 8, P], BF16, "tps", es)
            pA = PSlots(k, 3, es, "pA")
            pT = Ring(k, 2, [P, 512], F32, "pT", es, psum=True)
            s4 = sss.next()
            hn_n = hns1.next()
            self.norm_part(hsrc, 0, hts1.next(), hn_n, s4.sub(0), s4.sub(1), junk)
            uT_n = uTs.next()
            self.tr_part(hn_n, tps, uT_n[:, :, :])
            for ti in range(NT):
                ts_ = slice(ti * P, (ti + 1) * P)
                uT = uT_n
                sq, sk, qk = sq_o.next(), sk_o.next(), qk_o.next()
                for n_ in range(4):
                    ps = pA.next()
                    for kd in range(8):
                        k.mm(ps[:, :], win[:, kd, n_ * P:(n_ + 1) * P], uT[:, kd, :], start=(kd == 0), stop=(kd == 7))
                    k.copy("act", sq[:, n_, :], ps[:, :])
                for n_ in range(4):
                    ps = pA.next()
                    for kd in range(8):
                        k.mm(ps[:, :], win[:, kd, 512 + n_ * P:512 + (n_ + 1) * P], uT[:, kd, :], start=(kd == 0), stop=(kd == 7))
                    k.act(sk[:, n_, :], ps[:, :], AF.Copy, scale=RS8)
                k.dma("pool", U(dQT[:, :, ts_]), sq[:, :, :])
                k.dma("pool", U(dKT[:, :, ts_]), sk[:, :, :])
                if ti + 1 < NT:
                    s4 = sss.next()
                    hn_n = hns1.next()
                    self.norm_part(hsrc, ti + 1, hts1.next(), hn_n, s4.sub(0), s4.sub(1), junk)
                for n_ in range(8):
                    ps = pA.next()
                    for kd in range(8):
                        k.mm(ps[:, :], win[:, kd, 1536 + n_ * P:1536 + (n_ + 1) * P], uT[:, kd, :], start=(kd == 0), stop=(kd == 7))
                    k.copy("act", cq[:, n_, 3:P + 3], ps[:, :])
                for n_ in range(8):
                    k.ts("dve", acc[:, n_, :], cq[:, n_, 0:P], cw_[:, n_, 0:1], ALU.mult)
                    for j in range(1, 4):
                        k.stt(acc[:, n_, :], cq[:, n_, j:j + P], cw_[:, n_, j:j + 1], acc[:, n_, :], ALU.mult, ALU.add)
                k.act(qk[:, :, :], acc[:, :, :], AF.Silu)
                k.copy("pool", acc[:, :, 0:3], cq[:, :, P:P + 3])
                k.copy("pool", cq[:, :, 0:3], acc[:, :, 0:3])
                k.dma("pool", U(dMQK[:, :, ts_]), qk[:, :, :])
                for col0, dd, fn in ((1024, dV, None), (2560, dMV, None), (3072, dOG, AF.Sigmoid)):
                    ps = pT.next()
                    for kd in range(8):
                        k.mm(ps[:, :], uT[:, kd, :], win[:, kd, col0:col0 + 512], start=(kd == 0), stop=(kd == 7))
                    to = tok_o.next()
                    if fn is None:
                        k.copy("dve", to[:, :], ps[:, :])
                    else:
                        k.act(to[:, :], ps[:, :], fn)
                    k.dma("pool", U(dd[ts_, :]), to[:, :])
                ps = pA.next()
                for kd in range(8):
                    k.mm(ps[0:4, :], win[:, kd, 3584:3588], uT[:, kd, :], start=(kd == 0), stop=(kd == 7))
                k.act(R1[:, ts_], ps[0:4, :], AF.Identity, bias=bi[:, 0:1])
                ps = pA.next()
                for kd in range(8):
                    k.mm(ps[0:4, :], win[:, kd, 3588:3592], uT[:, kd, :], start=(kd == 0), stop=(kd == 7))
                k.act(gtmp[:, :], ps[0:4, :], AF.Exp, bias=nbf[:, 0:1], scale=-1.0)
                k.act(R2[:, ts_], gtmp[:, :], AF.Ln, bias=1.0)
                if ti + 1 < NT:
                    uT_n = uTs.next()
                    self.tr_part(hn_n, tps, uT_n[:, :, :])
            self.barrier()
        if EVSTOP == 1:
            k.dead = True
        with ExitStack() as es:
            k.scan(R3[:, :], R2[:, :], R2[:, :], 0.0, ALU.add, ALU.max)
            k.tt("dve", R1[:, :], R1[:, :], R3[:, :], ALU.add)
            k.scan(R2[:, :], R1[:, :], R1[:, :], 0.0, ALU.max, ALU.max)
            ge = k.sb([4, NT], F32, "ge", es)
            rc = k.sb([4, NT], F32, "rc", es)
            wst = k.sb([4, NT], F32, "wst", es)
            g3 = R2.h[:, :].rearrange("p (c t) -> p c t", t=P)
            k.copy("dve", ge.v(ge.h[:, :].unsqueeze(2)), View(g3[:, :, P - 1:P], R2.buf))
            k.memset("dve", rc[:, :], 0.0)
            if NT > 1:
                k.copy("dve", rc[:, 1:NT], ge[:, 0:NT - 1])
            k.tt("dve", wst[:, :], rc[:, :], ge[:, :], ALU.subtract)
            k.act(wst[:, :], wst[:, :], AF.Exp)
            rcb = lambda: View(rc.h[:, :].unsqueeze(2).to_broadcast([4, NT, P]), rc.buf)
            r13 = R1.v(R1.h[:, :].rearrange("p (c t) -> p c t", t=P))
            r33 = R3.v(R3.h[:, :].rearrange("p (c t) -> p c t", t=P))
            k.tt("dve", r13, r13, rcb(), ALU.subtract)
            k.act(R1[:, :], R1[:, :], AF.Exp)
            k.tt("dve", r33, r33, rcb(), ALU.subtract)
            k.act(R3[:, :], R3[:, :], AF.Exp)
            pt1 = k.ps([P, 512], F32, "pt1", es)
            pt2 = k.ps([P, 512], F32, "pt2", es)
            pt3 = k.ps([P, 512], F32, "pt3", es)
            for c in range(NT):
                k.tr(pt1[:, c * 4:(c + 1) * 4], R1[0:4, c * P:(c + 1) * P], self.ident_f[0:4, 0:4])
            k.copy("dve", ECt.v(ECt.h[:, :, :].rearrange("p c h -> p (c h)")), pt1[:, 0:NT * 4])
            for c in range(NT):
                k.tr(pt2[:, c * 4:(c + 1) * 4], R3[0:4, c * P:(c + 1) * P], self.ident_f[0:4, 0:4])
            k.copy("dve", THRt.v(THRt.h[:, :, :].rearrange("p c h -> p (c h)")), pt2[:, 0:NT * 4])
            sel = k.sb([4, 4, P], F32, "sel", es)
            k.memset("pool", sel[:, :, :], 1.0)
            k.affsel(sel[:, :, :], sel[:, :, :], [[-1, 4], [0, P]], ALU.is_equal, 0.0, 0, 1)
            for h in range(4):
                k.mm(pt3[:, h * NT:(h + 1) * NT], sel[:, h, :], wst[:, :])
            k.copy("dve", wstB.v(wstB.h[:, :, :].rearrange("p h c -> p (h c)")), pt3[:, 0:4 * NT])
            k.tt("dve", EC2t[:, :, :], ECt[:, :, :], wstB.v(wstB.h[:, :, :].rearrange("p h c -> p c h")), ALU.mult)
            self.barrier()
        esr.close()
        with ExitStack() as es:
            QW = min(512, S)
            QT = k.sb([P, 4, S], BF16, "QT", es)
            KT = k.sb([P, 4, S], BF16, "KT", es)
            Vt = k.sb([P, NT, 512], BF16, "Vt", es)
            AO = k.sb([P, 4, S], BF16, "AO", es)
            k.dma("sp", QT[:, :, :], U(dQT[:, :, :]))
            k.dma("sp", KT[:, :, :], U(dKT[:, :, :]))
            k.dma("sp", Vt[:, :, :], U(dV.rearrange("(n p) d -> p n d", p=P)))
            ntri = k.sb([P, P], F32, "ntri", es)
            nones = k.sb([P, P], F32, "nones", es)
            ntri.buf.f32r = True
            nones.buf.f32r = True
            ctmp = k.sb([P, P], F32, "ctmp", es)
            k.memset("pool", ctmp[:, :], -1.0)
            k.copy("dve", nones[:, :], ctmp[:, :])
            k.affsel(ctmp[:, :], ctmp[:, :], [[-1, P]], ALU.is_ge, 0.0, 0, 1)
            k.copy("dve", ntri[:, :], ctmp[:, :])
            nd = QW // P
            dm = k.sb([P, nd, QW], F32, "dm", es)
            dmb = k.sb([P, nd, QW], BF16, "dmb", es)
            k.memset("pool", dm[:, :, :], 1.0)
            for jj in range(nd):
                k.affsel(dm[:, jj, :], dm[:, jj, :], [[1, QW]], ALU.is_gt, 0.0, -P * jj, -1)
            k.copy("dve", dmb[:, :, :], dm[:, :, :])
            er = [Ring(k, 2, [P, QW], F32, "e", es) for _ in range(2)]
            spr = [Ring(k, 4, [P, QW], F32, "sp", es) for _ in range(2)]
            rsr = [Ring(k, 3, [P, QW], F32, "rs", es) for _ in range(2)]
            wr_ = [Ring(k, 3, [P, QW], BF16, "w", es) for _ in range(2)]
            for par in range(2):
                for t_ in spr[par].t + rsr[par].t:
                    t_.buf.f32r = True
            pz = [Ring(k, 1, [P, 512], F32, "pz", es, psum=True) for _ in range(2)]
            pd = [Ring(k, 1, [P, 512], F32, "pd", es, psum=True) for _ in range(2)]
            pav = [Ring(k, 1, [P, 512], F32, "pav", es, psum=True) for _ in range(2)]
            for tq in range(S // QW):
                t0 = tq * QW
                Jtop = (t0 + QW) // P - 1
                for hp in range(4):
                    c = hp
                    RSs = [None, None]
                    avs = [pav[0].next(), pav[1].next()]
                    hsl = [slice(par * 64, par * 64 + 64) for par in range(2)]
                    qvs = [QT[hsl[par], c, t0:t0 + QW] for par in range(2)]

                    def front(J):
                        s0 = J * P
                        diag = (s0 + P - 1 >= t0)
                        jj = (s0 - t0) // P if diag else None
                        kvs = [KT[hsl[par], c, s0:s0 + P] for par in range(2)]
                        zts, es_, sps = [], [], []
                        for par in range(2):
                            zt = pz[par].next()
                            k.mm(zt[:, 0:QW], kvs[par], qvs[par])
                            zts.append(zt)
                        for par in range(2):
                            e_ = er[par].next()
                            k.act(e_[:, :], zts[par][:, 0:QW], AF.Exp)
                            es_.append(e_)
                        for par in range(2):
                            sp = spr[par].next()
                            k.act(sp[:, :], es_[par][:, :], AF.Ln, bias=1.0)
                            sps.append(sp)
                        if diag:
                            for par in range(2):
                                k.tt("dve", sps[par][:, :], sps[par][:, :], dm[:, jj, :], ALU.mult)
                        return (J, diag, jj, kvs, sps)

                    def back1(fr):
                        J, diag, jj, kvs, sps = fr
                        ds, ws = [], []
                        for par in range(2):
                            RS = RSs[par]
                            d_ = pd[par].next()
                            k.mm(d_[:, 0:QW], kvs[par], qvs[par], start=True, stop=False)
                            k.mm(d_[:, 0:QW], ntri[:, :], sps[par][:, :], start=False, stop=(RS is None))
                            if RS is not None:
                                k.mm(d_[:, 0:QW], nones[:, :], RS[:, :], start=False, stop=True)
                            ds.append(d_)
                        for par in range(2):
                            w_ = wr_[par].next()
                            k.act(w_[:, :], ds[par][:, 0:QW], AF.Exp)
                            ws.append(w_)
                        if diag:
                            for par in range(2):
                                k.tt("dve", ws[par][:, :], ws[par][:, :], dmb[:, jj, :], ALU.mult)
                        if J > 0:
                            for par in range(2):
                                if RSs[par] is None:
                                    RSs[par] = sps[par]
                                else:
                                    RSn = rsr[par].next()
                                    k.tt("dve", RSn[:, :], RSs[par][:, :], sps[par][:, :], ALU.add)
                                    RSs[par] = RSn
                        return (J, ws)

                    def back2(pend):
                        J, ws = pend
                        for par in range(2):
                            h = 2 * hp + par
                            k.mm(avs[par][hsl[par], 0:QW], Vt[:, J, h * 64:(h + 1) * 64], ws[par][:, :],
                                 start=(J == Jtop), stop=(J == 0))

                    fr = front(Jtop)
                    pend = None
                    for J in range(Jtop, -1, -1):
                        nxt = front(J - 1) if J > 0 else None
                        cur = back1(fr)
                        if pend is not None:
                            back2(pend)
                        pend = cur
                        fr = nxt
                    back2(pend)
                    for par in range(2):
                        k.copy("dve", AO[hsl[par], c, t0:t0 + QW], avs[par][hsl[par], 0:QW])
            k.dma("sp", U(dAH[:, 0:4, :]), AO[:, :, :])
            self.barrier()
        if EVSTOP == 3:
            k.dead = True
        with ExitStack() as es:
            MQK = k.sb([P, 8, S], BF16, "MQK", es)
            MV = k.sb([P, NT, 512], BF16, "MV", es)
            OG = k.sb([P, NT, 512], BF16, "OG", es)
            k.dma("sp", MQK[:, :, :], U(dMQK[:, :, :]))
            k.dma("sp", MV[:, :, :], U(dMV.rearrange("(n p) d -> p n d", p=P)))
            k.dma("sp", OG[:, :, :], U(dOG.rearrange("(n p) d -> p n d", p=P)))
            hgb = k.sb([P, 512], F32, "hgb", es)
            k.dma("sp", hgb[:, :], U(self.e_head_g.h[e, :, :].rearrange("h d -> (h d)").partition_broadcast(P)))
            mlm = k.sb([P, P], F32, "mlm", es)
            k.memset("pool", mlm[:, :], RSD)
            k.affsel(mlm[:, :], mlm[:, :], [[1, P]], ALU.is_ge, 0.0, 0, -1)
            Cst = [k.sb([P, 132], F32, "Cst%d" % h, es) for h in range(4)]
            Cbf = [k.sb([P, 132], BF16, "Cbf%d" % h, es) for h in range(4)]
            for h in range(4):
                k.memset("pool", Cst[h][:, :], 0.0)
                k.memset("pool", Cbf[h][:, :], 0.0)
            va1 = Ring(k, 4, [P, 132], BF16, "va1", es)
            va2 = Ring(k, 4, [P, 132], BF16, "va2", es)
            pmr = Ring(k, 4, [P, P], BF16, "pm", es)
            ktr = Ring(k, 2, [P, 2, P], BF16, "kt", es)
            sm = Ring(k, 8, [P, 8], F32, "sm", es)
            tmpf = Ring(k, 4, [P, P], F32, "tmpf", es)
            junk = k.sb([P, 2, P], BF16, "junk", es)
            Htk = Ring(k, 2, [P, 512], BF16, "Htk", es)
            HTs = Ring(k, 2, [P, 4, P], BF16, "HTs", es)
            pst = Ring(k, 2, [P, 512], F32, "pst", es, psum=True)
            px = Ring(k, 2, [P, 512], F32, "px", es, psum=True)
            pu = Ring(k, 2, [P, 512], F32, "pu", es, psum=True)
            pkt = Ring(k, 1, [P, 8, P], BF16, "pkt", es, psum=True)
            pht = Ring(k, 1, [P, 8, P], BF16, "pht", es, psum=True)
            for c in range(NT):
                cs = slice(c * P, (c + 1) * P)
                Ht = Htk.next()
                for hp2 in range(2):
                    hh = [2 * hp2, 2 * hp2 + 1]
                    hcs = [slice(h * P, (h + 1) * P) for h in hh]
                    qTs = [MQK[:, h, cs] for h in hh]
                    kTs = [MQK[:, 4 + h, cs] for h in hh]
                    v1s, v2s = [], []
                    for i, h in enumerate(hh):
                        v1, v2 = va1.next(), va2.next()
                        k.ts("pool", v1[:, 0:P], MV[:, c, hcs[i]], ECt[:, c, h:h + 1], ALU.mult)
                        k.copy("pool", v1[:, P:P + 1], ECt[:, c, h:h + 1])
                        k.ts("pool", v2[:, 0:P], MV[:, c, hcs[i]], EC2t[:, c, h:h + 1], ALU.mult)
                        k.copy("pool", v2[:, P:P + 1], EC2t[:, c, h:h + 1])
                        v1s.append(v1)
                        v2s.append(v2)
                    sts = []
                    for i in range(2):
                        st_ = pst.next()
                        k.mm(st_[:, 0:P], kTs[i], qTs[i])
                        sts.append(st_)
                    kp_ = pkt.next()
                    for i in range(2):
                        k.tr(kp_[:, i, :], kTs[i], self.ident_b[:, :])
                    pms = []
                    for i in range(2):
                        pm = pmr.next()
                        k.tt("dve", pm[:, :], sts[i][:, 0:P], mlm[:, :], ALU.mult)
                        pms.append(pm)
                    kt_ = ktr.next()
                    k.copy("act", kt_[:, :, :], kp_[:, 0:2, :])
                    xs, us = [], []
                    for i, h in enumerate(hh):
                        x_ = px.next()
                        k.mm(x_[:, 0:P + 1], pms[i][:, :], v1s[i][:, 0:P + 1], start=True, stop=False)
                        k.mm(x_[:, 0:P + 1], qTs[i], Cbf[h][:, 0:P + 1], start=False, stop=True)
                        xs.append(x_)
                    for i in range(2):
                        u_ = pu.next()
                        k.mm(u_[:, 0:P + 1], kt_[:, i, :], v2s[i][:, 0:P + 1])
                        us.append(u_)
                    for i, h in enumerate(hh):
                        k.stt(Cst[h][:, 0:P + 1], Cst[h][:, 0:P + 1], wstB[:, h, c:c + 1], us[i][:, 0:P + 1], ALU.mult, ALU.add)
                    for i, h in enumerate(hh):
                        k.ts("pool", Cbf[h][:, 0:P + 1], Cst[h][:, 0:P + 1], RSD, ALU.mult)
                    ss_ = [sm.next(), sm.next()]
                    for i in range(2):
                        k.ts("dve", ss_[i][:, 5:6], xs[i][:, P:P + 1], -1.0, ALU.mult)
                    for i in range(2):
                        k.tt("dve", ss_[i][:, 0:1], xs[i][:, P:P + 1], ss_[i][:, 5:6], ALU.max)
                    for i, h in enumerate(hh):
                        k.ts("dve", ss_[i][:, 0:1], ss_[i][:, 0:1], THRt[:, c, h:h + 1], ALU.max)
                    for i in range(2):
                        k.recip(ss_[i][:, 1:2], ss_[i][:, 0:1])
                    for i in range(2):
                        k.act(View(junk.h[:, i, :], Buf()), xs[i][:, 0:P], AF.Square, scale=ss_[i][:, 1:2], accum=ss_[i][:, 2:3])
                    for i in range(2):
                        k.act(ss_[i][:, 3:4], ss_[i][:, 2:3], AF.Ln, bias=self.eps_t[:, 0:1], scale=1.0 / P)
                    for i in range(2):
                        k.act(ss_[i][:, 3:4], ss_[i][:, 3:4], AF.Exp, scale=-0.5)
                    for i in range(2):
                        k.tt("dve", ss_[i][:, 4:5], ss_[i][:, 3:4], ss_[i][:, 1:2], ALU.mult)
                    tfs = []
                    for i in range(2):
                        tf = tmpf.next()
                        k.stt(tf[:, :], xs[i][:, 0:P], ss_[i][:, 4:5], hgb[:, hcs[i]], ALU.mult, ALU.mult)
                        tfs.append(tf)
                    for i in range(2):
                        k.tt("pool", Ht[:, hcs[i]], tfs[i][:, :], OG[:, c, hcs[i]], ALU.mult)
                hp = pht.next()
                for h in range(4):
                    k.tr(hp[:, h, :], Ht[:, h * P:(h + 1) * P], self.ident_b[:, :])
                HT = HTs.next()
                k.copy("act", HT[:, :, :], hp[:, 0:4, :])
                k.dma("pool", U(dAH[:, 4:8, cs]), HT[:, :, :])
            self.barrier()
        if EVSTOP == 4:
            k.dead = True
        with ExitStack() as es:
            wo = k.sb([P, 8, D], BF16, "wo", es)
            with ExitStack() as es2:
                stage = Ring(k, 2, [P, 2048], F32, "stg", es2)
                self.load_w(lambda kd, c0, cw: wo[:, kd, c0:c0 + cw],
                            lambda kd, c0, cw: self.e_w_out[e, kd * P:(kd + 1) * P, c0:c0 + cw], 8, D, stage, None)
                self.barrier()
            g1b = k.sb([P, D], F32, "g1b", es)
            k.dma("sp", g1b[:, :], U(self.norm_g.h[layer, 1, :].partition_broadcast(P)))
            ahs = Ring(k, 2, [P, 8, P], BF16, "ah", es)
            hts = Ring(k, 2, [P, D], F32, "ht", es)
            yts = Ring(k, 2, [P, D], F32, "yt", es)
            junk = k.sb([P, D], BF16, "junk", es)
            sss = Ring(k, 4, [P, 4], F32, "ss", es)
            dpsr = Ring(k, 4, [P, 512], F32, "dps", es, psum=True)
            for ti in range(NT):
                ts_ = slice(ti * P, (ti + 1) * P)
                ah = ahs.next()
                k.dma("sp", ah[:, :, :], U(dAH[:, :, ts_]))
                ht = hts.next()
                k.dma("sp", ht[:, :], hsrc.sub(ti)[ts_, :])
                dps = [dpsr.next(), dpsr.next()]
                for hf in range(2):
                    for c in range(8):
                        k.mm(dps[hf][:, :], ah[:, c, :], wo[:, c, hf * 512:(hf + 1) * 512], start=(c == 0), stop=(c == 7))
                self.post_norm_add(dps, ht, g1b, hdst, ti, sss.next(), junk, yts.next())
            k.dead = False
            self.barrier()


Prog.even = _even


def build_full(S=SEQ, depth=DEPTH):
    prog = Prog(S=S)
    prog.declare()
    prog.consts()
    cur = prog.x
    bufs = [prog.hA, prog.hB]
    bi = 0
    for layer in range(depth):
        last = (layer == depth - 1)
        mid = bufs[bi]
        bi ^= 1
        if layer % 2 == 0:
            prog.even(layer, cur, mid)
        else:
            prog.rwkv(layer, cur, mid)
        prog.barrier()
        dst = prog.out if last else bufs[bi]
        bi ^= 1
        prog.mlp(layer, mid, dst)
        prog.barrier()
        cur = dst
    prog.finish(prog.out)
    return prog


def kernel(**inputs):
    inputs = {k_: np.asarray(v) for k_, v in inputs.items()}
    prog = build_full()
    in_maps = [prog.in_map(inputs, b) for b in range(BATCH)]
    res = run_bass_kernel_spmd(prog.nc, in_maps, core_ids=list(range(BATCH)))
    return np.stack([np.asarray(res.results[b]["out"]) for b in range(BATCH)], axis=0).astype(np.float32)
```

```python
import numpy as np
from contextlib import ExitStack
import concourse.bass as bass
import concourse.mybir as mybir
from concourse.bass_utils import run_bass_kernel_spmd

F32 = mybir.dt.float32
F32R = mybir.dt.float32r
BF16 = mybir.dt.bfloat16
AF = mybir.ActivationFunctionType
ALU = mybir.AluOpType
AX = mybir.AxisListType

P = 128
D = 1024
DFF = 4096
SEQ = 4096
BATCH = 8
DEPTH = 4
NORM_EPS = 1e-6
GN_EPS = 64e-5
IN_COLS = 3592
NDS = 20
USE_F32R = True
import os as _os
RWSTOP = int(_os.environ.get('RWSTOP', '0'))
EVSTOP = int(_os.environ.get('EVSTOP', '0'))


class Buf:
    __slots__ = ("w", "r", "f32r")

    def __init__(self):
        self.w = None
        self.r = {}
        self.f32r = False


class View:
    __slots__ = ("ap", "buf")

    def __init__(self, ap, buf):
        self.ap = ap
        self.buf = buf


class TT:
    def __init__(self, h, buf=None):
        self.h = h
        self.buf = buf if buf is not None else Buf()
        self._subs = {}

    def __getitem__(self, key):
        return View(self.h[key], self.buf)

    def sub(self, key):
        if key not in self._subs:
            self._subs[key] = TT(self.h)
        return self._subs[key]

    def v(self, ap):
        return View(ap, self.buf)


class K:
    def __init__(self, nc, es):
        self.nc = nc
        self.es = es
        self.E = {"pe": nc.tensor, "act": nc.scalar, "dve": nc.vector, "pool": nc.gpsimd, "sp": nc.sync}
        self.esem = {}
        self.ecnt = {}
        for e in ("pe", "act", "dve", "pool"):
            self.esem[e] = es.enter_context(nc.semaphore("es_" + e))
            self.ecnt[e] = 0
        self.seen = {e: {} for e in self.E}
        self.dq = {}
        for q in ("sp", "pool", "act"):
            sems = [es.enter_context(nc.semaphore("ds_%s%d" % (q, i))) for i in range(NDS)]
            self.dq[q] = {"sems": sems, "cnt": [0] * NDS, "i": 0}
        self.semname = {}
        self.uid = 0
        self.n_ins = 0
        self.dead = False

    def stop(self, n):
        if RWSTOP == n:
            self.dead = True

    def sb(self, shape, dt, name=None, es=None):
        self.uid += 1
        h = (es or self.es).enter_context(self.nc.sbuf_tensor("%s_%d" % (name or "sb", self.uid), list(shape), dt))
        return TT(h)

    def ps(self, shape, dt=F32, name=None, es=None):
        self.uid += 1
        h = (es or self.es).enter_context(self.nc.psum_tensor("%s_%d" % (name or "ps", self.uid), list(shape), dt))
        return TT(h)

    def dram(self, name, shape, dt, kind="Internal"):
        return TT(self.nc.dram_tensor(name, list(shape), dt, kind=kind).ap())

    def _key(self, sem):
        return id(sem)

    def _waits(self, en, outs, ins, extra=()):
        need = {}

        def add(tok):
            if tok is None:
                return
            s, v = tok
            if en == "pe" and s is self.esem["pe"]:
                return
            k = id(s)
            if k not in need or need[k][1] < v:
                need[k] = (s, v)

        for x in ins:
            add(x.buf.w)
        for x in outs:
            add(x.buf.w)
            for t in x.buf.r.values():
                add(t)
        for t in extra:
            add(t)
        eng = self.E[en]
        seen = self.seen[en]
        for k, (s, v) in need.items():
            if seen.get(k, 0) < v:
                eng.wait_ge(s, v)
                seen[k] = v
                self.n_ins += 1

    def _done(self, tok, outs, ins):
        k = id(tok[0])
        for x in ins:
            x.buf.r[k] = tok
        for x in outs:
            x.buf.w = tok
            x.buf.r = {}

    def emit(self, en, fn, outs, ins):
        if self.dead:
            return
        outs = [o for o in outs if isinstance(o, View)]
        ins = [i for i in ins if isinstance(i, View)]
        self._waits(en, outs, ins)
        ins_obj = fn()
        self.ecnt[en] += 1
        tok = (self.esem[en], self.ecnt[en])
        ins_obj.then_inc(tok[0], 1)
        self.n_ins += 1
        self._done(tok, outs, ins)

    def dma(self, q, out, in_):
        if self.dead:
            return
        dq = self.dq[q]
        i = dq["i"] % NDS
        dq["i"] += 1
        sem = dq["sems"][i]
        self._waits(q, [out], [in_], extra=[(sem, dq["cnt"][i])] if dq["cnt"][i] else [])
        ins_obj = self.E[q].dma_start(out=out.ap, in_=in_.ap)
        dq["cnt"][i] += 16
        tok = (sem, dq["cnt"][i])
        ins_obj.then_inc(sem, 16)
        self.n_ins += 1
        self._done(tok, [out], [in_])

    def wait_all(self, en, views):
        self._waits(en, [], views)

    @staticmethod
    def _a(x):
        return x.ap if isinstance(x, View) else x

    @staticmethod
    def _o(x):
        return x.ap.bitcast(F32R) if (x.buf.f32r and USE_F32R) else x.ap

    def mm(self, out, lhsT, rhs, start=True, stop=True, r=None):
        if r is None:
            r = lhsT.buf.f32r and rhs.buf.f32r
        r = r and USE_F32R
        la, ra = (lhsT.ap.bitcast(F32R), rhs.ap.bitcast(F32R)) if r else (lhsT.ap, rhs.ap)
        self.emit("pe", lambda: self.nc.tensor.matmul(out.ap, lhsT=la, rhs=ra, start=start, stop=stop),
                  [out], [lhsT, rhs])

    def tr(self, out, in_, ident):
        self.emit("pe", lambda: self.nc.tensor.transpose(out.ap, in_.ap, ident.ap), [out], [in_, ident])

    def act(self, out, in_, func, bias=None, scale=None, accum=None):
        kw = {}
        if bias is not None:
            kw["bias"] = self._a(bias)
        if scale is not None:
            kw["scale"] = self._a(scale)
        if accum is not None:
            kw["accum_out"] = accum.ap
        self.emit("act", lambda: self.nc.scalar.activation(out=self._o(out), in_=in_.ap, func=func, **kw),
                  [out, accum], [in_, bias, scale])

    def tt(self, en, out, in0, in1, op):
        self.emit(en, lambda: self.E[en].tensor_tensor(out=self._o(out), in0=in0.ap, in1=in1.ap, op=op), [out], [in0, in1])

    def ts(self, en, out, in0, s1, op0, s2=None, op1=None, accum=None):
        kw = {}
        if op1 is not None:
            kw["op1"] = op1
        if accum is not None:
            kw["accum_out"] = accum.ap
        self.emit(en, lambda: self.E[en].tensor_scalar(out=self._o(out), in0=in0.ap, scalar1=self._a(s1),
                                                       scalar2=self._a(s2), op0=op0, **kw),
                  [out, accum], [in0, s1, s2])

    def stt(self, out, in0, scalar, in1, op0, op1, accum=None):
        kw = {}
        if accum is not None:
            kw["accum_out"] = accum.ap
        self.emit("dve", lambda: self.nc.vector.scalar_tensor_tensor(out=self._o(out), in0=in0.ap, scalar=self._a(scalar),
                                                                     in1=in1.ap, op0=op0, op1=op1, **kw),
                  [out, accum], [in0, scalar, in1])

    def copy(self, en, out, in_):
        if en == "act":
            self.emit("act", lambda: self.nc.scalar.copy(out=self._o(out), in_=in_.ap), [out], [in_])
        else:
            self.emit(en, lambda: self.E[en].tensor_copy(out=self._o(out), in_=in_.ap), [out], [in_])

    def recip(self, out, in_):
        self.emit("dve", lambda: self.nc.vector.reciprocal(out=out.ap, in_=in_.ap), [out], [in_])

    def memset(self, en, out, val):
        self.emit(en, lambda: self.E[en].memset(out.ap, val), [out], [])

    def scan(self, out, d0, d1, init, op0, op1):
        self.emit("dve", lambda: self.nc.vector.tensor_tensor_scan(out=out.ap, data0=d0.ap, data1=d1.ap,
                                                                   initial=self._a(init), op0=op0, op1=op1),
                  [out], [d0, d1, init])

    def reduce(self, out, in_, op, axis=AX.X):
        self.emit("dve", lambda: self.nc.vector.tensor_reduce(out=out.ap, in_=in_.ap, axis=axis, op=op), [out], [in_])

    def affsel(self, out, in_, pattern, cmp, fill, base, cm):
        self.emit("pool", lambda: self.nc.gpsimd.affine_select(out=self._o(out), in_=in_.ap, pattern=pattern, compare_op=cmp,
                                                               fill=fill, base=base, channel_multiplier=cm),
                  [out], [in_])


class Ring:
    def __init__(self, k, n, shape, dt, name, es=None, psum=False):
        self.t = [(k.ps if psum else k.sb)(shape, dt, name=name, es=es) for _ in range(n)]
        self.i = 0

    def next(self):
        t = self.t[self.i % len(self.t)]
        self.i += 1
        return t


class Prog:
    def __init__(self, S=SEQ, layers=None, dbg=None):
        self.S = S
        self.NT = S // P
        self.layers = list(range(DEPTH)) if layers is None else layers
        self.dbg = dbg or {}
        nc = bass.Bass("TRN2", target_bir_lowering=False)
        self.nc = nc
        self.es = ExitStack()
        self.k = K(nc, self.es)

    SHAPES = {
        "norm_g": [DEPTH, 4, D], "e_w_in": [2, D, IN_COLS], "e_b_if": [2, 8], "e_conv_w": [2, 4, 1024],
        "e_head_g": [2, 4, 128], "e_w_out": [2, D, D], "r_mu": [2, 6, D], "r_w_rkv": [2, 3, D, D],
        "r_w0": [2, D], "r_w1": [2, D, 64], "r_w2": [2, 64, D], "r_a0": [2, D], "r_a1": [2, D, 64],
        "r_a2": [2, 64, D], "r_g1": [2, D, 128], "r_g2": [2, 128, D], "r_k_k": [2, D], "r_k_a": [2, D],
        "r_r_k": [2, D], "r_ln_g": [2, D], "r_ln_b": [2, D], "r_w_out": [2, D, D],
        "mlp_w_up": [DEPTH, D, DFF], "mlp_w_down": [DEPTH, DFF, D],
    }

    def __getattr__(self, name):
        if name in Prog.SHAPES:
            t = self.k.dram(name, Prog.SHAPES[name], F32, kind="ExternalInput")
            self.used.append(name)
            setattr(self, name, t)
            return t
        raise AttributeError(name)

    def declare(self):
        k = self.k
        S = self.S
        self.used = []
        self.x = k.dram("x", [S, D], F32, kind="ExternalInput")
        self.out = k.dram("out", [S, D], F32, kind="ExternalOutput")
        self.hA = k.dram("hA", [S, D], F32)
        self.hB = k.dram("hB", [S, D], F32)

    def in_map(self, inputs, b):
        m = {"x": np.ascontiguousarray(inputs["x"][b, :self.S])}
        for name in self.used:
            m[name] = np.ascontiguousarray(inputs[name]).reshape(Prog.SHAPES[name])
        return m

    def barrier(self):
        k = self.k
        toks = [(k.esem[e], k.ecnt[e]) for e in k.esem if k.ecnt[e]]
        for q in k.dq.values():
            for s_, c_ in zip(q["sems"], q["cnt"]):
                if c_:
                    toks.append((s_, c_))
        for en in ("pe", "act", "dve", "pool", "sp"):
            seen = k.seen[en]
            for s_, v_ in toks:
                if seen.get(id(s_), 0) < v_:
                    k.E[en].wait_ge(s_, v_)
                    seen[id(s_)] = v_
                    k.n_ins += 1

    def consts(self):
        k = self.k
        nc = self.nc
        self.ident_f = k.sb([P, P], F32, "identf")
        self.ident_b = k.sb([P, P], BF16, "identb")
        k.memset("pool", self.ident_f[:, :], 1.0)
        k.affsel(self.ident_f[:, :], self.ident_f[:, :], [[-1, P]], ALU.is_equal, 0.0, 0, 1)
        k.copy("dve", self.ident_b[:, :], self.ident_f[:, :])
        self.eps_t = k.sb([P, 1], F32, "eps")
        k.memset("dve", self.eps_t[:, :], NORM_EPS)

    def bcast_load(self, dst, src_ap, n):
        self.k.dma("sp", dst, View(src_ap.partition_broadcast(P), Buf()))

    def rstd(self, out, ss, dn, eps_t=None):
        k = self.k
        k.act(out, ss, AF.Ln, bias=(eps_t or self.eps_t)[:, 0:1], scale=1.0 / dn)
        k.act(out, out, AF.Exp, scale=-0.5)

    def norm_T(self, hsrc, ti, ht, hn, ss, rs, junk, tps, uT_dst):
        k = self.k
        k.dma("sp", ht[:, :], hsrc.sub(ti)[ti * P:(ti + 1) * P, :])
        k.act(View(junk.h[:, :], Buf()), ht[:, :], AF.Square, accum=ss[:, 0:1])
        self.rstd(rs[:, 0:1], ss[:, 0:1], D)
        k.act(hn[:, :], ht[:, :], AF.Copy, scale=rs[:, 0:1])
        for c in range(8):
            k.tr(tps[:, c, :], hn[:, c * P:(c + 1) * P], self.ident_b[:, :])
        k.copy("dve", uT_dst, tps[:, :, :])

    def norm_part(self, hsrc, ti, ht, hn, ss, rs, junk):
        k = self.k
        k.dma("sp", ht[:, :], hsrc.sub(ti)[ti * P:(ti + 1) * P, :])
        k.act(View(junk.h[:, :], Buf()), ht[:, :], AF.Square, accum=ss[:, 0:1])
        self.rstd(rs[:, 0:1], ss[:, 0:1], D)
        k.act(hn[:, :], ht[:, :], AF.Copy, scale=rs[:, 0:1])

    def tr_part(self, hn, tps, uT_dst):
        k = self.k
        for c in range(8):
            k.tr(tps[:, c, :], hn[:, c * P:(c + 1) * P], self.ident_b[:, :])
        k.copy("dve", uT_dst, tps[:, :, :])

    def mlp(self, layer, hsrc, hdst):
        k = self.k
        nc = self.nc
        with ExitStack() as es:
            wu = k.sb([P, 8, DFF], BF16, "wu", es)
            wd = k.sb([P, 32, D], BF16, "wd", es)
            g2 = k.sb([P, 8], F32, "g2", es)
            g3b = k.sb([P, D], F32, "g3b", es)
            stage = Ring(k, 2, [P, 2048], F32, "stg", es)
            with nc.allow_non_contiguous_dma(reason="tiny param"):
                k.dma("sp", g2[:, :], self.norm_g.v(self.norm_g.h[layer, 2, :].rearrange("(c p) -> p c", p=P)))
            k.dma("sp", g3b[:, :], self.norm_g.v(self.norm_g.h[layer, 3, :].partition_broadcast(P)))
            n = 0
            for kd in range(8):
                for hf in range(2):
                    st = stage.next()
                    k.dma("sp", st[:, :], self.mlp_w_up[layer, kd * P:(kd + 1) * P, hf * 2048:(hf + 1) * 2048])
                    if n % 2 == 0:
                        k.act(wu[:, kd, hf * 2048:(hf + 1) * 2048], st[:, :], AF.Copy, scale=g2[:, kd:kd + 1])
                    else:
                        k.ts("pool", wu[:, kd, hf * 2048:(hf + 1) * 2048], st[:, :], g2[:, kd:kd + 1], ALU.mult)
                    n += 1
            for c2 in range(16):
                st = stage.next()
                src = self.mlp_w_down.h[layer, c2 * 256:(c2 + 1) * 256, :].rearrange("(c p) n -> p c n", p=P)
                k.dma("sp", st.v(st.h[:, :].rearrange("p (c n) -> p c n", c=2)), self.mlp_w_down.v(src))
                dst = wd.v(wd.h[:, 2 * c2:2 * c2 + 2, :])
                sv = st.v(st.h[:, :].rearrange("p (c n) -> p c n", c=2))
                if n % 2 == 0:
                    k.copy("dve", dst, sv)
                else:
                    k.copy("pool", dst, sv)
                n += 1

            TS = 256
            nsub = TS // P
            hts = Ring(k, 2 * nsub, [P, D], F32, "ht", es)
            hns = Ring(k, 2, [P, D], BF16, "hn", es)
            junk = k.sb([P, D], BF16, "junk", es)
            sss = Ring(k, 4, [P, 4], F32, "ss", es)
            uTs = Ring(k, 2, [P, 8, TS], BF16, "uT", es)
            aT = Ring(k, 1, [P, 32, TS], BF16, "aT", es)
            tpsr = Ring(k, 2, [P, 8, P], BF16, "tps", es, psum=True)
            upsr = Ring(k, 2, [P, 512], F32, "ups", es, psum=True)
            dpsr = Ring(k, 4, [P, 512], F32, "dps", es, psum=True)
            ytmp = Ring(k, 2, [P, D], F32, "yt", es)
            for st_i in range(self.S // TS):
                uT = uTs.next()
                hl = []
                for j in range(nsub):
                    ht = hts.next()
                    hl.append(ht)
                    s4 = sss.next()
                    self.norm_T(hsrc, st_i * nsub + j, ht, hns.next(), s4.sub(0), s4.sub(1), junk, tpsr.next(),
                                uT.v(uT.h[:, :, j * P:(j + 1) * P]))
                a = aT.next()
                for fc in range(32):
                    ups = upsr.next()
                    for kd in range(8):
                        k.mm(ups[:, 0:TS], wu[:, kd, fc * P:(fc + 1) * P], uT[:, kd, :], start=(kd == 0), stop=(kd == 7))
                    if fc % 2 == 0:
                        k.act(a[:, fc, :], ups[:, 0:TS], AF.Relu)
                        k.tt("pool", a[:, fc, :], a[:, fc, :], a[:, fc, :], ALU.mult)
                    else:
                        k.ts("dve", a[:, fc, :], ups[:, 0:TS], 0.0, ALU.max)
                        k.tt("pool", a[:, fc, :], a[:, fc, :], a[:, fc, :], ALU.mult)
                for j in range(nsub):
                    dps = [dpsr.next(), dpsr.next()]
                    for hf in range(2):
                        for kc in range(32):
                            k.mm(dps[hf][:, :], a[:, kc, j * P:(j + 1) * P], wd[:, kc, hf * 512:(hf + 1) * 512],
                                 start=(kc == 0), stop=(kc == 31))
                    self.post_norm_add(dps, hl[j], g3b, hdst, st_i * nsub + j, sss.next(), junk, ytmp.next())

    def post_norm_add(self, dps, ht, gb, hdst, ti, s4, junk, yt):
        k = self.k
        ssa, ssb, rs = s4.sub(0), s4.sub(1), s4.sub(2)
        k.act(View(junk.h[:, 0:512], Buf()), dps[0][:, :], AF.Square, accum=ssa[:, 0:1])
        k.act(View(junk.h[:, 512:1024], Buf()), dps[1][:, :], AF.Square, accum=ssb[:, 1:2])
        k.tt("dve", rs[:, 2:3], ssa[:, 0:1], ssb[:, 1:2], ALU.add)
        self.rstd(rs[:, 2:3], rs[:, 2:3], D)
        for hf in range(2):
            sl = slice(hf * 512, (hf + 1) * 512)
            k.stt(yt[:, sl], dps[hf][:, :], rs[:, 2:3], gb[:, sl], ALU.mult, ALU.mult)
            k.tt("pool", yt[:, sl], yt[:, sl], ht[:, sl], ALU.add)
        k.dma("pool", hdst.sub(ti)[ti * P:(ti + 1) * P, :], yt[:, :])

    def finish(self, last):
        k = self.k
        vs = [last.sub(ti)[:, :] for ti in range(self.NT)]
        k.wait_all("sp", vs)
        k.wait_all("pool", vs)


class PSlots:
    def __init__(self, k, nbanks, es, name):
        self.slots = []
        for b in range(nbanks):
            t = k.ps([P, 4, P], F32, name=name, es=es)
            self.slots.append((t, 0))
        self.i = 0

    def next(self):
        s = self.slots[self.i % len(self.slots)]
        self.i += 1
        return Slot(*s)


class Slot:
    def __init__(self, tt, j):
        self.tt = tt
        self.j = j

    def __getitem__(self, key):
        ps, fs = key
        if self.j is None:
            if fs == slice(None):
                fs = slice(0, P)
            return View(self.tt.h[ps, fs], self.tt.buf)
        return View(self.tt.h[ps, self.j, fs], self.tt.buf)


def bc3(tt, ap2, n):
    shp = list(ap2.shape)
    return View(ap2.unsqueeze(2).to_broadcast([shp[0], shp[1], n]), tt.buf)


def _load_w(self, dst_view_fn, src_rows_fn, nchunks, cols, es_stage, gain=None):
    k = self.k
    for kd in range(nchunks):
        for c0 in range(0, cols, 2048):
            cw = min(2048, cols - c0)
            st = es_stage.next()
            k.dma("sp", st[:, 0:cw], src_rows_fn(kd, c0, cw))
            self._wn = getattr(self, "_wn", 0) + 1
            dst = dst_view_fn(kd, c0, cw)
            if gain is not None:
                if self._wn % 2 == 0:
                    k.act(dst, st[:, 0:cw], AF.Copy, scale=gain[:, kd:kd + 1])
                else:
                    k.ts("pool", dst, st[:, 0:cw], gain[:, kd:kd + 1], ALU.mult)
            else:
                k.copy("dve" if self._wn % 2 == 0 else "pool", dst, st[:, 0:cw])


Prog.load_w = _load_w


def _fm_param(self, dst, src_ap, es=None):
    with self.nc.allow_non_contiguous_dma(reason="tiny param"):
        self.k.dma("sp", dst, View(src_ap.rearrange("(c p) -> p c", p=P), Buf()))


Prog.fm_param = _fm_param


def _rwkv(self, layer, hsrc, hdst):
    k = self.k
    nc = self.nc
    o = layer // 2
    with ExitStack() as es:
        g0 = k.sb([P, 8], F32, "g0", es)
        self.fm_param(g0[:, :], self.norm_g.h[layer, 0, :])
        g1b = k.sb([P, D], F32, "g1b", es)
        k.dma("sp", g1b[:, :], View(self.norm_g.h[layer, 1, :].partition_broadcast(P), Buf()))
        lngb = k.sb([P, D], F32, "lngb", es)
        k.dma("sp", lngb[:, :], View(self.r_ln_g.h[o, :].partition_broadcast(P), Buf()))
        lnbb = k.sb([P, D], F32, "lnbb", es)
        k.dma("sp", lnbb[:, :], View(self.r_ln_b.h[o, :].partition_broadcast(P), Buf()))
        mu = k.sb([P, 6, 8], F32, "mu", es)
        for i in range(6):
            self.fm_param(mu[:, i, :], self.r_mu.h[o, i, :])
        fp = {}
        for nm, src in (("w0", self.r_w0), ("a0", self.r_a0), ("kk", self.r_k_k), ("ka", self.r_k_a), ("rk", self.r_r_k)):
            fp[nm] = k.sb([P, 8], F32, nm, es)
            self.fm_param(fp[nm][:, :], src.h[o, :])
        gneps = k.sb([P, 1], F32, "gneps", es)
        k.memset("dve", gneps[:, :], GN_EPS)
        wr = k.sb([P, 8, D], BF16, "wr", es)
        wk = k.sb([P, 8, D], BF16, "wk", es)
        wv = k.sb([P, 8, D], BF16, "wv", es)
        wo = k.sb([P, 8, D], BF16, "wo", es)
        w1 = k.sb([P, 8, 64], BF16, "w1", es)
        a1 = k.sb([P, 8, 64], BF16, "a1", es)
        g1 = k.sb([P, 8, P], BF16, "g1", es)
        w2 = k.sb([64, D], BF16, "w2", es)
        a2 = k.sb([64, D], BF16, "a2", es)
        g2 = k.sb([P, D], BF16, "g2", es)
        with ExitStack() as es2:
            stage = Ring(k, 2, [P, 2048], F32, "stg", es2)
            for wt, i in ((wr, 0), (wk, 1), (wv, 2)):
                self.load_w(lambda kd, c0, cw, wt=wt: wt[:, kd, c0:c0 + cw],
                            lambda kd, c0, cw, i=i: self.r_w_rkv[o, i, kd * P:(kd + 1) * P, c0:c0 + cw], 8, D, stage, g0)
            self.load_w(lambda kd, c0, cw: wo[:, kd, c0:c0 + cw],
                        lambda kd, c0, cw: self.r_w_out[o, kd * P:(kd + 1) * P, c0:c0 + cw], 8, D, stage, None)
            for wt, src, cols in ((w1, self.r_w1, 64), (a1, self.r_a1, 64), (g1, self.r_g1, P)):
                self.load_w(lambda kd, c0, cw, wt=wt: wt[:, kd, c0:c0 + cw],
                            lambda kd, c0, cw, src=src: src[o, kd * P:(kd + 1) * P, c0:c0 + cw], 8, cols, stage, g0)
            for wt, src in ((w2, self.r_w2), (a2, self.r_a2)):
                st = stage.next()
                k.dma("sp", st[0:64, 0:D], src[o, :, :])
                k.copy("dve", wt[:, :], st[0:64, 0:D])
            st = stage.next()
            k.dma("sp", st[:, 0:D], self.r_g2[o, :, :])
            k.copy("dve", g2[:, :], st[:, 0:D])
            self.barrier()
        m_ts = k.sb([P, P], F32, "m_ts", es)
        mT_s = k.sb([P, P], F32, "mT_s", es)
        mT_i = k.sb([P, P], F32, "mT_i", es)
        bdones = k.sb([P, P], F32, "bdones", es)
        for m_, pat, cm, cmp in ((m_ts, -1, 1, ALU.is_gt), (mT_s, 1, -1, ALU.is_gt), (mT_i, 1, -1, ALU.is_ge)):
            k.memset("pool", m_[:, :], 0.0)
            for b in range(2):
                sl = slice(b * 64, b * 64 + 64)
                k.memset("pool", m_[sl, sl], 1.0)
                k.affsel(m_[sl, sl], m_[sl, sl], [[pat, 64]], cmp, 0.0, 0, cm)
        k.memset("pool", bdones[:, :], 0.0)
        for b in range(2):
            sl = slice(b * 64, b * 64 + 64)
            k.memset("pool", bdones[sl, sl], 1.0)
        headsel = k.sb([P, 8, 16], F32, "headsel", es)
        k.memset("pool", headsel[:, :, :], 0.0)
        for c in range(8):
            for b in range(2):
                k.memset("pool", headsel[b * 64:b * 64 + 64, c, 2 * c + b:2 * c + b + 1], 1.0)
        rmask = k.sb([P, P], F32, "rmask", es)
        k.memset("pool", rmask[:, :], 1.0)
        k.memset("pool", rmask[:, 0:1], 0.0)
        k.memset("pool", rmask[:, 64:65], 0.0)
        Fm = lambda nm: k.sb([P, 8, P], F32, nm, es)
        rT, kT, aT, sg, kk, scr, cum = Fm("rT"), Fm("kT"), Fm("aT"), Fm("sg"), Fm("kk"), Fm("scr"), Fm("cum")
        epos, eneg, eexc, erem = Fm("epos"), Fm("eneg"), Fm("eexc"), Fm("erem")
        ktT, alT, beT = Fm("ktT"), Fm("alT"), Fm("beT")
        kka, rbT = aT, epos
        for t_ in (rT, kT, aT, kk, scr, eneg, eexc, erem, epos, ktT, alT, beT):
            t_.buf.f32r = True
        Tm = lambda nm: k.sb([P, D], F32, nm, es)
        vtok, gate, Atok, Bhat, Khat, ytok, ht = Tm("vtok"), Tm("gate"), Tm("Atok"), Tm("Bhat"), Tm("Khat"), Tm("ytok"), Tm("ht")
        gl = k.sb([P, 16], F32, "gl", es)
        bonus = k.sb([P, 16], F32, "bonus", es)
        uTs = Ring(k, 2, [P, 8, P + 1], BF16, "uTh", es)
        xis = Ring(k, 2, [P, 8, P], BF16, "xi", es)
        hn = k.sb([P, D], BF16, "hn", es)
        junk = k.sb([P, D], BF16, "junk", es)
        sss = Ring(k, 4, [P, 4], F32, "ss", es)
        hw = k.sb([64, P], BF16, "hw", es)
        ha = k.sb([64, P], BF16, "ha", es)
        hg = k.sb([P, P], BF16, "hg", es)
        St = k.sb([P, 8, 64], F32, "St", es)
        for g in range(4):
            k.memset("pool", St.sub(g)[:, 2 * g:2 * g + 2, :], 0.0)
        gnt = [k.sb([P, 16], F32, "gn%d" % i, es) for i in range(4)] + [k.sb([P, D], F32, "ysq", es)]
        WB = []
        for t_ in (rT, kT, aT, kk, scr, eneg, eexc, erem):
            for half in range(2):
                WB.append(TT(t_.h[:, half * 4:(half + 1) * 4, :]))
        xcnt, acnt = [0], [0]
        for t_ in WB:
            t_.buf.f32r = True
        vtok.buf.f32r = True

        def handoff(srcs, dsts):
            toks = {}
            for s_ in srcs:
                for t in ([s_.buf.w] if s_.buf.w else []) + list(s_.buf.r.values()):
                    if id(t[0]) not in toks or toks[id(t[0])][1] < t[1]:
                        toks[id(t[0])] = t
            for d_ in dsts:
                d_.buf.w = None
                d_.buf.r = dict(toks)

        Yl = k.sb([P, 4, 64], F32, "Yl", es)
        Qe = k.sb([P, 2, P], F32, "Qe", es)
        MT = k.sb([P, 4, 64], F32, "MT", es)
        Nn = k.sb([P, 4, 64], F32, "Nn", es)
        glD = k.sb([P, 16, 64], F32, "glD", es)
        sid = k.sb([P, 64], F32, "sid", es)
        k.copy("pool", sid[0:64, :], self.ident_f[0:64, 0:64])
        k.copy("pool", sid[64:128, :], self.ident_f[64:128, 64:128])
        zb = k.sb([P, D], BF16, "zb", es)
        zT = k.sb([P, 8, P], BF16, "zT", es)
        tps = k.ps([P, 8, P], BF16, "tps", es)
        pG = Ring(k, 7, [P, 512], F32, "pG", es, psum=True)

        class _PA:
            def next(self_):
                t_ = pG.next()
                return Slot(t_, None)
        pA = _PA()
        pT = pG

        prev_uT = None
        for ti in range(self.NT):
            uT = uTs.next()
            s4 = sss.next()
            self.norm_T(hsrc, ti, ht, hn, s4.sub(0), s4.sub(1), junk, tps, uT.v(uT.h[:, :, 1:P + 1]))
            if ti == 0:
                k.memset("pool", uT[:, :, 0:1], 0.0)
            else:
                k.copy("pool", uT[:, :, 0:1], prev_uT[:, :, P:P + 1])
            prev_uT = uT
            xx = ktT
            k.tt("dve", xx[:, :, :], uT[:, :, 0:P], uT[:, :, 1:P + 1], ALU.subtract)
            LD = -0.6065306597126334

            def mix(i):
                xi = xis.next()
                k.tt("pool", xi[:, :, :], xx[:, :, :], bc3(mu, mu.h[:, i, :], P), ALU.mult)
                k.tt("dve", xi[:, :, :], xi[:, :, :], uT[:, :, 1:P + 1], ALU.add)
                return xi

            def proj_fm(wt, xi, dst):
                for n_ in range(8):
                    ps = pA.next()
                    for kd in range(8):
                        k.mm(ps[:, :], wt[:, kd, n_ * P:(n_ + 1) * P], xi[:, kd, :], start=(kd == 0), stop=(kd == 7))
                    k.copy("act", dst[:, n_, :], ps[:, :])

            xi_k = mix(2)
            xi_w = mix(1)
            proj_fm(wk, xi_k, kT)
            k.tt("pool", kk[:, :, :], kT[:, :, :], bc3(fp["kk"], fp["kk"].h[:, :], P), ALU.mult)
            k.tt("dve", scr[:, :, :], kk[:, :, :], kk[:, :, :], ALU.mult)
            ps = pA.next()
            for kd in range(8):
                k.mm(ps[0:64, :], w1[:, kd, :], xi_w[:, kd, :], start=(kd == 0), stop=(kd == 7))
            k.act(hw[:, :], ps[0:64, :], AF.Tanh)
            for n_ in range(8):
                ps = pA.next()
                k.mm(ps[:, :], w2[:, n_ * P:(n_ + 1) * P], hw[:, :])
                k.act(sg[:, n_, :], ps[:, :], AF.Sigmoid, bias=fp["w0"][:, n_:n_ + 1])
            xi_a = mix(4)
            for c in range(8):
                k.scan(cum[:, c, :], rmask[:, :], sg[:, c, :], 0.0, ALU.mult, ALU.add)
            k.act(epos[:, :, :], cum[:, :, :], AF.Exp, scale=LD)
            k.act(eneg[:, :, :], cum[:, :, :], AF.Exp, scale=-LD)
            k.tt("pool", sg[:, :, :], cum[:, :, :], sg[:, :, :], ALU.subtract)
            k.act(eexc[:, :, :], sg[:, :, :], AF.Exp, scale=LD)
            c16 = cum.h[:, :, :].rearrange("p c (j t) -> p (c j) t", t=64)
            cl = View(c16[:, :, 63:64].to_broadcast([P, 16, 64]), cum.buf)
            s16 = sg.v(sg.h[:, :, :].rearrange("p c (j t) -> p (c j) t", t=64))
            k.tt("dve", s16, cl, View(c16, cum.buf), ALU.subtract)
            k.act(erem[:, :, :], sg[:, :, :], AF.Exp, scale=LD)
            e16 = epos.h[:, :, :].rearrange("p c (j t) -> p (c j) t", t=64)
            k.copy("pool", gl.v(gl.h[:, :].unsqueeze(2)), View(e16[:, :, 63:64], epos.buf))
            ps = pA.next()
            for kd in range(8):
                k.mm(ps[0:64, :], a1[:, kd, :], xi_a[:, kd, :], start=(kd == 0), stop=(kd == 7))
            k.copy("act", ha[:, :], ps[0:64, :])
            for n_ in range(8):
                ps = pA.next()
                k.mm(ps[:, :], a2[:, n_ * P:(n_ + 1) * P], ha[:, :])
                k.act(aT[:, n_, :], ps[:, :], AF.Sigmoid, bias=fp["a0"][:, n_:n_ + 1])
            xi_r = mix(0)
            for hf in range(2):
                ps = pT.next()
                for c4 in range(4):
                    c = hf * 4 + c4
                    k.mm(ps[:, c4 * P:(c4 + 1) * P], bdones[:, :], scr[:, c, :])
                dstv = scr.v(scr.h[:, hf * 4:(hf + 1) * 4, :].rearrange("p c t -> p (c t)"))
                k.ts("dve", dstv, ps[:, :], 1e-24, ALU.max)
            k.act(scr[:, :, :], scr[:, :, :], AF.Ln)
            k.act(scr[:, :, :], scr[:, :, :], AF.Exp, scale=-0.5)
            k.tt("dve", kk[:, :, :], kk[:, :, :], scr[:, :, :], ALU.mult)
            k.stt(scr[:, :, :], aT[:, :, :], -1.0, bc3(fp["ka"], fp["ka"].h[:, :], P), ALU.add, ALU.mult)
            k.stt(kT[:, :, :], scr[:, :, :], 1.0, kT[:, :, :], ALU.add, ALU.mult)
            k.tt("pool", kka[:, :, :], kk[:, :, :], aT[:, :, :], ALU.mult)
            proj_fm(wr, xi_r, rT)
            xi_v = mix(3)
            k.tt("pool", scr[:, :, :], rT[:, :, :], kT[:, :, :], ALU.mult)
            k.tt("dve", scr[:, :, :], scr[:, :, :], bc3(fp["rk"], fp["rk"].h[:, :], P), ALU.mult)
            for hf in range(2):
                ps = pT.next()
                for kd in range(8):
                    k.mm(ps[:, :], xi_v[:, kd, :], wv[:, kd, hf * 512:(hf + 1) * 512], start=(kd == 0), stop=(kd == 7))
                k.copy("act", vtok[:, hf * 512:(hf + 1) * 512], ps[:, :])
            xi_g = mix(5)
            ps = pA.next()
            for c in range(8):
                k.mm(ps[:, 0:16], scr[:, c, :], headsel[:, c, :], start=(c == 0), stop=(c == 7))
            k.copy("act", bonus[:, :], ps[:, 0:16])
            ps = pA.next()
            for kd in range(8):
                k.mm(ps[:, :], g1[:, kd, :], xi_g[:, kd, :], start=(kd == 0), stop=(kd == 7))
            k.act(hg[:, :], ps[:, :], AF.Sigmoid)
            for hf in range(2):
                ps = pT.next()
                k.mm(ps[:, :], hg[:, :], g2[:, hf * 512:(hf + 1) * 512])
                k.copy("act", gate[:, hf * 512:(hf + 1) * 512], ps[:, :])
            k.stt(alT[:, :, :], kk[:, :, :], -1.0, eexc[:, :, :], ALU.mult, ALU.mult)
            k.tt("dve", beT[:, :, :], kka[:, :, :], eneg[:, :, :], ALU.mult)
            k.tt("pool", eexc[:, :, :], kka[:, :, :], erem[:, :, :], ALU.mult)
            k.tt("dve", ktT[:, :, :], kT[:, :, :], eneg[:, :, :], ALU.mult)
            k.tt("dve", erem[:, :, :], kT[:, :, :], erem[:, :, :], ALU.mult)
            k.tt("dve", rbT[:, :, :], rT[:, :, :], epos[:, :, :], ALU.mult)
            n_ = 0
            for src, dst in ((alT, Atok), (eexc, Bhat), (erem, Khat)):
                for hf in range(2):
                    ps = pT.next()
                    for c4 in range(4):
                        k.tr(ps[:, c4 * P:(c4 + 1) * P], src[:, hf * 4 + c4, :], self.ident_f[:, :])
                    k.copy("act" if n_ % 2 == 0 else "dve", dst[:, hf * 512:(hf + 1) * 512], ps[:, :])
                    n_ += 1
            k.stop(1)
            handoff([rT, kT, aT, kk, scr, eneg, eexc, erem], WB)
            k.tt("pool", glD[:, :, :], View(sid.h[:, :].unsqueeze(1).to_broadcast([P, 16, 64]), sid.buf),
                 bc3(gl, gl.h[:, :], 64), ALU.mult)
            k.stop(2)
            q4 = lambda v_: v_.h[:, :].rearrange("p (q t) -> p q t", q=4)
            names5 = ("A0", "AT0", "AakT", "ArbT", "ArkT")
            gsets = [{n_: WB[i_] for i_, n_ in zip((0, 1, 2, 3, 4), names5)},
                     {n_: WB[i_] for i_, n_ in zip((7, 12, 13, 14, 15), names5)}]
            src = {"al": alT, "be": beT, "kt": ktT, "rb": rbT}

            def p1_units(g_):
                Gs = gsets[g_ % 2]
                units = []
                for (l_, r_, msk, dst) in (("al", "be", m_ts, "A0"), ("be", "al", mT_s, "AT0"), ("kt", "al", mT_s, "AakT"),
                                           ("be", "rb", mT_i, "ArbT"), ("kt", "rb", mT_i, "ArkT")):
                    for pi in range(2):
                        def unit(l_=l_, r_=r_, msk=msk, dst=dst, pi=pi):
                            ps = pG.next()
                            hs = slice(pi * 64, pi * 64 + 64)
                            for cl in range(2):
                                c = 2 * g_ + cl
                                k.mm(ps[:, cl * P:(cl + 1) * P], src[l_][hs, c, :], src[r_][hs, c, :])
                            k.tt("dve", Gs[dst][:, 2 * pi:2 * pi + 2, :], ps.v(ps.h[:, 0:256].rearrange("p (q t) -> p q t", q=2)),
                                 View(msk.h[:, :].unsqueeze(1).to_broadcast([P, 2, P]), msk.buf), ALU.mult)
                        units.append(unit)
                return units

            for g in range(4):
                heads = [(2 * (2 * g + cl) + pi, 2 * g + cl, pi * 64) for pi in range(2) for cl in range(2)]
                G_ = gsets[g % 2]
                if g == 0:
                    for u_ in p1_units(0):
                        u_()
                k.stop(3)
                X = WB[5 + (xcnt[0] % 2)]
                xcnt[0] += 1
                for pi in range(2):
                    k.copy("pool", X.v(X.h[:, 2 * pi:2 * pi + 2, 0:64]),
                           Atok.v(Atok.h[:, g * 256:(g + 1) * 256].rearrange("p (cl pi d) -> p pi cl d", pi=2, cl=2)[:, pi]))
                ps = pG.next()
                for q, (h, c, po) in enumerate(heads):
                    k.mm(ps[:, q * 64:(q + 1) * 64], G_["AakT"][:, q, :], vtok[:, h * 64:(h + 1) * 64])
                k.copy("act", X.v(X.h[:, :, 64:128]), ps.v(ps.h[:, 0:256].rearrange("p (q d) -> p q d", q=4)))
                A, AT = G_["A0"], G_["AT0"]
                nxt_units = p1_units(g + 1) if g + 1 < 4 else []
                for lvl in range(6):
                    ps = pG.next()
                    for q in range(4):
                        k.mm(ps[:, q * P:(q + 1) * P], AT[:, q, :], X[:, q, :])
                    Xn = WB[5 + (xcnt[0] % 2)]
                    xcnt[0] += 1
                    k.tt("dve", Xn[:, :, :], ps.v(q4(ps)), X[:, :, :], ALU.add)
                    X = Xn
                    if lvl < 5:
                        ps2 = pG.next()
                        for q in range(4):
                            k.mm(ps2[:, q * P:(q + 1) * P], A[:, q, :], AT[:, q, :])
                        A2T = WB[8 + (acnt[0] % 4)]
                        acnt[0] += 1
                        k.copy("act", A2T[:, :, :], ps2.v(q4(ps2)))
                        A2 = None
                        if lvl < 4:
                            ps3 = pG.next()
                            for q in range(4):
                                k.mm(ps3[:, q * P:(q + 1) * P], AT[:, q, :], A[:, q, :])
                            A2 = WB[8 + (acnt[0] % 4)]
                            acnt[0] += 1
                            k.copy("act", A2[:, :, :], ps3.v(q4(ps3)))
                        A, AT = A2, A2T
                    for u_ in nxt_units[2 * lvl:2 * lvl + 2]:
                        u_()
                k.stop(4)
                ps = pG.next()
                for q, (h, c, po) in enumerate(heads):
                    k.mm(ps[:, q * 64:(q + 1) * 64], G_["ArkT"][:, q, :], vtok[:, h * 64:(h + 1) * 64], start=True, stop=False)
                    k.mm(ps[:, q * 64:(q + 1) * 64], G_["ArbT"][:, q, :], X[:, q, 64:128], start=False, stop=True)
                k.copy("act", Yl[:, :, :], ps.v(ps.h[:, 0:256].rearrange("p (q d) -> p q d", q=4)))
                ps = pG.next()
                for q, (h, c, po) in enumerate(heads):
                    hs = slice(po, po + 64)
                    cl = q % 2
                    k.mm(ps[hs, cl * P:(cl + 1) * P], X[:, q, 0:64], G_["ArbT"][:, q, :], r=False)
                k.tt("dve", Qe[:, :, :], ps.v(ps.h[:, 0:256].rearrange("p (c t) -> p c t", c=2)), rbT[:, 2 * g:2 * g + 2, :], ALU.add)
                for cc in range(2):
                    cs = slice(cc * 64, cc * 64 + 64)
                    psM = pG.next()
                    psN = pG.next()
                    for q, (h, c, po) in enumerate(heads):
                        hs = slice(po, po + 64)
                        hc = slice(h * 64, h * 64 + 64)
                        cl = q % 2
                        k.mm(psM[hs, cl * 64:(cl + 1) * 64], X[cs, q, 0:64], Bhat[cs, hc], r=False)
                        k.mm(psN[hs, cl * 64:(cl + 1) * 64], Bhat[cs, hc], X[cs, q, 64:128], start=True, stop=False, r=False)
                        k.mm(psN[hs, cl * 64:(cl + 1) * 64], Khat[cs, hc], vtok[cs, hc], start=False, stop=True, r=False)
                    gd = glD.v(glD.h[:, 4 * g:4 * g + 4, :].rearrange("p (cl cc) d -> p cl cc d", cc=2)[:, :, cc, :])
                    k.tt("dve", MT.v(MT.h[:, :, :].rearrange("p (cl cc) d -> p cl cc d", cc=2)[:, :, cc, :]),
                         psM.v(psM.h[:, 0:128].rearrange("p (c d) -> p c d", c=2)), gd, ALU.add)
                    k.copy("act", Nn.v(Nn.h[:, :, :].rearrange("p (cl cc) d -> p cl cc d", cc=2)[:, :, cc, :]),
                           psN.v(psN.h[:, 0:128].rearrange("p (c d) -> p c d", c=2)))
                k.stop(5)
                Sg = St.sub(g)
                psy = [pG.next(), pG.next()]
                for cc in range(2):
                    cs = slice(cc * 64, cc * 64 + 64)
                    psS = [pG.next(), pG.next()]
                    for q, (h, c, po) in enumerate(heads):
                        hs = slice(po, po + 64)
                        pi, cl = q // 2, q % 2
                        k.mm(psy[pi][cs, cl * 64:(cl + 1) * 64], Qe[hs, cl, cs], Sg[hs, c, :])
                        k.mm(psS[pi][hs, cl * 64:(cl + 1) * 64], MT[hs, cl * 2 + cc, :], Sg[hs, c, :])
                    for pi in range(2):
                        hs = slice(pi * 64, pi * 64 + 64)
                        k.tt("dve", Sg[hs, 2 * g:2 * g + 2, :], psS[pi].v(psS[pi].h[hs, 0:128].rearrange("p (c d) -> p c d", c=2)),
                             Nn.v(Nn.h[hs, :, :].rearrange("p (cl cc) d -> p cl cc d", cc=2)[:, :, cc, :]), ALU.add)
                for pi in range(2):
                    k.tt("dve", ytok.v(ytok.h[:, g * 256:(g + 1) * 256].rearrange("p (cl pi d) -> p pi cl d", pi=2, cl=2)[:, pi]),
                         psy[pi].v(psy[pi].h[:, 0:128].rearrange("p (c d) -> p c d", c=2)), Yl[:, 2 * pi:2 * pi + 2, :], ALU.add)
            k.stop(6)
            handoff(WB, [rT, kT, aT, kk, scr, eneg, eexc, erem])
            y3 = lambda t_: t_.v(t_.h[:, :].rearrange("p (h d) -> p h d", d=64))
            sA, sB, sM, sR, ysq = gnt
            k.reduce(sA[:, :], y3(ytok), ALU.add)
            k.tt("dve", ysq[:, :], ytok[:, :], ytok[:, :], ALU.mult)
            k.reduce(sB[:, :], y3(ysq), ALU.add)
            k.ts("dve", sM[:, :], sA[:, :], 1.0 / 64, ALU.mult)
            k.tt("dve", sA[:, :], sM[:, :], sM[:, :], ALU.mult)
            k.stt(sB[:, :], sB[:, :], 1.0 / 64, sA[:, :], ALU.mult, ALU.subtract)
            k.act(sR[:, :], sB[:, :], AF.Ln, bias=gneps[:, 0:1])
            k.act(sR[:, :], sR[:, :], AF.Exp, scale=-0.5)
            k.tt("dve", y3(ytok), y3(ytok), bc3(sM, sM.h[:, :], 64), ALU.subtract)
            k.tt("dve", y3(ytok), y3(ytok), bc3(sR, sR.h[:, :], 64), ALU.mult)
            k.tt("dve", ytok[:, :], ytok[:, :], lngb[:, :], ALU.mult)
            k.tt("dve", ytok[:, :], ytok[:, :], lnbb[:, :], ALU.add)
            k.tt("dve", y3(ysq), y3(vtok), bc3(bonus, bonus.h[:, :], 64), ALU.mult)
            k.tt("dve", ytok[:, :], ytok[:, :], ysq[:, :], ALU.add)
            k.tt("dve", zb[:, :], ytok[:, :], gate[:, :], ALU.mult)
            for c in range(8):
                k.tr(tps[:, c, :], zb[:, c * P:(c + 1) * P], self.ident_b[:, :])
            k.copy("dve", zT[:, :, :], tps[:, :, :])
            dps = [pT.next(), pT.next()]
            for hf in range(2):
                for c in range(8):
                    k.mm(dps[hf][:, :], zT[:, c, :], wo[:, c, hf * 512:(hf + 1) * 512], start=(c == 0), stop=(c == 7))
            self.post_norm_add(dps, ht, g1b, hdst, ti, sss.next(), junk, ysq)
        k.dead = False
        self.barrier()


Prog.rwkv = _rwkv


def U(ap):
    return View(ap, Buf())


def _even(self, layer, hsrc, hdst):
    k = self.k
    nc = self.nc
    e = layer // 2
    S, NT = self.S, self.NT
    RS8 = 1.0 / 8.0
    RSD = 1.0 / (128.0 ** 0.5)
    if not hasattr(self, "dQT"):
        self.dQT = nc.dram_tensor("dQT", [P, 4, S], BF16).ap()
        self.dKT = nc.dram_tensor("dKT", [P, 4, S], BF16).ap()
        self.dMQK = nc.dram_tensor("dMQK", [P, 8, S], BF16).ap()
        self.dV = nc.dram_tensor("dV", [S, 512], BF16).ap()
        self.dMV = nc.dram_tensor("dMV", [S, 512], BF16).ap()
        self.dOG = nc.dram_tensor("dOG", [S, 512], BF16).ap()
        self.dAH = nc.dram_tensor("dAH", [P, 8, S], BF16).ap()
    dQT, dKT, dMQK, dV, dMV, dOG, dAH = self.dQT, self.dKT, self.dMQK, self.dV, self.dMV, self.dOG, self.dAH
    with ExitStack() as esg:
        ECt = k.sb([P, NT, 4], F32, "ECt", esg)
        EC2t = k.sb([P, NT, 4], F32, "EC2t", esg)
        THRt = k.sb([P, NT, 4], F32, "THRt", esg)
        wstB = k.sb([P, 4, NT], F32, "wstB", esg)
        esr = ExitStack()
        R1 = k.sb([4, S], F32, "R1", esr)
        R2 = k.sb([4, S], F32, "R2", esr)
        R3 = k.sb([4, S], F32, "R3", esr)
        with ExitStack() as es:
            g0 = k.sb([P, 8], F32, "g0", es)
            self.fm_param(g0[:, :], self.norm_g.h[layer, 0, :])
            win = k.sb([P, 8, IN_COLS], BF16, "win", es)
            with ExitStack() as es2:
                stage = Ring(k, 2, [P, 2048], F32, "stg", es2)
                self.load_w(lambda kd, c0, cw: win[:, kd, c0:c0 + cw],
                            lambda kd, c0, cw: self.e_w_in[e, kd * P:(kd + 1) * P, c0:c0 + cw], 8, IN_COLS, stage, g0)
                self.barrier()
            cw_ = k.sb([P, 8, 4], F32, "cw", es)
            for j in range(4):
                self.fm_param(cw_[:, :, j], self.e_conv_w.h[e, j, :])
            bi = k.sb([4, 1], F32, "bi", es)
            nbf = k.sb([4, 1], F32, "nbf", es)
            with nc.allow_non_contiguous_dma(reason="tiny"):
                k.dma("sp", bi[:, :], U(self.e_b_if.h[e, 0:4].rearrange("(p o) -> p o", o=1)))
                k.dma("sp", nbf[:, :], U(self.e_b_if.h[e, 4:8].rearrange("(p o) -> p o", o=1)))
            k.ts("dve", nbf[:, :], nbf[:, :], -1.0, ALU.mult)
            hts1 = Ring(k, 2, [P, D], F32, "ht", es)
            hns1 = Ring(k, 2, [P, D], BF16, "hn", es)
            junk = k.sb([P, D], BF16, "junk", es)
            sss = Ring(k, 4, [P, 4], F32, "ss", es)
            uTs = Ring(k, 2, [P, 8, P], BF16, "uT", es)
            cq = k.sb([P, 8, P + 3], F32, "cq", es)
            k.memset("pool", cq[:, :, 0:3], 0.0)
            acc = k.sb([P, 8, P], F32, "acc", es)
            qk_o = Ring(k, 2, [P, 8, P], BF16, "qko", es)
            sq_o = Ring(k, 2, [P, 4, P], BF16, "sqo", es)
            sk_o = Ring(k, 2, [P, 4, P], BF16, "sko", es)
            tok_o = Ring(k, 3, [P, 512], BF16, "toko", es)
            gtmp = k.sb([4, P], F32, "gtmp", es)
            tps = k.ps([P, 8, P], BF16, "tps", es)
            pA = PSlots(k, 3, es, "pA")
            pT = Ring(k, 2, [P, 512], F32, "pT", es, psum=True)
            s4 = sss.next()
            hn_n = hns1.next()
            self.norm_part(hsrc, 0, hts1.next(), hn_n, s4.sub(0), s4.sub(1), junk)
            uT_n = uTs.next()
            self.tr_part(hn_n, tps, uT_n[:, :, :])
            for ti in range(NT):
                ts_ = slice(ti * P, (ti + 1) * P)
                uT = uT_n
                sq, sk, qk = sq_o.next(), sk_o.next(), qk_o.next()
                for n_ in range(4):
                    ps = pA.next()
                    for kd in range(8):
                        k.mm(ps[:, :], win[:, kd, n_ * P:(n_ + 1) * P], uT[:, kd, :], start=(kd == 0), stop=(kd == 7))
                    k.copy("act", sq[:, n_, :], ps[:, :])
                for n_ in range(4):
                    ps = pA.next()
                    for kd in range(8):
                        k.mm(ps[:, :], win[:, kd, 512 + n_ * P:512 + (n_ + 1) * P], uT[:, kd, :], start=(kd == 0), stop=(kd == 7))
                    k.act(sk[:, n_, :], ps[:, :], AF.Copy, scale=RS8)
                k.dma("pool", U(dQT[:, :, ts_]), sq[:, :, :])
                k.dma("pool", U(dKT[:, :, ts_]), sk[:, :, :])
                if ti + 1 < NT:
                    s4 = sss.next()
                    hn_n = hns1.next()
                    self.norm_part(hsrc, ti + 1, hts1.next(), hn_n, s4.sub(0), s4.sub(1), junk)
                for n_ in range(8):
                    ps = pA.next()
                    for kd in range(8):
                        k.mm(ps[:, :], win[:, kd, 1536 + n_ * P:1536 + (n_ + 1) * P], uT[:, kd, :], start=(kd == 0), stop=(kd == 7))
                    k.copy("act", cq[:, n_, 3:P + 3], ps[:, :])
                for n_ in range(8):
                    k.ts("dve", acc[:, n_, :], cq[:, n_, 0:P], cw_[:, n_, 0:1], ALU.mult)
                    for j in range(1, 4):
                        k.stt(acc[:, n_, :], cq[:, n_, j:j + P], cw_[:, n_, j:j + 1], acc[:, n_, :], ALU.mult, ALU.add)
                k.act(qk[:, :, :], acc[:, :, :], AF.Silu)
                k.copy("pool", acc[:, :, 0:3], cq[:, :, P:P + 3])
                k.copy("pool", cq[:, :, 0:3], acc[:, :, 0:3])
                k.dma("pool", U(dMQK[:, :, ts_]), qk[:, :, :])
                for col0, dd, fn in ((1024, dV, None), (2560, dMV, None), (3072, dOG, AF.Sigmoid)):
                    ps = pT.next()
                    for kd in range(8):
                        k.mm(ps[:, :], uT[:, kd, :], win[:, kd, col0:col0 + 512], start=(kd == 0), stop=(kd == 7))
                    to = tok_o.next()
                    if fn is None:
                        k.copy("dve", to[:, :], ps[:, :])
                    else:
                        k.act(to[:, :], ps[:, :], fn)
                    k.dma("pool", U(dd[ts_, :]), to[:, :])
                ps = pA.next()
                for kd in range(8):
                    k.mm(ps[0:4, :], win[:, kd, 3584:3588], uT[:, kd, :], start=(kd == 0), stop=(kd == 7))
                k.act(R1[:, ts_], ps[0:4, :], AF.Identity, bias=bi[:, 0:1])
                ps = pA.next()
                for kd in range(8):
                    k.mm(ps[0:4, :], win[:, kd, 3588:3592], uT[:, kd, :], start=(kd == 0), stop=(kd == 7))
                k.act(gtmp[:, :], ps[0:4, :], AF.Exp, bias=nbf[:, 0:1], scale=-1.0)
                k.act(R2[:, ts_], gtmp[:, :], AF.Ln, bias=1.0)
                if ti + 1 < NT:
                    uT_n = uTs.next()
                    self.tr_part(hn_n, tps, uT_n[:, :, :])
            self.barrier()
        if EVSTOP == 1:
            k.dead = True
        with ExitStack() as es:
            k.scan(R3[:, :], R2[:, :], R2[:, :], 0.0, ALU.add, ALU.max)
            k.tt("dve", R1[:, :], R1[:, :], R3[:, :], ALU.add)
            k.scan(R2[:, :], R1[:, :], R1[:, :], 0.0, ALU.max, ALU.max)
            ge = k.sb([4, NT], F32, "ge", es)
            rc = k.sb([4, NT], F32, "rc", es)
            wst = k.sb([4, NT], F32, "wst", es)
            g3 = R2.h[:, :].rearrange("p (c t) -> p c t", t=P)
            k.copy("dve", ge.v(ge.h[:, :].unsqueeze(2)), View(g3[:, :, P - 1:P], R2.buf))
            k.memset("dve", rc[:, :], 0.0)
            if NT > 1:
                k.copy("dve", rc[:, 1:NT], ge[:, 0:NT - 1])
            k.tt("dve", wst[:, :], rc[:, :], ge[:, :], ALU.subtract)
            k.act(wst[:, :], wst[:, :], AF.Exp)
            rcb = lambda: View(rc.h[:, :].unsqueeze(2).to_broadcast([4, NT, P]), rc.buf)
            r13 = R1.v(R1.h[:, :].rearrange("p (c t) -> p c t", t=P))
            r33 = R3.v(R3.h[:, :].rearrange("p (c t) -> p c t", t=P))
            k.tt("dve", r13, r13, rcb(), ALU.subtract)
            k.act(R1[:, :], R1[:, :], AF.Exp)
            k.tt("dve", r33, r33, rcb(), ALU.subtract)
            k.act(R3[:, :], R3[:, :], AF.Exp)
            pt1 = k.ps([P, 512], F32, "pt1", es)
            pt2 = k.ps([P, 512], F32, "pt2", es)
            pt3 = k.ps([P, 512], F32, "pt3", es)
            for c in range(NT):
                k.tr(pt1[:, c * 4:(c + 1) * 4], R1[0:4, c * P:(c + 1) * P], self.ident_f[0:4, 0:4])
            k.copy("dve", ECt.v(ECt.h[:, :, :].rearrange("p c h -> p (c h)")), pt1[:, 0:NT * 4])
            for c in range(NT):
                k.tr(pt2[:, c * 4:(c + 1) * 4], R3[0:4, c * P:(c + 1) * P], self.ident_f[0:4, 0:4])
            k.copy("dve", THRt.v(THRt.h[:, :, :].rearrange("p c h -> p (c h)")), pt2[:, 0:NT * 4])
            sel = k.sb([4, 4, P], F32, "sel", es)
            k.memset("pool", sel[:, :, :], 1.0)
            k.affsel(sel[:, :, :], sel[:, :, :], [[-1, 4], [0, P]], ALU.is_equal, 0.0, 0, 1)
            for h in range(4):
                k.mm(pt3[:, h * NT:(h + 1) * NT], sel[:, h, :], wst[:, :])
            k.copy("dve", wstB.v(wstB.h[:, :, :].rearrange("p h c -> p (h c)")), pt3[:, 0:4 * NT])
            k.tt("dve", EC2t[:, :, :], ECt[:, :, :], wstB.v(wstB.h[:, :, :].rearrange("p h c -> p c h")), ALU.mult)
            self.barrier()
        esr.close()
        with ExitStack() as es:
            QW = min(512, S)
            QT = k.sb([P, 4, S], BF16, "QT", es)
            KT = k.sb([P, 4, S], BF16, "KT", es)
            Vt = k.sb([P, NT, 512], BF16, "Vt", es)
            AO = k.sb([P, 4, S], BF16, "AO", es)
            k.dma("sp", QT[:, :, :], U(dQT[:, :, :]))
            k.dma("sp", KT[:, :, :], U(dKT[:, :, :]))
            k.dma("sp", Vt[:, :, :], U(dV.rearrange("(n p) d -> p n d", p=P)))
            ntri = k.sb([P, P], F32, "ntri", es)
            nones = k.sb([P, P], F32, "nones", es)
            ntri.buf.f32r = True
            nones.buf.f32r = True
            ctmp = k.sb([P, P], F32, "ctmp", es)
            k.memset("pool", ctmp[:, :], -1.0)
            k.copy("dve", nones[:, :], ctmp[:, :])
            k.affsel(ctmp[:, :], ctmp[:, :], [[-1, P]], ALU.is_ge, 0.0, 0, 1)
            k.copy("dve", ntri[:, :], ctmp[:, :])
            nd = QW // P
            dm = k.sb([P, nd, QW], F32, "dm", es)
            dmb = k.sb([P, nd, QW], BF16, "dmb", es)
            k.memset("pool", dm[:, :, :], 1.0)
            for jj in range(nd):
                k.affsel(dm[:, jj, :], dm[:, jj, :], [[1, QW]], ALU.is_gt, 0.0, -P * jj, -1)
            k.copy("dve", dmb[:, :, :], dm[:, :, :])
            er = [Ring(k, 2, [P, QW], F32, "e", es) for _ in range(2)]
            spr = [Ring(k, 4, [P, QW], F32, "sp", es) for _ in range(2)]
            rsr = [Ring(k, 3, [P, QW], F32, "rs", es) for _ in range(2)]
            wr_ = [Ring(k, 3, [P, QW], BF16, "w", es) for _ in range(2)]
            for par in range(2):
                for t_ in spr[par].t + rsr[par].t:
                    t_.buf.f32r = True
            pz = [Ring(k, 1, [P, 512], F32, "pz", es, psum=True) for _ in range(2)]
            pd = [Ring(k, 1, [P, 512], F32, "pd", es, psum=True) for _ in range(2)]
            pav = [Ring(k, 1, [P, 512], F32, "pav", es, psum=True) for _ in range(2)]
            for tq in range(S // QW):
                t0 = tq * QW
                Jtop = (t0 + QW) // P - 1
                for hp in range(4):
                    c = hp
                    RSs = [None, None]
                    avs = [pav[0].next(), pav[1].next()]
                    hsl = [slice(par * 64, par * 64 + 64) for par in range(2)]
                    qvs = [QT[hsl[par], c, t0:t0 + QW] for par in range(2)]

                    def front(J):
                        s0 = J * P
                        diag = (s0 + P - 1 >= t0)
                        jj = (s0 - t0) // P if diag else None
                        kvs = [KT[hsl[par], c, s0:s0 + P] for par in range(2)]
                        zts, es_, sps = [], [], []
                        for par in range(2):
                            zt = pz[par].next()
                            k.mm(zt[:, 0:QW], kvs[par], qvs[par])
                            zts.append(zt)
                        for par in range(2):
                            e_ = er[par].next()
                            k.act(e_[:, :], zts[par][:, 0:QW], AF.Exp)
                            es_.append(e_)
                        for par in range(2):
                            sp = spr[par].next()
                            k.act(sp[:, :], es_[par][:, :], AF.Ln, bias=1.0)
                            sps.append(sp)
                        if diag:
                            for par in range(2):
                                k.tt("dve", sps[par][:, :], sps[par][:, :], dm[:, jj, :], ALU.mult)
                        return (J, diag, jj, kvs, sps)

                    def back1(fr):
                        J, diag, jj, kvs, sps = fr
                        ds, ws = [], []
                        for par in range(2):
                            RS = RSs[par]
                            d_ = pd[par].next()
                            k.mm(d_[:, 0:QW], kvs[par], qvs[par], start=True, stop=False)
                            k.mm(d_[:, 0:QW], ntri[:, :], sps[par][:, :], start=False, stop=(RS is None))
                            if RS is not None:
                                k.mm(d_[:, 0:QW], nones[:, :], RS[:, :], start=False, stop=True)
                            ds.append(d_)
                        for par in range(2):
                            w_ = wr_[par].next()
                            k.act(w_[:, :], ds[par][:, 0:QW], AF.Exp)
                            ws.append(w_)
                        if diag:
                            for par in range(2):
                                k.tt("dve", ws[par][:, :], ws[par][:, :], dmb[:, jj, :], ALU.mult)
                        if J > 0:
                            for par in range(2):
                                if RSs[par] is None:
                                    RSs[par] = sps[par]
                                else:
                                    RSn = rsr[par].next()
                                    k.tt("dve", RSn[:, :], RSs[par][:, :], sps[par][:, :], ALU.add)
                                    RSs[par] = RSn
                        return (J, ws)

                    def back2(pend):
                        J, ws = pend
                        for par in range(2):
                            h = 2 * hp + par
                            k.mm(avs[par][hsl[par], 0:QW], Vt[:, J, h * 64:(h + 1) * 64], ws[par][:, :],
                                 start=(J == Jtop), stop=(J == 0))

                    fr = front(Jtop)
                    pend = None
                    for J in range(Jtop, -1, -1):
                        nxt = front(J - 1) if J > 0 else None
                        cur = back1(fr)
                        if pend is not None:
                            back2(pend)
                        pend = cur
                        fr = nxt
                    back2(pend)
                    for par in range(2):
                        k.copy("dve", AO[hsl[par], c, t0:t0 + QW], avs[par][hsl[par], 0:QW])
            k.dma("sp", U(dAH[:, 0:4, :]), AO[:, :, :])
            self.barrier()
        if EVSTOP == 3:
            k.dead = True
        with ExitStack() as es:
            MQK = k.sb([P, 8, S], BF16, "MQK", es)
            MV = k.sb([P, NT, 512], BF16, "MV", es)
            OG = k.sb([P, NT, 512], BF16, "OG", es)
            k.dma("sp", MQK[:, :, :], U(dMQK[:, :, :]))
            k.dma("sp", MV[:, :, :], U(dMV.rearrange("(n p) d -> p n d", p=P)))
            k.dma("sp", OG[:, :, :], U(dOG.rearrange("(n p) d -> p n d", p=P)))
            hgb = k.sb([P, 512], F32, "hgb", es)
            k.dma("sp", hgb[:, :], U(self.e_head_g.h[e, :, :].rearrange("h d -> (h d)").partition_broadcast(P)))
            mlm = k.sb([P, P], F32, "mlm", es)
            k.memset("pool", mlm[:, :], RSD)
            k.affsel(mlm[:, :], mlm[:, :], [[1, P]], ALU.is_ge, 0.0, 0, -1)
            Cst = [k.sb([P, 132], F32, "Cst%d" % h, es) for h in range(4)]
            Cbf = [k.sb([P, 132], BF16, "Cbf%d" % h, es) for h in range(4)]
            for h in range(4):
                k.memset("pool", Cst[h][:, :], 0.0)
                k.memset("pool", Cbf[h][:, :], 0.0)
            va1 = Ring(k, 4, [P, 132], BF16, "va1", es)
            va2 = Ring(k, 4, [P, 132], BF16, "va2", es)
            pmr = Ring(k, 4, [P, P], BF16, "pm", es)
            ktr = Ring(k, 2, [P, 2, P], BF16, "kt", es)
            sm = Ring(k, 8, [P, 8], F32, "sm", es)
            tmpf = Ring(k, 4, [P, P], F32, "tmpf", es)
            junk = k.sb([P, 2, P], BF16, "junk", es)
            Htk = Ring(k, 2, [P, 512], BF16, "Htk", es)
            HTs = Ring(k, 2, [P, 4, P], BF16, "HTs", es)
            pst = Ring(k, 2, [P, 512], F32, "pst", es, psum=True)
            px = Ring(k, 2, [P, 512], F32, "px", es, psum=True)
            pu = Ring(k, 2, [P, 512], F32, "pu", es, psum=True)
            pkt = Ring(k, 1, [P, 8, P], BF16, "pkt", es, psum=True)
            pht = Ring(k, 1, [P, 8, P], BF16, "pht", es, psum=True)
            for c in range(NT):
                cs = slice(c * P, (c + 1) * P)
                Ht = Htk.next()
                for hp2 in range(2):
                    hh = [2 * hp2, 2 * hp2 + 1]
                    hcs = [slice(h * P, (h + 1) * P) for h in hh]
                    qTs = [MQK[:, h, cs] for h in hh]
                    kTs = [MQK[:, 4 + h, cs] for h in hh]
                    v1s, v2s = [], []
                    for i, h in enumerate(hh):
                        v1, v2 = va1.next(), va2.next()
                        k.ts("pool", v1[:, 0:P], MV[:, c, hcs[i]], ECt[:, c, h:h + 1], ALU.mult)
                        k.copy("pool", v1[:, P:P + 1], ECt[:, c, h:h + 1])
                        k.ts("pool", v2[:, 0:P], MV[:, c, hcs[i]], EC2t[:, c, h:h + 1], ALU.mult)
                        k.copy("pool", v2[:, P:P + 1], EC2t[:, c, h:h + 1])
                        v1s.append(v1)
                        v2s.append(v2)
                    sts = []
                    for i in range(2):
                        st_ = pst.next()
                        k.mm(st_[:, 0:P], kTs[i], qTs[i])
                        sts.append(st_)
                    kp_ = pkt.next()
                    for i in range(2):
                        k.tr(kp_[:, i, :], kTs[i], self.ident_b[:, :])
                    pms = []
                    for i in range(2):
                        pm = pmr.next()
                        k.tt("dve", pm[:, :], sts[i][:, 0:P], mlm[:, :], ALU.mult)
                        pms.append(pm)
                    kt_ = ktr.next()
                    k.copy("act", kt_[:, :, :], kp_[:, 0:2, :])
                    xs, us = [], []
                    for i, h in enumerate(hh):
                        x_ = px.next()
                        k.mm(x_[:, 0:P + 1], pms[i][:, :], v1s[i][:, 0:P + 1], start=True, stop=False)
                        k.mm(x_[:, 0:P + 1], qTs[i], Cbf[h][:, 0:P + 1], start=False, stop=True)
                        xs.append(x_)
                    for i in range(2):
                        u_ = pu.next()
                        k.mm(u_[:, 0:P + 1], kt_[:, i, :], v2s[i][:, 0:P + 1])
                        us.append(u_)
                    for i, h in enumerate(hh):
                        k.stt(Cst[h][:, 0:P + 1], Cst[h][:, 0:P + 1], wstB[:, h, c:c + 1], us[i][:, 0:P + 1], ALU.mult, ALU.add)
                    for i, h in enumerate(hh):
                        k.ts("pool", Cbf[h][:, 0:P + 1], Cst[h][:, 0:P + 1], RSD, ALU.mult)
                    ss_ = [sm.next(), sm.next()]
                    for i in range(2):
                        k.ts("dve", ss_[i][:, 5:6], xs[i][:, P:P + 1], -1.0, ALU.mult)
                    for i in range(2):
                        k.tt("dve", ss_[i][:, 0:1], xs[i][:, P:P + 1], ss_[i][:, 5:6], ALU.max)
                    for i, h in enumerate(hh):
                        k.ts("dve", ss_[i][:, 0:1], ss_[i][:, 0:1], THRt[:, c, h:h + 1], ALU.max)
                    for i in range(2):
                        k.recip(ss_[i][:, 1:2], ss_[i][:, 0:1])
                    for i in range(2):
                        k.act(View(junk.h[:, i, :], Buf()), xs[i][:, 0:P], AF.Square, scale=ss_[i][:, 1:2], accum=ss_[i][:, 2:3])
                    for i in range(2):
                        k.act(ss_[i][:, 3:4], ss_[i][:, 2:3], AF.Ln, bias=self.eps_t[:, 0:1], scale=1.0 / P)
                    for i in range(2):
                        k.act(ss_[i][:, 3:4], ss_[i][:, 3:4], AF.Exp, scale=-0.5)
                    for i in range(2):
                        k.tt("dve", ss_[i][:, 4:5], ss_[i][:, 3:4], ss_[i][:, 1:2], ALU.mult)
                    tfs = []
                    for i in range(2):
                        tf = tmpf.next()
                        k.stt(tf[:, :], xs[i][:, 0:P], ss_[i][:, 4:5], hgb[:, hcs[i]], ALU.mult, ALU.mult)
                        tfs.append(tf)
                    for i in range(2):
                        k.tt("pool", Ht[:, hcs[i]], tfs[i][:, :], OG[:, c, hcs[i]], ALU.mult)
                hp = pht.next()
                for h in range(4):
                    k.tr(hp[:, h, :], Ht[:, h * P:(h + 1) * P], self.ident_b[:, :])
                HT = HTs.next()
                k.copy("act", HT[:, :, :], hp[:, 0:4, :])
                k.dma("pool", U(dAH[:, 4:8, cs]), HT[:, :, :])
            self.barrier()
        if EVSTOP == 4:
            k.dead = True
        with ExitStack() as es:
            wo = k.sb([P, 8, D], BF16, "wo", es)
            with ExitStack() as es2:
                stage = Ring(k, 2, [P, 2048], F32, "stg", es2)
                self.load_w(lambda kd, c0, cw: wo[:, kd, c0:c0 + cw],
                            lambda kd, c0, cw: self.e_w_out[e, kd * P:(kd + 1) * P, c0:c0 + cw], 8, D, stage, None)
                self.barrier()
            g1b = k.sb([P, D], F32, "g1b", es)
            k.dma("sp", g1b[:, :], U(self.norm_g.h[layer, 1, :].partition_broadcast(P)))
            ahs = Ring(k, 2, [P, 8, P], BF16, "ah", es)
            hts = Ring(k, 2, [P, D], F32, "ht", es)
            yts = Ring(k, 2, [P, D], F32, "yt", es)
            junk = k.sb([P, D], BF16, "junk", es)
            sss = Ring(k, 4, [P, 4], F32, "ss", es)
            dpsr = Ring(k, 4, [P, 512], F32, "dps", es, psum=True)
            for ti in range(NT):
                ts_ = slice(ti * P, (ti + 1) * P)
                ah = ahs.next()
                k.dma("sp", ah[:, :, :], U(dAH[:, :, ts_]))
                ht = hts.next()
                k.dma("sp", ht[:, :], hsrc.sub(ti)[ts_, :])
                dps = [dpsr.next(), dpsr.next()]
                for hf in range(2):
                    for c in range(8):
                        k.mm(dps[hf][:, :], ah[:, c, :], wo[:, c, hf * 512:(hf + 1) * 512], start=(c == 0), stop=(c == 7))
                self.post_norm_add(dps, ht, g1b, hdst, ti, sss.next(), junk, yts.next())
            k.dead = False
            self.barrier()


Prog.even = _even


def build_full(S=SEQ, depth=DEPTH):
    prog = Prog(S=S)
    prog.declare()
    prog.consts()
    cur = prog.x
    bufs = [prog.hA, prog.hB]
    bi = 0
    for layer in range(depth):
        last = (layer == depth - 1)
        mid = bufs[bi]
        bi ^= 1
        if layer % 2 == 0:
            prog.even(layer, cur, mid)
        else:
            prog.rwkv(layer, cur, mid)
        prog.barrier()
        dst = prog.out if last else bufs[bi]
        bi ^= 1
        prog.mlp(layer, mid, dst)
        prog.barrier()
        cur = dst
    prog.finish(prog.out)
    return prog


def kernel(**inputs):
    inputs = {k_: np.asarray(v) for k_, v in inputs.items()}
    prog = build_full()
    in_maps = [prog.in_map(inputs, b) for b in range(BATCH)]
    res = run_bass_kernel_spmd(prog.nc, in_maps, core_ids=list(range(BATCH)))
    return np.stack([np.asarray(res.results[b]["out"]) for b in range(BATCH)], axis=0).astype(np.float32)
```
